# Optimizing a Trainium2 kernel written in Bass

```python
import jax, jax.numpy as jnp
from jax import lax
import numpy as np

D_MODEL = 1024
BATCH = 16
SEQ = 4096
DEPTH = 4

N_BRANCHES = 4
BRANCH_WIDTH = 512
RMS_EPS = 1e-6
S5_GROUP = 16
S5_GROUPS = BRANCH_WIDTH // S5_GROUP
S5_STATE = 64
S5_DT_MIN = 1e-3
S5_DT_MAX = 1e-1
POOL_WINDOWS = (2, 4, 8, 16)
POOL_GROUP = BRANCH_WIDTH // len(POOL_WINDOWS)
SCONV_WIDTH = 3
MLSTM_HEADS = 4
MLSTM_HEAD_DIM = BRANCH_WIDTH // MLSTM_HEADS
MLSTM_CONV_WIDTH = 4
MLSTM_CHUNK = 64
MLSTM_F_BIAS_LO = 3.0
MLSTM_F_BIAS_HI = 6.0

IN_SIZES = (BRANCH_WIDTH, BRANCH_WIDTH,
            BRANCH_WIDTH, BRANCH_WIDTH,
            BRANCH_WIDTH, BRANCH_WIDTH, BRANCH_WIDTH, BRANCH_WIDTH,
            2 * BRANCH_WIDTH, BRANCH_WIDTH, BRANCH_WIDTH, MLSTM_HEADS, MLSTM_HEADS, BRANCH_WIDTH,
            N_BRANCHES * D_MODEL)
N_IN = sum(IN_SIZES)

kernel_name = 'hybrid_s5_pool_sconv_mlstm_gated_parallel'


def rms_norm(x, w):
    x32 = x.astype(jnp.float32)
    y = x32 * lax.rsqrt(jnp.mean(x32 * x32, axis=-1, keepdims=True) + RMS_EPS)
    return (y * w.astype(jnp.float32)).astype(x.dtype)


def causal_depthwise_conv(x, w):
    k_width = w.shape[0]
    s = x.shape[1]
    xp = jnp.pad(x, ((0, 0), (k_width - 1, 0), (0, 0)))
    return sum(xp[:, k:k + s, :] * w[k] for k in range(k_width))


def s5_ssm(u, a_re, a_im, log_dt, b_re, b_im, c_re, c_im, d_skip):
    bsz, s, _ = u.shape
    f32 = jnp.float32
    u32 = u.astype(f32)
    lam = lax.complex(a_re.astype(f32), a_im.astype(f32))
    dt = jnp.exp(log_dt.astype(f32))[:, None]
    a_bar = jnp.exp(lam * dt)
    b_mat = lax.complex(b_re.astype(f32), b_im.astype(f32))
    b_bar = ((a_bar - 1.0) / lam)[..., None] * b_mat
    c_mat = lax.complex(c_re.astype(f32), c_im.astype(f32))
    ug = u32.reshape(bsz, s, S5_GROUPS, S5_GROUP).astype(jnp.complex64)
    bu = jnp.einsum('bsgp,gnp->bsgn', ug, b_bar)
    a_seq = jnp.broadcast_to(a_bar, (s, S5_GROUPS, S5_STATE))

    def combine(left, right):
        a_l, x_l = left
        a_r, x_r = right
        return a_r * a_l, a_r * x_l + x_r

    def scan_one(bu_b):
        return lax.associative_scan(combine, (a_seq, bu_b), axis=0)[1]

    states = jax.vmap(scan_one)(bu)
    y = jnp.einsum('bsgn,gpn->bsgp', states, c_mat).real.reshape(bsz, s, BRANCH_WIDTH)
    y = y + d_skip.astype(f32) * u32
    return y.astype(u.dtype)


def pool_mixer(u, pool_w, pool_scale):
    bsz, s, _ = u.shape
    u32 = u.astype(jnp.float32).reshape(bsz, s, len(POOL_WINDOWS), POOL_GROUP)
    cs = jnp.cumsum(u32, axis=1)
    t = jnp.arange(1, s + 1, dtype=jnp.float32)[None, :, None]
    outs = []
    for gi, win in enumerate(POOL_WINDOWS):
        cs_g = cs[:, :, gi]
        prev = jnp.pad(cs_g, ((0, 0), (win, 0), (0, 0)))[:, :s]
        outs.append((cs_g - prev) / jnp.minimum(t, win) - u32[:, :, gi])
    pooled = jnp.stack(outs, axis=2)
    mixed = jnp.einsum('bsgc,gcd->bsgd', pooled, pool_w.astype(jnp.float32))
    return (mixed.reshape(bsz, s, BRANCH_WIDTH) * pool_scale.astype(jnp.float32)).astype(u.dtype)


def mlstm_mixer(qk, v, o_pre, i_pre, f_pre, conv_w, b_i, b_f, norm_w):
    bsz, s, _ = v.shape
    hh, dh, ll = MLSTM_HEADS, MLSTM_HEAD_DIM, MLSTM_CHUNK
    nc = s // ll
    f32 = jnp.float32
    qk = jax.nn.silu(causal_depthwise_conv(qk, conv_w)).astype(f32)
    q, k = jnp.split(qk, 2, axis=-1)
    q = q * (dh ** -0.5)

    def chunk_heads(t):
        return t.astype(f32).reshape(bsz, nc, ll, hh, dh).transpose(0, 3, 1, 2, 4)

    def chunk_gate(t):
        return t.astype(f32).reshape(bsz, nc, ll, hh).transpose(0, 3, 1, 2)

    qc, kc, vc = chunk_heads(q), chunk_heads(k), chunk_heads(v)
    i_log = chunk_gate(i_pre + b_i)
    log_f = jax.nn.log_sigmoid(chunk_gate(f_pre + b_f))
    b = jnp.cumsum(log_f, axis=-1)
    g = b[..., -1]

    a = g[..., None] - b + i_log
    m_loc = jnp.max(a, axis=-1)
    w_loc = jnp.exp(a - m_loc[..., None])
    c_loc = jnp.einsum('bhcl,bhcld,bhcle->bhcde', w_loc, kc, vc)
    n_loc = jnp.einsum('bhcl,bhcld->bhcd', w_loc, kc)

    def step(carry, inp):
        c_st, n_st, m_st = carry
        g_c, m_c, c_c, n_c = inp
        m_new = jnp.maximum(g_c + m_st, m_c)
        s_old = jnp.exp(g_c + m_st - m_new)
        s_new = jnp.exp(m_c - m_new)
        c_next = s_old[..., None, None] * c_st + s_new[..., None, None] * c_c
        n_next = s_old[..., None] * n_st + s_new[..., None] * n_c
        return (c_next, n_next, m_new), (c_st, n_st, m_st)

    init = (jnp.zeros((bsz, hh, dh, dh), f32), jnp.zeros((bsz, hh, dh), f32), jnp.zeros((bsz, hh), f32))
    xs = (jnp.moveaxis(g, 2, 0), jnp.moveaxis(m_loc, 2, 0), jnp.moveaxis(c_loc, 2, 0), jnp.moveaxis(n_loc, 2, 0))
    _, (c_prev, n_prev, m_prev) = lax.scan(step, init, xs)
    c_prev = jnp.moveaxis(c_prev, 0, 2)
    n_prev = jnp.moveaxis(n_prev, 0, 2)
    m_prev = jnp.moveaxis(m_prev, 0, 2)

    causal = jnp.tril(jnp.ones((ll, ll), dtype=bool))
    d_log = jnp.where(causal, b[..., :, None] - b[..., None, :] + i_log[..., None, :], -jnp.inf)
    e_log = b + m_prev[..., None]
    m_t = jnp.maximum(jnp.max(d_log, axis=-1), e_log)
    w_intra = jnp.exp(d_log - m_t[..., None]) * jnp.einsum('bhcld,bhcmd->bhclm', qc, kc)
    s_inter = jnp.exp(e_log - m_t)
    num = (jnp.einsum('bhclm,bhcme->bhcle', w_intra, vc)
           + s_inter[..., None] * jnp.einsum('bhcld,bhcde->bhcle', qc, c_prev))
    den = jnp.sum(w_intra, axis=-1) + s_inter * jnp.einsum('bhcld,bhcd->bhcl', qc, n_prev)
    h = num / jnp.maximum(jnp.abs(den), jnp.exp(-m_t))[..., None]
    h = h.transpose(0, 2, 3, 1, 4).reshape(bsz, s, hh, dh)
    h = h * jax.nn.sigmoid(o_pre.astype(f32)).reshape(bsz, s, hh, dh)
    h = h * lax.rsqrt(jnp.mean(h * h, axis=-1, keepdims=True) + RMS_EPS)
    return (h.reshape(bsz, s, BRANCH_WIDTH) * norm_w.astype(f32)).astype(v.dtype)


def setup_inputs(seed: int = 0) -> dict:
    key = jax.random.key(seed)
    ks = jax.random.split(key, 22)
    f32 = jnp.float32
    W, G, N, P, H = BRANCH_WIDTH, S5_GROUPS, S5_STATE, S5_GROUP, MLSTM_HEADS
    nrm = lambda k, shape: jax.random.normal(k, shape, f32)
    x = nrm(ks[0], (BATCH, SEQ, D_MODEL))
    norm_pre_w = 1.0 + 0.02 * nrm(ks[1], (DEPTH, D_MODEL))
    norm_post_w = 1.0 + 0.02 * nrm(ks[2], (DEPTH, D_MODEL))
    w_in = nrm(ks[3], (DEPTH, D_MODEL, N_IN)) * D_MODEL ** -0.5
    s5_A_re = -0.5 + 0.01 * nrm(ks[4], (DEPTH, G, N))
    s5_A_im = jnp.pi * jnp.arange(N, dtype=f32)[None, None, :] + 0.01 * nrm(ks[5], (DEPTH, G, N))
    s5_log_dt = jax.random.uniform(ks[6], (DEPTH, G), f32, float(np.log(S5_DT_MIN)), float(np.log(S5_DT_MAX)))
    s5_B_re = nrm(ks[7], (DEPTH, G, N, P)) * (2 * P) ** -0.5
    s5_B_im = nrm(ks[8], (DEPTH, G, N, P)) * (2 * P) ** -0.5
    s5_C_re = nrm(ks[9], (DEPTH, G, P, N)) * N ** -0.5
    s5_C_im = nrm(ks[10], (DEPTH, G, P, N)) * N ** -0.5
    s5_D = nrm(ks[11], (DEPTH, W))
    s5_w_glu = nrm(ks[12], (DEPTH, W, W)) * W ** -0.5
    pool_w = nrm(ks[13], (DEPTH, len(POOL_WINDOWS), POOL_GROUP, POOL_GROUP)) * POOL_GROUP ** -0.5
    pool_scale = 1.0 + 0.02 * nrm(ks[14], (DEPTH, W))
    sconv_w = nrm(ks[15], (DEPTH, SCONV_WIDTH, W)) * SCONV_WIDTH ** -0.5
    mlstm_conv_w = nrm(ks[16], (DEPTH, MLSTM_CONV_WIDTH, 2 * W)) * MLSTM_CONV_WIDTH ** -0.5
    mlstm_b_i = 0.1 * nrm(ks[17], (DEPTH, H))
    mlstm_b_f = jnp.linspace(MLSTM_F_BIAS_LO, MLSTM_F_BIAS_HI, H, dtype=f32)[None, :] + 0.01 * nrm(ks[18], (DEPTH, H))
    mlstm_norm_w = 1.0 + 0.02 * nrm(ks[19], (DEPTH, W))
    w_branch = nrm(ks[20], (DEPTH, N_BRANCHES, W, D_MODEL)) * W ** -0.5
    w_out = nrm(ks[21], (DEPTH, D_MODEL, D_MODEL)) * D_MODEL ** -0.5
    return {'x': x, 'norm_pre_w': norm_pre_w, 'norm_post_w': norm_post_w, 'w_in': w_in,
            's5_A_re': s5_A_re, 's5_A_im': s5_A_im, 's5_log_dt': s5_log_dt,
            's5_B_re': s5_B_re, 's5_B_im': s5_B_im, 's5_C_re': s5_C_re, 's5_C_im': s5_C_im,
            's5_D': s5_D, 's5_w_glu': s5_w_glu, 'pool_w': pool_w, 'pool_scale': pool_scale,
            'sconv_w': sconv_w, 'mlstm_conv_w': mlstm_conv_w, 'mlstm_b_i': mlstm_b_i,
            'mlstm_b_f': mlstm_b_f, 'mlstm_norm_w': mlstm_norm_w, 'w_branch': w_branch, 'w_out': w_out}


def reference(x, norm_pre_w, norm_post_w, w_in, s5_A_re, s5_A_im, s5_log_dt, s5_B_re, s5_B_im,
              s5_C_re, s5_C_im, s5_D, s5_w_glu, pool_w, pool_scale, sconv_w, mlstm_conv_w,
              mlstm_b_i, mlstm_b_f, mlstm_norm_w, w_branch, w_out):
    bsz, s, _ = x.shape
    split_points = np.cumsum(IN_SIZES)[:-1].tolist()
    for l in range(DEPTH):
        h = rms_norm(x, norm_pre_w[l])
        proj = h @ w_in[l]
        (s5_u, s5_z, pool_u, pool_z, sc_x, sc_b, sc_c, sc_z,
         ml_qk, ml_v, ml_o, ml_i, ml_f, ml_z, gate_pre) = jnp.split(proj, split_points, axis=-1)
        y_a = s5_ssm(s5_u, s5_A_re[l], s5_A_im[l], s5_log_dt[l], s5_B_re[l], s5_B_im[l],
                     s5_C_re[l], s5_C_im[l], s5_D[l])
        y_a = jax.nn.gelu(y_a)
        y_a = y_a * jax.nn.sigmoid(y_a @ s5_w_glu[l])
        y_b = pool_mixer(pool_u, pool_w[l], pool_scale[l])
        y_c = sc_b * causal_depthwise_conv(sc_c * sc_x, sconv_w[l])
        y_d = mlstm_mixer(ml_qk, ml_v, ml_o, ml_i, ml_f, mlstm_conv_w[l], mlstm_b_i[l],
                          mlstm_b_f[l], mlstm_norm_w[l])
        gates = jax.nn.sigmoid(gate_pre).reshape(bsz, s, N_BRANCHES, D_MODEL)
        branches = ((y_a, s5_z), (y_b, pool_z), (y_c, sc_z), (y_d, ml_z))
        merged = sum(gates[:, :, bi] * ((y * jax.nn.silu(z)) @ w_branch[l, bi])
                     for bi, (y, z) in enumerate(branches))
        out = merged @ w_out[l]
        x = x + rms_norm(out, norm_post_w[l])
    return x
```

```python
import math
import contextlib
import numpy as np
import concourse.bass as bass
import concourse.mybir as mybir
from concourse.bass_utils import run_bass_kernel_spmd

F32 = mybir.dt.float32
BF16 = mybir.dt.bfloat16
I32 = mybir.dt.int32
AF = mybir.ActivationFunctionType
ALU = mybir.AluOpType

SAME_SYNC = True
D = 1024
W = 512
NIN = 10760
T = 512
EPS = 1e-6
C_S5U, C_S5Z, C_PU, C_PZ, C_SX, C_SB, C_SC, C_SZ = 0, 512, 1024, 1536, 2048, 2560, 3072, 3584
C_Q, C_K, C_V, C_O, C_IF, C_MZ, C_G = 4096, 4608, 5120, 5632, 6144, 6152, 6664
POOL_WINDOWS = (2, 4, 8, 16)


class Tok:
    __slots__ = ("sem", "val")

    def __init__(self, sem, val):
        self.sem = sem
        self.val = val


class Buf:
    def __init__(self, name=""):
        self.name = name
        self.w = None
        self.r = {}


class DSem:
    def __init__(self, sem):
        self.sem = sem
        self.cnt = 0


class KB:
    def __init__(self, nc, es):
        self.nc = nc
        self.es = es
        self.eng = {"pe": nc.tensor, "act": nc.scalar, "dve": nc.vector, "pool": nc.gpsimd, "sp": nc.sync}
        self.sem = {e: es.enter_context(nc.semaphore("s_" + e)) for e in self.eng}
        self.cnt = {e: 0 for e in self.eng}
        self.seen = {e: {} for e in self.eng}
        self.nins = 0
        self.uid = 0

    def dsem(self, name):
        return DSem(self.es.enter_context(self.nc.semaphore(name)))

    def new_epoch(self, tag):
        self.sem = {e: self.es.enter_context(self.nc.semaphore("s_" + e + tag)) for e in self.eng}
        self.cnt = {e: 0 for e in self.eng}

    def sb(self, name, shape, dt):
        return self.es.enter_context(self.nc.sbuf_tensor(name, list(shape), dt))

    def _wait(self, e, toks):
        need = {}
        for t in toks:
            if t is None:
                continue
            k = id(t.sem)
            if k not in need or need[k].val < t.val:
                need[k] = t
        for k, t in need.items():
            if t.sem is self.sem[e] and (e == "pe" or not SAME_SYNC):
                continue
            if self.seen[e].get(k, 0) >= t.val:
                continue
            self.eng[e].wait_ge(t.sem, t.val)
            self.seen[e][k] = t.val

    def _deps(self, reads, writes):
        toks = []
        for b in reads:
            toks.append(b.w)
        for b in writes:
            toks.append(b.w)
            toks.extend(b.r.values())
        return toks

    def _upd(self, tok, reads, writes):
        k = id(tok.sem)
        for b in reads:
            b.r[k] = tok
        for b in writes:
            b.w = tok
            b.r = {}

    def op(self, e, fn, reads=(), writes=()):
        self._wait(e, self._deps(reads, writes))
        ins = fn(self.eng[e])
        self.cnt[e] += 1
        ins.then_inc(self.sem[e], 1)
        tok = Tok(self.sem[e], self.cnt[e])
        self._upd(tok, reads, writes)
        self.nins += 1
        return tok

    def dma(self, e, ds, fn, reads=(), writes=()):
        self._wait(e, self._deps(reads, writes))
        ins = fn(self.eng[e])
        ds.cnt += 16
        ins.then_inc(ds.sem, 16)
        tok = Tok(ds.sem, ds.cnt)
        self._upd(tok, reads, writes)
        self.nins += 1
        return tok


def host_consts():
    ident = np.eye(128, dtype=np.float32)
    m = np.arange(128)[:, None]
    l = np.arange(128)[None, :]
    tri = (m <= l).astype(np.float32)
    ones = np.ones((128, 128), np.float32)
    band = np.zeros((3, 4, 128, 128), np.float32)
    for g, win in enumerate(POOL_WINDOWS):
        dlt = l - m
        inw = (dlt >= 0) & (dlt < win)
        band[0, g] = inw / win - ident
        dp = l + 128 - m
        band[1, g] = ((dp >= 0) & (dp < win)) / win
        band[2, g] = inw / np.minimum(l + 1, win) - ident
    return {"c_ident": ident, "c_tri": tri, "c_ones": ones,
            "c_band": np.ascontiguousarray(band.transpose(2, 0, 1, 3)).reshape(128, 12 * 128)}


def build(L, NSEQ, NT, en=(1, 1, 1, 1), dbg=False):
    S = NT * T
    nc = bass.Bass("TRN2", target_bir_lowering=False)
    es = contextlib.ExitStack()
    kb = KB(nc, es)

    def din(name, shape):
        return nc.dram_tensor(name, list(shape), F32, kind="ExternalInput").ap()

    x_d = din("x", [NSEQ, S, D])
    npw_d = din("norm_pre_w", [L, D]); ppw_d = din("norm_post_w", [L, D])
    win_d = din("w_in", [L, D, NIN])
    are_d = din("s5_A_re", [L, 32, 64]); aim_d = din("s5_A_im", [L, 32, 64]); ldt_d = din("s5_log_dt", [L, 32])
    bre_d = din("s5_B_re", [L, 32, 64, 16]); bim_d = din("s5_B_im", [L, 32, 64, 16])
    cre_d = din("s5_C_re", [L, 32, 16, 64]); cim_d = din("s5_C_im", [L, 32, 16, 64])
    s5d_d = din("s5_D", [L, W]); glu_d = din("s5_w_glu", [L, W, W])
    pw_d = din("pool_w", [L, 4, 128, 128]); psc_d = din("pool_scale", [L, W])
    scw_d = din("sconv_w", [L, 3, W]); mcw_d = din("mlstm_conv_w", [L, 4, 2 * W])
    mbi_d = din("mlstm_b_i", [L, 4]); mbf_d = din("mlstm_b_f", [L, 4]); mnw_d = din("mlstm_norm_w", [L, W])
    wbr_d = din("w_branch", [L, 4, W, D]); wo_d = din("w_out", [L, D, D])
    cid_d = din("c_ident", [128, 128]); ctri_d = din("c_tri", [128, 128]); cone_d = din("c_ones", [128, 128])
    cband_d = din("c_band", [128, 12 * 128])
    y_d = nc.dram_tensor("y", [NSEQ, S, D], F32, kind="ExternalOutput").ap()
    dbg_d = nc.dram_tensor("dbg", [4, 128, 4, T], F32, kind="ExternalOutput").ap() if dbg else None
    scr_d = [nc.dram_tensor("xscr%d" % i, [NSEQ, S, D], F32, kind="Internal").ap() for i in range(2)] if L > 1 else []
    dram_bufs = {}

    def dbuf(key):
        if key not in dram_bufs:
            dram_bufs[key] = Buf("dram")
        return dram_bufs[key]

    uid = [0]

    def sbt(shape, dt, name=None):
        uid[0] += 1
        return kb.sb((name or "t") + str(uid[0]), shape, dt), Buf(name or "t")
    sbt_global = sbt

    NPS = 8
    psb = [es.enter_context(nc.psum_tensor("ps%d" % i, [128, 512], F32)) for i in range(NPS)]
    psB = [Buf("ps%d" % i) for i in range(NPS)]
    psi = [0]

    def nextps():
        i = psi[0] % NPS
        psi[0] += 1
        return psb[i], psB[i]

    def E(e, fn, r=(), w=()):
        return kb.op(e, fn, r, w)

    cdsem = kb.dsem("cdsem")

    cbufs = []

    def cbarrier():
        for b_ in cbufs:
            b_.w = Tok(cdsem.sem, cdsem.cnt)
        del cbufs[:]

    def cload(e, out_ap, in_ap, wb, slow=False):
        cbufs.append(wb)
        return kb.dma(e, cdsem, lambda q: q.dma_start(out=out_ap, in_=in_ap, allow_slow_non_contiguous=True) if slow
                      else q.dma_start(out=out_ap, in_=in_ap), writes=[wb])

    identf, Bidf = sbt([128, 128], F32, "identf")
    identb, Bidb = sbt([128, 128], BF16, "identb")
    trif, Btrif = sbt([128, 128], F32, "trif")
    onesf, Bonesf = sbt([128, 128], F32, "onesf")
    band, Bband = sbt([128, 3, 4, 128], BF16, "band")
    cload("sp", identf[:], cid_d, Bidf)
    cload("pool", identb[:], cid_d, Bidb)
    cload("sp", trif[:], ctri_d, Btrif)
    cload("sp", onesf[:], cone_d, Bonesf)
    cload("pool", band[:].rearrange("p a g l -> p (a g l)"), cband_d, Bband)
    cbarrier()

    xt = [sbt([128, 4, D], F32, "xt") for _ in range(1)]
    xds = [kb.dsem("xds%d" % i) for i in range(1)]
    ods = kb.dsem("ods")
    htok, Bhtok = sbt([128, D], BF16, "htok")
    hT, BhT = sbt([128, 8, T], BF16, "hT")
    junk, Bjunk = sbt([128, D], F32, "junk")
    st4, Bst4 = sbt([128, 16], F32, "st4")
    NSL = 2
    SLW = 8 * 520
    wsl = [sbt([128, SLW], BF16, "wsl") for _ in range(NSL)]
    wds = [kb.dsem("wds%d" % i) for i in range(NSL)]
    wsi = [0]

    def load_w(views):
        i = wsi[0] % NSL
        wsi[0] += 1
        slot, Bs = wsl[i]
        for (off, kk, cc, src) in views:
            dst = slot[:, off:off + kk * cc].rearrange("p (k c) -> p k c", k=kk)
            kb.dma("pool", wds[i], lambda q, dst=dst, src=src: q.dma_start(out=dst, in_=src), writes=[Bs])
        return slot, Bs

    def win_view(l, c0, n):
        return win_d[l, :, c0:c0 + n].rearrange("(k p) c -> p k c", p=128)

    def load_win(l, c0, n=512):
        slot, Bs = load_w([(0, 8, n, win_view(l, c0, n))])
        return slot[:, 0:8 * n].rearrange("p (k c) -> p k c", k=8), Bs

    def proj_fm(wv, Bw, jb):
        ps, Bp = nextps()
        for kc in range(8):
            E("pe", lambda e, kc=kc: e.matmul(ps[:], wv[:, kc, jb * 128:(jb + 1) * 128], hT[:, kc, :], start=(kc == 0), stop=(kc == 7)),
              [Bw, BhT], [Bp])
        return ps, Bp

    def proj_tm(wv, Bw, tb, c0, n):
        ps, Bp = nextps()
        for kc in range(8):
            E("pe", lambda e, kc=kc: e.matmul(ps[:, 0:n], hT[:, kc, tb * 128:(tb + 1) * 128], wv[:, kc, c0:c0 + n], start=(kc == 0), stop=(kc == 7)),
              [Bw, BhT], [Bp])
        return ps, Bp

    yb = [sbt([128, 4, T], BF16, "yb") for _ in range(4)]
    SC = [sbt([128, 4, T + 4], F32, "scr") for _ in range(4)]

    def scflat(i):
        return SC[i][0][:].rearrange("p a b -> p (a b)")

    def sc_half(i, h):
        return scflat(i)[:, h * 1024:(h + 1) * 1024].rearrange("p (g c) -> p g c", g=16)
    qkT, BqkT = sbt([128, 8, T], BF16, "qkT")
    mrgT, BmrgT = qkT, BqkT
    gsb = [sbt([128, T], F32, "gsb") for _ in range(1)]
    tmpm = [sbt([128, T], F32, "tmpm") for _ in range(1)]

    npw, Bnpw = sbt([128, D], F32, "npw"); ppw, Bppw = sbt([128, D], F32, "ppw")
    scw, Bscw = sbt([128, 3, 4], F32, "scw"); mcw, Bmcw = sbt([128, 4, 8], F32, "mcw")
    psc, Bpsc = sbt([128, 4], F32, "psc"); s5dd, Bs5dd = sbt([128, 4], F32, "s5dd")
    mnw, Bmnw = sbt([128, W], F32, "mnw"); mbi, Bmbi = sbt([128, 4], F32, "mbi"); mbf, Bmbf = sbt([128, 4], F32, "mbf")
    poolw, Bpoolw = sbt([128, 4, 128], BF16, "poolw")
    schalo, Bschalo = sbt([128, 4, 2], F32, "schalo")
    putok, Bputok = sbt([128, 5, W], BF16, "putok")
    mlhalo, Bmlhalo = sbt([128, 8, 3], F32, "mlhalo")
    C32, BC32 = sbt([128, 4, 129], F32, "C32"); Cbf, BCbf = sbt([128, 4, 129], BF16, "Cbf")
    car = [sbt([128, 16], F32, "car") for _ in range(2)]
    Min, BMin = sbt([128, 4, 8, 2, 128], BF16, "Min")
    Mout, BMout = sbt([128, 16, 8, 2, 32], BF16, "Mout")
    Ktoe, BKtoe = sbt([128, 4, 8, 128], BF16, "Ktoe")
    Etab = [sbt([128, 16, 64], F32, "Etab") for _ in range(2)]
    rho, Brho = sbt([128, 16], F32, "rho")

    class _V:
        def __init__(self, ap):
            self.ap = ap

        def __getitem__(self, k):
            return self.ap[k]
    Zpad = [(_V(scflat(i)[:, 0:2048].rearrange("p (g c) -> p g c", g=16)), SC[i][1]) for i in range(2)]
    Cpad = [(_V(scflat(i)[:, 0:2048].rearrange("p (g c) -> p g c", g=16)), SC[i][1]) for i in range(2, 4)]
    E("dve", lambda e: e.memset(Mout[:], 0.0), w=[BMout])
    qT, BqT = _V(qkT[:, 0:4, :]), BqkT; kT, BkT = _V(qkT[:, 4:8, :]), BqkT
    vtok, Bvtok = sbt([128, 4, 4, 129], BF16, "vtok")
    E("dve", lambda e: e.memset(vtok[:], 1.0), w=[Bvtok])
    ogt, Bogt = sbt([128, 4, W], BF16, "ogt"); zwt, Bzwt = sbt([128, 4, W], BF16, "zwt")
    gt, Bgt = sbt([128, 4, 40], F32, "gt")
    ktil, Bktil = sbt([128, 128], BF16, "ktil"); PT, BPT = sbt([128, 128], BF16, "PT")
    hh, Bhh = sbt([128, 4, 128], F32, "hh"); ydtok, Bydtok = sbt([128, W], BF16, "ydtok")
    sm, Bsm = sbt([128, 16], F32, "sm")
    uT, BuT = sbt([128, 4, T], BF16, "uT"); u32, Bu32 = uT, BuT
    s5t = [(_V(sc_half(0, h)), SC[0][1]) for h in range(2)]
    Hs = [(_V(sc_half(1, h)), SC[1][1]) for h in range(2)]
    Xr = [(_V(sc_half(2, h)), SC[2][1]) for h in range(2)]
    Hu = Xr
    HpT, BHpT = sbt([128, 2, 16, 64], BF16, "HpT")
    Hp = [(_V(HpT[:, c]), BHpT) for c in range(2)]
    pooledT, BpooledT = _V(HpT[:].rearrange("p a g c -> p (a g c)").rearrange("p (g t) -> p g t", g=4)), BHpT

    def mul(e, o, a, b, r, w):
        return E(e, lambda q: q.tensor_tensor(out=o, in0=a, in1=b, op=ALU.mult), r, w)

    def tt(e, o, a, b, op, r, w):
        return E(e, lambda q: q.tensor_tensor(out=o, in0=a, in1=b, op=op), r, w)

    prep_cache = {}

    def layer_prep(l):
        pidx = [0]

        def sbt(shape, dt, name=None):
            pidx[0] += 1
            k = (name, pidx[0])
            if k not in prep_cache:
                prep_cache[k] = sbt_global(shape, dt, name)
            return prep_cache[k]

        def bc(v):
            return v.partition_broadcast(128)
        cload("sp", npw[:], bc(npw_d[l]), Bnpw); cload("sp", ppw[:], bc(ppw_d[l]), Bppw)
        cload("sp", scw[:], scw_d[l].rearrange("k (cb p) -> p k cb", p=128), Bscw, slow=True)
        cload("sp", mcw[:], mcw_d[l].rearrange("k (cb p) -> p k cb", p=128), Bmcw, slow=True)
        cload("sp", psc[:], psc_d[l].rearrange("(cb p) -> p cb", p=128), Bpsc, slow=True)
        cload("sp", s5dd[:], s5d_d[l].rearrange("(cb p) -> p cb", p=128), Bs5dd, slow=True)
        cload("sp", mnw[:], bc(mnw_d[l]), Bmnw); cload("sp", mbi[:], bc(mbi_d[l]), Bmbi); cload("sp", mbf[:], bc(mbf_d[l]), Bmbf)
        cload("pool", poolw[:], pw_d[l].rearrange("g c d -> c g d"), Bpoolw)
        if not en[0]:
            cbarrier()
            return
        for tt_, bb_ in Zpad + Cpad:
            E("dve", lambda e, tt_=tt_: e.memset(tt_[:], 0.0), w=[bb_])
        def arena(i):
            return yb[i][0][:].rearrange("p a b -> p (a b)").bitcast(F32)

        def carve(i, off, shp):
            n = shp[0] * shp[1]
            return (_V(arena(i)[:, off:off + n].rearrange("p (a b) -> p a b", a=shp[0])), yb[i][1])
        A = [sbt([128, 16], F32, "A") for _ in range(2)]
        ldt, Bldt = sbt([128, 16], F32, "ldt")
        Bm = [carve(0, c * 256, (16, 16)) for c in range(2)]
        Cm = [carve(0, 512 + c * 256, (16, 16)) for c in range(2)]
        cload("sp", A[0][0][:], are_d[l].rearrange("(gp gl) n -> (gl n) gp", gl=2), A[0][1], slow=True)
        cload("sp", A[1][0][:], aim_d[l].rearrange("(gp gl) n -> (gl n) gp", gl=2), A[1][1], slow=True)
        for gl in range(2):
            cload("sp", ldt[gl * 64:(gl + 1) * 64, :], ldt_d[l, gl::2].partition_broadcast(64), Bldt, slow=True)
        for c, src in enumerate((bre_d, bim_d)):
            cload("sp", Bm[c][0][:], src[l].rearrange("(gp gl) n q -> (gl n) gp q", gl=2), Bm[c][1], slow=True)
        for c, src in enumerate((cre_d, cim_d)):
            for gl in range(2):
                for gp in range(16):
                    cload("sp", Cm[c][0][gl * 64:(gl + 1) * 64, gp, :], src[l, 2 * gp + gl].rearrange("p n -> n p"), Cm[c][1], slow=True)
        cbarrier()
        tn = [sbt([128, 16], F32, "tn") for _ in range(12)]

        def V(i):
            return tn[i][0][:]

        def Bv(i):
            return tn[i][1]
        Are, Aim = A[0][0][:], A[1][0][:]
        BA = [A[0][1], A[1][1]]
        E("act", lambda e: e.activation(out=ldt[:], in_=ldt[:], func=AF.Exp), [Bldt], [Bldt])
        mul("dve", V(0), Are, ldt[:], [BA[0], Bldt], [Bv(0)])
        mul("dve", V(1), Aim, ldt[:], [BA[1], Bldt], [Bv(1)])
        E("act", lambda e: e.activation(out=V(2), in_=V(0), func=AF.Exp), [Bv(0)], [Bv(2)])
        E("act", lambda e: e.activation(out=V(11), in_=V(0), func=AF.Exp, scale=8.0), [Bv(0)], [Bv(11)])
        ki, Bki = sbt([128, 16], I32, "ki")
        E("dve", lambda e: e.tensor_scalar(out=V(3), in0=V(1), scalar1=1.0 / (2 * math.pi), scalar2=None, op0=ALU.mult), [Bv(1)], [Bv(3)])
        E("dve", lambda e: e.tensor_copy(out=ki[:], in_=V(3)), [Bv(3)], [Bki])
        E("dve", lambda e: e.tensor_copy(out=V(3), in_=ki[:]), [Bki], [Bv(3)])
        E("dve", lambda e: e.scalar_tensor_tensor(out=V(4), in0=V(3), scalar=-2 * math.pi, in1=V(1), op0=ALU.mult, op1=ALU.add), [Bv(3), Bv(1)], [Bv(4)])
        E("act", lambda e: e.activation(out=V(5), in_=V(4), func=AF.Sin, scale=0.5), [Bv(4)], [Bv(5)])
        E("act", lambda e: e.activation(out=V(6), in_=V(4), func=AF.Sin, scale=0.25), [Bv(4)], [Bv(6)])
        mul("dve", V(7), V(6), V(6), [Bv(6)], [Bv(7)])
        E("dve", lambda e: e.tensor_scalar(out=V(7), in0=V(7), scalar1=-2.0, scalar2=1.0, op0=ALU.mult, op1=ALU.add), [Bv(7)], [Bv(7)])
        mul("dve", V(8), V(5), V(7), [Bv(5), Bv(7)], [Bv(8)])
        mul("dve", V(5), V(5), V(5), [Bv(5)], [Bv(5)])
        E("dve", lambda e: e.tensor_scalar(out=V(5), in0=V(5), scalar1=-2.0, scalar2=1.0, op0=ALU.mult, op1=ALU.add), [Bv(5)], [Bv(5)])
        Pw = [sbt([128, 9, 16], F32, "Pw") for _ in range(2)]
        Pr, Pi = Pw[0][0], Pw[1][0]
        BP = [Pw[0][1], Pw[1][1]]
        E("dve", lambda e: e.memset(Pr[:, 0, :], 1.0), w=[BP[0]])
        E("dve", lambda e: e.memset(Pi[:, 0, :], 0.0), w=[BP[1]])
        mul("dve", Pr[:, 1, :], V(2), V(5), [Bv(2), Bv(5)], [BP[0]])
        E("dve", lambda e: e.scalar_tensor_tensor(out=Pi[:, 1, :], in0=V(8), scalar=2.0, in1=V(2), op0=ALU.mult, op1=ALU.mult), [Bv(8), Bv(2)], [BP[1]])

        def cmul(o_r, o_i, xr, xi, yr, yi, r, w, t1, t2, Bt):
            mul("dve", t1, xr, yr, r, Bt); mul("dve", t2, xi, yi, r, Bt)
            tt("dve", o_r, t1, t2, ALU.subtract, Bt, [w[0]])
            mul("dve", t1, xr, yi, r, Bt); mul("dve", t2, xi, yr, r, Bt)
            tt("dve", o_i, t1, t2, ALU.add, Bt, [w[1]])
        for j in range(1, 8):
            cmul(Pr[:, j + 1, :], Pi[:, j + 1, :], Pr[:, j, :], Pi[:, j, :], Pr[:, 1, :], Pi[:, 1, :], BP, BP, V(9), V(10), [Bv(9), Bv(10)])
        E("dve", lambda e: e.reciprocal(out=V(0), in_=V(11)), [Bv(11)], [Bv(0)])
        wl = [sbt([128, 16], F32, "wl") for _ in range(4)]
        mul("dve", wl[0][0][:], Pr[:, 8, :], V(0), [BP[0], Bv(0)], [wl[0][1]])
        E("dve", lambda e: e.scalar_tensor_tensor(out=wl[1][0][:], in0=Pi[:, 8, :], scalar=-1.0, in1=V(0), op0=ALU.mult, op1=ALU.mult), [BP[1], Bv(0)], [wl[1][1]])
        Er, Ei = Etab[0][0], Etab[1][0]
        BE = [Etab[0][1], Etab[1][1]]
        E("dve", lambda e: e.tensor_copy(out=Er[:, :, 0], in_=wl[0][0][:]), [wl[0][1]], [BE[0]])
        E("dve", lambda e: e.tensor_copy(out=Ei[:, :, 0], in_=wl[1][0][:]), [wl[1][1]], [BE[1]])
        t3a, Bt3a = carve(2, 0, (16, 32)); t3b, Bt3b = carve(2, 512, (16, 32))
        cur = 0
        Lw = 1
        while Lw < 64:
            wr = wl[cur][0][:].unsqueeze(2).to_broadcast([128, 16, Lw]); wi = wl[cur + 1][0][:].unsqueeze(2).to_broadcast([128, 16, Lw])
            Bw = [wl[cur][1], wl[cur + 1][1]]
            cmul(Er[:, :, Lw:2 * Lw], Ei[:, :, Lw:2 * Lw], Er[:, :, 0:Lw], Ei[:, :, 0:Lw], wr, wi, BE + Bw, BE, t3a[:, :, 0:Lw], t3b[:, :, 0:Lw], [Bt3a, Bt3b])
            nxt = 2 - cur
            cmul(wl[nxt][0][:], wl[nxt + 1][0][:], wl[cur][0][:], wl[cur + 1][0][:], wl[cur][0][:], wl[cur + 1][0][:], Bw, [wl[nxt][1], wl[nxt + 1][1]], V(9), V(10), [Bv(9), Bv(10)])
            cur = nxt
            Lw *= 2
        E("dve", lambda e: e.tensor_copy(out=rho[:], in_=V(11)), [Bv(11)], [Brho])
        E("dve", lambda e: e.tensor_scalar(out=V(0), in0=Pr[:, 1, :], scalar1=-1.0, scalar2=None, op0=ALU.add), [BP[0]], [Bv(0)])
        mul("dve", V(1), Are, Are, [BA[0]], [Bv(1)]); mul("dve", V(2), Aim, Aim, [BA[1]], [Bv(2)])
        tt("dve", V(1), V(1), V(2), ALU.add, [Bv(1), Bv(2)], [Bv(1)])
        E("dve", lambda e: e.reciprocal(out=V(1), in_=V(1)), [Bv(1)], [Bv(1)])
        mul("dve", V(2), V(0), Are, [Bv(0), BA[0]], [Bv(2)]); mul("dve", V(3), Pi[:, 1, :], Aim, [BP[1], BA[1]], [Bv(3)])
        tt("dve", V(2), V(2), V(3), ALU.add, [Bv(2), Bv(3)], [Bv(2)]); mul("dve", V(4), V(2), V(1), [Bv(2), Bv(1)], [Bv(4)])
        mul("dve", V(2), Pi[:, 1, :], Are, [BP[1], BA[0]], [Bv(2)]); mul("dve", V(3), V(0), Aim, [Bv(0), BA[1]], [Bv(3)])
        tt("dve", V(2), V(2), V(3), ALU.subtract, [Bv(2), Bv(3)], [Bv(2)]); mul("dve", V(5), V(2), V(1), [Bv(2), Bv(1)], [Bv(5)])
        Bb = [carve(1, c * 256, (16, 16)) for c in range(2)]
        Zz = [carve(1, 512 + c * 256, (16, 16)) for c in range(2)]
        t4a, Bt4a = carve(3, 0, (16, 16)); t4b, Bt4b = carve(3, 256, (16, 16))

        def b16(v):
            return v.unsqueeze(2).to_broadcast([128, 16, 16])
        cmul(Bb[0][0][:], Bb[1][0][:], b16(V(4)), b16(V(5)), Bm[0][0][:], Bm[1][0][:], [Bv(4), Bv(5), Bm[0][1], Bm[1][1]], [Bb[0][1], Bb[1][1]], t4a[:], t4b[:], [Bt4a, Bt4b])
        E("dve", lambda e: e.tensor_scalar(out=t4a[:], in0=Cm[1][0][:], scalar1=-1.0, scalar2=None, op0=ALU.mult), [Cm[1][1]], [Bt4a])
        csrc = [(Cm[0][0], Cm[0][1]), (t4a, Bt4a)]

        def padfill(dst, Bd, src, Bs):
            d5 = dst[:].rearrange("p (gq j) (jj c) -> p gq j jj c", j=4, jj=4)
            s4 = src[:].rearrange("p (gq j) q -> p gq j q", j=4)
            for j in range(4):
                for gl in range(2):
                    E("act", lambda e, j=j, gl=gl: e.copy(out=d5[gl * 64:(gl + 1) * 64, :, j, j, gl * 16:(gl + 1) * 16], in_=s4[gl * 64:(gl + 1) * 64, :, j, :]), [Bs], [Bd])
        for c in range(2):
            padfill(Cpad[c][0], Cpad[c][1], csrc[c][0], csrc[c][1])
        for jp in range(8):
            cmul(Zz[0][0][:], Zz[1][0][:], b16(Pr[:, jp, :]), b16(Pi[:, jp, :]), Bb[0][0][:], Bb[1][0][:], BP + [Bb[0][1], Bb[1][1]], [Zz[0][1], Zz[1][1]], t4a[:], t4b[:], [Bt4a, Bt4b])
            for c in range(2):
                padfill(Zpad[c][0], Zpad[c][1], Zz[c][0], Zz[c][1])
            s = 7 - jp
            for gq in range(4):
                for c in range(2):
                    ps, Bp = nextps()
                    for j in range(4):
                        E("pe", lambda e, j=j, c=c, gq=gq: e.matmul(ps[:, 0:128], Zpad[c][0][:, 4 * gq + j, :], identf[:], start=(j == 0), stop=(j == 3)), [Zpad[c][1], Bidf], [Bp])
                    E("dve", lambda e, c=c, gq=gq, ps=ps: e.tensor_copy(out=Min[:, gq, s, c, :], in_=ps[:, 0:128]), [Bp], [BMin])
                ps, Bp = nextps()
                for j in range(4):
                    for c in range(2):
                        E("pe", lambda e, j=j, c=c, gq=gq: e.matmul(ps[:, 0:128], Zpad[c][0][:, 4 * gq + j, :], Cpad[c][0][:, 4 * gq + j, :], start=(j == 0 and c == 0), stop=(j == 3 and c == 1)),
                          [Zpad[c][1], Cpad[c][1]], [Bp])
                E("dve", lambda e, gq=gq, ps=ps: e.tensor_copy(out=Ktoe[:, gq, jp, :], in_=ps[:, 0:128]), [Bp], [BKtoe])
        for t in range(8):
            cmul(Zz[0][0][:], Zz[1][0][:], b16(Pr[:, t + 1, :]), b16(Pi[:, t + 1, :]), Cm[0][0][:], Cm[1][0][:], BP + [Cm[0][1], Cm[1][1]], [Zz[0][1], Zz[1][1]], t4a[:], t4b[:], [Bt4a, Bt4b])
            for gl in range(2):
                E("dve", lambda e, gl=gl, t=t: e.tensor_copy(out=Mout[gl * 64:(gl + 1) * 64, :, t, 0, gl * 16:(gl + 1) * 16], in_=Zz[0][0][gl * 64:(gl + 1) * 64]), [Zz[0][1]], [BMout])
                E("dve", lambda e, gl=gl, t=t: e.tensor_scalar(out=Mout[gl * 64:(gl + 1) * 64, :, t, 1, gl * 16:(gl + 1) * 16], in0=Zz[1][0][gl * 64:(gl + 1) * 64], scalar1=-1.0, scalar2=None, op0=ALU.mult), [Zz[1][1]], [BMout])

    def seq_reset():
        E("dve", lambda e: e.memset(schalo[:], 0.0), w=[Bschalo])
        E("dve", lambda e: e.memset(putok[:, 0, :], 0.0), w=[Bputok])
        E("dve", lambda e: e.memset(mlhalo[:], 0.0), w=[Bmlhalo])
        E("dve", lambda e: e.memset(C32[:], 0.0), w=[BC32])
        E("dve", lambda e: e.memset(Cbf[:], 0.0), w=[BCbf])
        for c in range(2):
            E("dve", lambda e, c=c: e.memset(car[c][0][:], 0.0), w=[car[c][1]])

    def branch_sconv(l):
        xs, Bxs = SC[0]; prod, Bprod = SC[1]; acc, Bacc = SC[2]; zs, Bzs = SC[3]
        wv, Bw = load_win(l, C_SX)
        for cb in range(4):
            ps, Bp = proj_fm(wv, Bw, cb)
            E("act", lambda e, cb=cb, ps=ps: e.copy(out=xs[:, cb, 0:T], in_=ps[:]), [Bp], [Bxs])
        E("dve", lambda e: e.tensor_copy(out=prod[:, :, 0:2], in_=schalo[:]), [Bschalo], [Bprod])
        wv, Bw = load_win(l, C_SC)
        for cb in range(4):
            ps, Bp = proj_fm(wv, Bw, cb)
            mul("dve", prod[:, cb, 2:T + 2], ps[:], xs[:, cb, 0:T], [Bp, Bxs], [Bprod])
        E("dve", lambda e: e.tensor_copy(out=schalo[:], in_=prod[:, :, T:T + 2]), [Bprod], [Bschalo])
        for cb in range(4):
            E("dve", lambda e, cb=cb: e.tensor_scalar(out=acc[:, cb, 0:T], in0=prod[:, cb, 2:T + 2], scalar1=scw[:, 2, cb:cb + 1], scalar2=None, op0=ALU.mult), [Bprod, Bscw], [Bacc])
            for k in (1, 0):
                E("dve", lambda e, cb=cb, k=k: e.scalar_tensor_tensor(out=acc[:, cb, 0:T], in0=prod[:, cb, k:k + T], scalar=scw[:, k, cb:cb + 1], in1=acc[:, cb, 0:T], op0=ALU.mult, op1=ALU.add),
                  [Bprod, Bscw, Bacc], [Bacc])
        wv, Bw = load_win(l, C_SB)
        for cb in range(4):
            ps, Bp = proj_fm(wv, Bw, cb)
            mul("dve", acc[:, cb, 0:T], ps[:], acc[:, cb, 0:T], [Bp, Bacc], [Bacc])
        wv, Bw = load_win(l, C_SZ)
        for cb in range(4):
            ps, Bp = proj_fm(wv, Bw, cb)
            E("act", lambda e, cb=cb, ps=ps: e.activation(out=zs[:, cb, 0:T], in_=ps[:], func=AF.Silu), [Bp], [Bzs])
            mul("dve", yb[2][0][:, cb, :], acc[:, cb, 0:T], zs[:, cb, 0:T], [Bacc, Bzs], [yb[2][1]])

    def branch_pool(l, first):
        import os
        stage = int(os.environ.get("POOLDBG", "9"))
        zs, Bzs = SC[3]
        wv, Bw = load_win(l, C_PU)
        for tb in range(4):
            if stage == -2:
                continue
            ps, Bp = proj_tm(wv, Bw, tb, 0, 512)
            if stage == -1:
                continue
            E("dve", lambda e, tb=tb, ps=ps: e.tensor_copy(out=putok[:, tb + 1, :], in_=ps[:]), [Bp], [Bputok])
        if stage < 1:
            E("dve", lambda e: e.memset(yb[1][0][:], 0.0), w=[yb[1][1]])
            return
        for g in range(4):
            ps, Bp = nextps()
            for tb in range(4):
                f0 = first and tb == 0
                E("pe", lambda e, g=g, tb=tb, ps=ps, f0=f0: e.matmul(ps[:, tb * 128:(tb + 1) * 128], putok[:, tb + 1, g * 128:(g + 1) * 128], band[:, 2 if f0 else 0, g, :], start=True, stop=f0),
                  [Bputok, Bband], [Bp])
                if not f0:
                    E("pe", lambda e, g=g, tb=tb, ps=ps: e.matmul(ps[:, tb * 128:(tb + 1) * 128], putok[:, tb, g * 128:(g + 1) * 128], band[:, 1, g, :], start=False, stop=True),
                      [Bputok, Bband], [Bp])
            E("dve", lambda e, g=g, ps=ps: e.tensor_copy(out=pooledT[:, g, :], in_=ps[:]), [Bp], [BpooledT])
        E("dve", lambda e: e.tensor_copy(out=putok[:, 0, :], in_=putok[:, 4, :]), [Bputok], [Bputok])
        if stage < 2:
            E("dve", lambda e: e.memset(yb[1][0][:], 0.0), w=[yb[1][1]])
            return
        wv, Bw = load_win(l, C_PZ)
        for g in range(4):
            ps, Bp = proj_fm(wv, Bw, g)
            E("act", lambda e, g=g, ps=ps: e.activation(out=zs[:, g, 0:T], in_=ps[:], func=AF.Silu), [Bp], [Bzs])
            ps2, Bp2 = nextps()
            E("pe", lambda e, g=g, ps2=ps2: e.matmul(ps2[:], poolw[:, g, :], pooledT[:, g, :], start=True, stop=True), [Bpoolw, BpooledT], [Bp2])
            E("dve", lambda e, g=g, ps2=ps2: e.scalar_tensor_tensor(out=yb[1][0][:, g, :], in0=ps2[:], scalar=psc[:, g:g + 1], in1=zs[:, g, 0:T], op0=ALU.mult, op1=ALU.mult),
              [Bp2, Bpsc, Bzs], [yb[1][1]])

    def branch_mlstm(l):
        cq, Bcq = SC[0]; ca, Bca = SC[1]
        for qi, (c0, dst, Bd) in enumerate(((C_Q, qT, BqT), (C_K, kT, BkT))):
            wv, Bw = load_win(l, c0)
            E("dve", lambda e, qi=qi: e.tensor_copy(out=cq[:, :, 0:3], in_=mlhalo[:, qi * 4:(qi + 1) * 4, :]), [Bmlhalo], [Bcq])
            for cb in range(4):
                ps, Bp = proj_fm(wv, Bw, cb)
                E("act", lambda e, cb=cb, ps=ps: e.copy(out=cq[:, cb, 3:T + 3], in_=ps[:]), [Bp], [Bcq])
            E("dve", lambda e, qi=qi: e.tensor_copy(out=mlhalo[:, qi * 4:(qi + 1) * 4, :], in_=cq[:, :, T:T + 3]), [Bcq], [Bmlhalo])
            for cb in range(4):
                ch = qi * 4 + cb
                E("dve", lambda e, cb=cb, ch=ch: e.tensor_scalar(out=ca[:, cb, 0:T], in0=cq[:, cb, 3:T + 3], scalar1=mcw[:, 3, ch:ch + 1], scalar2=None, op0=ALU.mult), [Bcq, Bmcw], [Bca])
                for k in (2, 1, 0):
                    E("dve", lambda e, cb=cb, ch=ch, k=k: e.scalar_tensor_tensor(out=ca[:, cb, 0:T], in0=cq[:, cb, k:k + T], scalar=mcw[:, k, ch:ch + 1], in1=ca[:, cb, 0:T], op0=ALU.mult, op1=ALU.add),
                      [Bcq, Bmcw, Bca], [Bca])
                E("act", lambda e, cb=cb, dst=dst: e.activation(out=dst[:, cb, :], in_=ca[:, cb, 0:T], func=AF.Silu), [Bca], [Bd])
        wv, Bw = load_win(l, C_V)
        for tb in range(4):
            ps, Bp = proj_tm(wv, Bw, tb, 0, 512)
            E("dve", lambda e, tb=tb, ps=ps: e.tensor_copy(out=vtok[:, tb, :, 0:128], in_=ps[:].rearrange("p (h d) -> p h d", h=4)), [Bp], [Bvtok])
        wv, Bw = load_win(l, C_O)
        for tb in range(4):
            ps, Bp = proj_tm(wv, Bw, tb, 0, 512)
            E("act", lambda e, tb=tb, ps=ps: e.activation(out=ogt[:, tb, :], in_=ps[:], func=AF.Sigmoid), [Bp], [Bogt])
        wv, Bw = load_win(l, C_IF, 520)
        for tb in range(4):
            ps, Bp = proj_tm(wv, Bw, tb, 8, 512)
            E("act", lambda e, tb=tb, ps=ps: e.activation(out=zwt[:, tb, :], in_=ps[:], func=AF.Silu), [Bp], [Bzwt])
            mul("dve", zwt[:, tb, :], zwt[:, tb, :], mnw[:], [Bzwt, Bmnw], [Bzwt])
            psg, Bpg = proj_tm(wv, Bw, tb, 0, 8)
            G = gt[:, tb, :]
            tt("dve", G[:, 0:4], psg[:, 0:4], mbi[:], ALU.add, [Bpg, Bmbi], [Bgt])
            tt("dve", G[:, 4:8], psg[:, 4:8], mbf[:], ALU.add, [Bpg, Bmbf], [Bgt])
            E("act", lambda e, G=G: e.activation(out=G[:, 4:8], in_=G[:, 4:8], func=AF.Exp, scale=-1.0), [Bgt], [Bgt])
            E("act", lambda e, G=G: e.activation(out=G[:, 4:8], in_=G[:, 4:8], func=AF.Ln, bias=1.0), [Bgt], [Bgt])
            ps2, Bp2 = nextps()
            E("pe", lambda e, G=G, ps2=ps2: e.matmul(ps2[:, 0:4], trif[:], G[:, 4:8], start=True, stop=True), [Btrif, Bgt], [Bp2])
            E("pe", lambda e, G=G, ps2=ps2: e.matmul(ps2[:, 4:8], onesf[:], G[:, 4:8], start=True, stop=True), [Bonesf, Bgt], [Bp2])
            E("act", lambda e, G=G, ps2=ps2: e.activation(out=G[:, 8:12], in_=ps2[:, 0:4], func=AF.Exp, scale=-1.0, bias=math.log(128 ** -0.5)), [Bp2], [Bgt])
            tt("dve", G[:, 24:28], G[:, 0:4], ps2[:, 0:4], ALU.add, [Bgt, Bp2], [Bgt])
            E("act", lambda e, G=G: e.activation(out=G[:, 12:16], in_=G[:, 24:28], func=AF.Exp), [Bgt], [Bgt])
            tt("dve", G[:, 24:28], G[:, 24:28], ps2[:, 4:8], ALU.subtract, [Bgt, Bp2], [Bgt])
            E("act", lambda e, G=G: e.activation(out=G[:, 16:20], in_=G[:, 24:28], func=AF.Exp), [Bgt], [Bgt])
            E("act", lambda e, G=G, ps2=ps2: e.activation(out=G[:, 20:24], in_=ps2[:, 4:8], func=AF.Exp, scale=-1.0), [Bp2], [Bgt])
        pbt = None
        for tb in range(4):
            G = gt[:, tb, :]
            sl = slice(tb * 128, (tb + 1) * 128)
            for hd in range(4):
                pk, Bpk = nextps()
                pkb = pk[:].bitcast(BF16)
                E("pe", lambda e, hd=hd, pkb=pkb: e.transpose(pkb[:, 0:128], kT[:, hd, sl], identb[:]), [BkT, Bidb], [Bpk])
                E("dve", lambda e, hd=hd, pkb=pkb, G=G: e.tensor_scalar(out=ktil[:], in0=pkb[:, 0:128], scalar1=G[:, 16 + hd:17 + hd], scalar2=None, op0=ALU.mult), [Bpk, Bgt], [Bktil])
                pS, BpS = nextps()
                E("pe", lambda e, hd=hd, pS=pS: e.matmul(pS[:, 0:128], kT[:, hd, sl], qT[:, hd, sl], start=True, stop=True), [BkT, BqT], [BpS])
                E("dve", lambda e, hd=hd, pS=pS, G=G: e.scalar_tensor_tensor(out=PT[:], in0=pS[:, 0:128], scalar=G[:, 12 + hd:13 + hd], in1=trif[:], op0=ALU.mult, op1=ALU.mult), [BpS, Bgt, Btrif], [BPT])
                pN, BpN = nextps()
                E("pe", lambda e, hd=hd, pN=pN: e.matmul(pN[:, 0:129], PT[:], vtok[:, tb, hd, :], start=True, stop=False), [BPT, Bvtok], [BpN])
                E("pe", lambda e, hd=hd, pN=pN: e.matmul(pN[:, 0:129], qT[:, hd, sl], Cbf[:, hd, :], start=False, stop=True), [BqT, BCbf], [BpN])
                pC, BpC = nextps()
                E("pe", lambda e, hd=hd, pC=pC: e.matmul(pC[:, 0:129], ktil[:], vtok[:, tb, hd, :], start=True, stop=True), [Bktil, Bvtok], [BpC])
                E("dve", lambda e, hd=hd, pC=pC, G=G: e.scalar_tensor_tensor(out=C32[:, hd, :], in0=C32[:, hd, :], scalar=G[:, 20 + hd:21 + hd], in1=pC[:, 0:129], op0=ALU.mult, op1=ALU.add), [BC32, Bgt, BpC], [BC32])
                E("dve", lambda e, hd=hd: e.tensor_copy(out=Cbf[:, hd, :], in_=C32[:, hd, :]), [BC32], [BCbf])
                E("act", lambda e, hd=hd, pN=pN, G=G: e.activation(out=sm[:, 0:1], in_=pN[:, 128:129], func=AF.Abs, scale=G[:, 8 + hd:9 + hd]), [BpN, Bgt], [Bsm])
                E("dve", lambda e: e.tensor_scalar(out=sm[:, 0:1], in0=sm[:, 0:1], scalar1=1.0, scalar2=None, op0=ALU.max), [Bsm], [Bsm])
                E("dve", lambda e: e.reciprocal(out=sm[:, 1:2], in_=sm[:, 0:1]), [Bsm], [Bsm])
                mul("dve", sm[:, 2:3], sm[:, 1:2], G[:, 8 + hd:9 + hd], [Bsm, Bgt], [Bsm])
                E("dve", lambda e, hd=hd, pN=pN: e.scalar_tensor_tensor(out=hh[:, hd, :], in0=pN[:, 0:128], scalar=sm[:, 2:3], in1=ogt[:, tb, hd * 128:(hd + 1) * 128], op0=ALU.mult, op1=ALU.mult), [BpN, Bsm, Bogt], [Bhh])
                E("act", lambda e, hd=hd: e.activation(out=junk[:, 0:128], in_=hh[:, hd, :], func=AF.Square, accum_out=sm[:, 4 + hd:5 + hd]), [Bhh], [Bjunk, Bsm])
            E("dve", lambda e: e.tensor_scalar(out=sm[:, 8:12], in0=sm[:, 4:8], scalar1=1.0 / 128, scalar2=EPS, op0=ALU.mult, op1=ALU.add), [Bsm], [Bsm])
            E("act", lambda e: e.activation(out=sm[:, 8:12], in_=sm[:, 8:12], func=AF.Sqrt), [Bsm], [Bsm])
            E("dve", lambda e: e.reciprocal(out=sm[:, 12:16], in_=sm[:, 8:12]), [Bsm], [Bsm])
            for hd in range(4):
                E("dve", lambda e, hd=hd: e.scalar_tensor_tensor(out=ydtok[:, hd * 128:(hd + 1) * 128], in0=hh[:, hd, :], scalar=sm[:, 12 + hd:13 + hd], in1=zwt[:, tb, hd * 128:(hd + 1) * 128], op0=ALU.mult, op1=ALU.mult),
                  [Bhh, Bsm, Bzwt], [Bydtok])
            pt, Bpt = nextps()
            ptb = pt[:].bitcast(BF16)
            for cb in range(4):
                E("pe", lambda e, cb=cb, ptb=ptb: e.transpose(ptb[:, cb * 128:(cb + 1) * 128], ydtok[:, cb * 128:(cb + 1) * 128], identb[:]), [Bydtok, Bidb], [Bpt])
            E("act", lambda e, ptb=ptb: e.copy(out=yb[3][0][:, :, sl], in_=ptb[:, 0:512].rearrange("p (c t) -> p c t", c=4)), [Bpt], [yb[3][1]])

    def branch_s5(l):
        zs, Bzs = SC[3]; ya, Bya = SC[1]; yg, Byg = SC[2]
        wv, Bw = load_win(l, C_S5U)
        for cb in range(4):
            ps, Bp = proj_fm(wv, Bw, cb)
            E("dve", lambda e, cb=cb, ps=ps: e.tensor_copy(out=uT[:, cb, :], in_=ps[:]), [Bp], [BuT])
        uS = uT[:].rearrange("p g (c s) -> p g s c", s=8)
        Er, Ei = Etab[0][0], Etab[1][0]
        BE = [Etab[0][1], Etab[1][1]]
        Xps = {}
        for c in range(2):
            for hf in range(2):
                Xps[(c, hf)] = nextps()
        for gp in range(16):
            gq, j = divmod(gp, 4)
            for c in range(2):
                ps, Bp = Xps[(c, gp // 8)]
                pv = ps[:].rearrange("p (g c) -> p g c", g=8)
                for s in range(8):
                    E("pe", lambda e, pv=pv, gp=gp, gq=gq, j=j, c=c, s=s: e.matmul(pv[:, gp % 8, :], Min[32 * j:32 * j + 32, gq, s, c, :], uS[32 * j:32 * j + 32, gq, s, :],
                                                                        start=(s == 0), stop=(s == 7), tile_position=(32 * j, 0)), [BMin, BuT], [Bp])
        for hf in range(2):
            gs = slice(hf * 8, hf * 8 + 8)
            xr, Bxr = Xps[(0, hf)]; xi, Bxi = Xps[(1, hf)]
            xrv = xr[:].rearrange("p (g c) -> p g c", g=8); xiv = xi[:].rearrange("p (g c) -> p g c", g=8)
            t1, Bt1 = s5t[0]; t2, Bt2 = s5t[1]
            mul("dve", t1[:, gs, :], xrv, Er[:, gs, :], [Bxr, BE[0]], [Bt1]); mul("dve", t2[:, gs, :], xiv, Ei[:, gs, :], [Bxi, BE[1]], [Bt2])
            tt("dve", Xr[0][0][:, gs, :], t1[:, gs, :], t2[:, gs, :], ALU.subtract, [Bt1, Bt2], [Xr[0][1]])
            mul("dve", t1[:, gs, :], xiv, Er[:, gs, :], [Bxi, BE[0]], [Bt1]); mul("dve", t2[:, gs, :], xrv, Ei[:, gs, :], [Bxr, BE[1]], [Bt2])
            tt("dve", Xr[1][0][:, gs, :], t1[:, gs, :], t2[:, gs, :], ALU.add, [Bt1, Bt2], [Xr[1][1]])
        for c in range(2):
            for gp in range(16):
                E("dve", lambda e, c=c, gp=gp: e.tensor_tensor_scan(out=Hs[c][0][:, gp, :], data0=rho[:, gp:gp + 1].to_broadcast([128, 64]), data1=Xr[c][0][:, gp, :], initial=car[c][0][:, gp:gp + 1], op0=ALU.mult, op1=ALU.add),
                  [Brho, Xr[c][1], car[c][1]], [Hs[c][1]])
        t1, Bt1 = s5t[0]; t2, Bt2 = s5t[1]
        mul("dve", t1[:], Er[:], Hs[0][0][:], [BE[0], Hs[0][1]], [Bt1]); mul("dve", t2[:], Ei[:], Hs[1][0][:], [BE[1], Hs[1][1]], [Bt2])
        tt("dve", Hu[0][0][:], t1[:], t2[:], ALU.add, [Bt1, Bt2], [Hu[0][1]])
        mul("dve", t1[:], Er[:], Hs[1][0][:], [BE[0], Hs[1][1]], [Bt1]); mul("dve", t2[:], Ei[:], Hs[0][0][:], [BE[1], Hs[0][1]], [Bt2])
        tt("dve", Hu[1][0][:], t1[:], t2[:], ALU.subtract, [Bt1, Bt2], [Hu[1][1]])
        for c in range(2):
            E("dve", lambda e, c=c: e.tensor_copy(out=Hp[c][0][:, :, 0], in_=car[c][0][:]), [car[c][1]], [Hp[c][1]])
            E("dve", lambda e, c=c: e.tensor_copy(out=Hp[c][0][:, :, 1:64], in_=Hu[c][0][:, :, 0:63]), [Hu[c][1]], [Hp[c][1]])
            E("dve", lambda e, c=c: e.tensor_copy(out=car[c][0][:], in_=Hu[c][0][:, :, 63]), [Hu[c][1]], [car[c][1]])
        for gq in range(4):
            ps, Bp = nextps()
            pv = ps[:].rearrange("p (t c) -> p t c", t=8)
            for t in range(8):
                for s in range(t + 1):
                    E("pe", lambda e, pv=pv, gq=gq, t=t, s=s: e.matmul(pv[:, t, :], Ktoe[:, gq, t - s, :], uS[:, gq, s, :], start=(s == 0), stop=False), [BKtoe, BuT], [Bp])
                for j in range(4):
                    gp = 4 * gq + j
                    for c in range(2):
                        E("pe", lambda e, pv=pv, gp=gp, j=j, t=t, c=c: e.matmul(pv[32 * j:32 * j + 32, t, :], Mout[:, gp, t, c, :], Hp[c][0][:, gp, :], start=False, stop=(c == 1), tile_position=(0, 32 * j)),
                          [BMout, Hp[c][1]], [Bp])
            E("dve", lambda e, gq=gq, pv=pv: e.scalar_tensor_tensor(out=ya[:, gq, 0:T].rearrange("p (c t) -> p c t", t=8), in0=u32[:, gq, :].rearrange("p (c t) -> p c t", t=8), scalar=s5dd[:, gq:gq + 1],
                                                                    in1=pv.rearrange("p t c -> p c t"), op0=ALU.mult, op1=ALU.add), [Bu32, Bs5dd, Bp], [Bya])
        t1, Bt1 = SC[0]
        for cb in range(4):
            Y = ya[:, cb, 0:T]
            mul("dve", t1[:, cb, 0:T], Y, Y, [Bya], [Bt1])
            E("dve", lambda e, cb=cb: e.tensor_scalar(out=t1[:, cb, 0:T], in0=t1[:, cb, 0:T], scalar1=0.044715, scalar2=1.0, op0=ALU.mult, op1=ALU.add), [Bt1], [Bt1])
            mul("dve", t1[:, cb, 0:T], t1[:, cb, 0:T], Y, [Bt1, Bya], [Bt1])
            E("act", lambda e, cb=cb: e.activation(out=t1[:, cb, 0:T], in_=t1[:, cb, 0:T], func=AF.Sigmoid, scale=2.0 * math.sqrt(2.0 / math.pi)), [Bt1], [Bt1])
            mul("dve", yg[:, cb, 0:T], t1[:, cb, 0:T], Y, [Bt1, Bya], [Byg])
            E("dve", lambda e, cb=cb: e.tensor_copy(out=uT[:, cb, :], in_=yg[:, cb, 0:T]), [Byg], [BuT])
        slot, Bs = load_w([(0, 4, 512, glu_d[l].rearrange("(k p) c -> p k c", p=128))])
        gv = slot[:, 0:2048].rearrange("p (k c) -> p k c", k=4)
        wv, Bw = load_win(l, C_S5Z)
        for cb in range(4):
            ps, Bp = nextps()
            for kc in range(4):
                E("pe", lambda e, cb=cb, kc=kc, ps=ps: e.matmul(ps[:], gv[:, kc, cb * 128:(cb + 1) * 128], uT[:, kc, :], start=(kc == 0), stop=(kc == 3)), [Bs, BuT], [Bp])
            E("act", lambda e, cb=cb, ps=ps: e.activation(out=t1[:, cb, 0:T], in_=ps[:], func=AF.Sigmoid), [Bp], [Bt1])
            mul("dve", yg[:, cb, 0:T], yg[:, cb, 0:T], t1[:, cb, 0:T], [Byg, Bt1], [Byg])
            ps, Bp = proj_fm(wv, Bw, cb)
            E("act", lambda e, cb=cb, ps=ps: e.activation(out=zs[:, cb, 0:T], in_=ps[:], func=AF.Silu), [Bp], [Bzs])
            mul("dve", yb[0][0][:, cb, :], yg[:, cb, 0:T], zs[:, cb, 0:T], [Byg, Bzs], [yb[0][1]])

    def tile_layer(l, s, t, xb, load_x, store_x):
        xtile, Bx = xt[xb]
        if L == 1:
            src_d, dst_d, skey, dkey = x_d, y_d, ("x", s, t), ("y", s, t)
        else:
            src_d = x_d if l == 0 else scr_d[(l - 1) % 2]
            dst_d = y_d if l == L - 1 else scr_d[l % 2]
            skey = ("x", s, t) if l == 0 else ((l - 1) % 2, s, t)
            dkey = ("y", s, t) if l == L - 1 else (l % 2, s, t)
        if load_x:
            kb.dma("sp", xds[xb], lambda q: q.dma_start(out=xtile[:], in_=src_d[s, t * T:(t + 1) * T, :].rearrange("(tb p) d -> p tb d", p=128)), reads=[dbuf(skey)], writes=[Bx])
        for tb in range(4):
            E("act", lambda e, tb=tb: e.activation(out=junk[:], in_=xtile[:, tb, :], func=AF.Square, accum_out=st4[:, tb:tb + 1]), [Bx], [Bjunk, Bst4])
        E("dve", lambda e: e.tensor_scalar(out=st4[:, 4:8], in0=st4[:, 0:4], scalar1=1.0 / D, scalar2=EPS, op0=ALU.mult, op1=ALU.add), [Bst4], [Bst4])
        E("act", lambda e: e.activation(out=st4[:, 4:8], in_=st4[:, 4:8], func=AF.Sqrt), [Bst4], [Bst4])
        E("dve", lambda e: e.reciprocal(out=st4[:, 8:12], in_=st4[:, 4:8]), [Bst4], [Bst4])
        for tb in range(4):
            E("dve", lambda e, tb=tb: e.scalar_tensor_tensor(out=htok[:], in0=xtile[:, tb, :], scalar=st4[:, 8 + tb:9 + tb], in1=npw[:], op0=ALU.mult, op1=ALU.mult), [Bx, Bst4, Bnpw], [Bhtok])
            pt, Bpt = nextps()
            ptb = pt[:].bitcast(BF16)
            for kc in range(8):
                E("pe", lambda e, kc=kc, ptb=ptb: e.transpose(ptb[:, kc * 128:(kc + 1) * 128], htok[:, kc * 128:(kc + 1) * 128], identb[:]), [Bhtok, Bidb], [Bpt])
            E("act", lambda e, tb=tb, ptb=ptb: e.copy(out=hT[:, :, tb * 128:(tb + 1) * 128], in_=ptb.rearrange("p (k c) -> p k c", k=8)), [Bpt], [BhT])
        for b in range(4):
            if not en[b]:
                E("dve", lambda e, b=b: e.memset(yb[b][0][:], 0.0), w=[yb[b][1]])
        if en[2]:
            branch_sconv(l)
        if en[1]:
            branch_pool(l, t == 0)
        if en[3]:
            branch_mlstm(l)
        if en[0]:
            branch_s5(l)
        if dbg and t == NT - 1 and s == 0 and l == L - 1:
            for b in range(4):
                dt_, Bdt = SC[0]
                E("act", lambda e, b=b: e.copy(out=dt_[:, :, 0:T], in_=yb[b][0][:]), [yb[b][1]], [Bdt])
                kb.dma("sp", ods, lambda q, b=b: q.dma_start(out=dbg_d[b], in_=dt_[:, :, 0:T]), reads=[Bdt])
        for b in range(4):
            for hf in range(2):
                slot, Bs = load_w([(0, 4, 512, wbr_d[l, b, :, hf * 512:(hf + 1) * 512].rearrange("(k p) c -> p k c", p=128))])
                wbv = slot[:, 0:2048].rearrange("p (k c) -> p k c", k=4)
                gvw, Bg = load_win(l, C_G + b * 1024 + hf * 512)
                for jj in range(4):
                    j = hf * 4 + jj
                    ps, Bp = proj_fm(gvw, Bg, jj)
                    gs_, Bgs = gsb[0]
                    E("act", lambda e, ps=ps, gs_=gs_: e.activation(out=gs_[:], in_=ps[:], func=AF.Sigmoid), [Bp], [Bgs])
                    ps2, Bp2 = nextps()
                    for cc in range(4):
                        E("pe", lambda e, cc=cc, jj=jj, ps2=ps2, b=b: e.matmul(ps2[:], wbv[:, cc, jj * 128:(jj + 1) * 128], yb[b][0][:, cc, :], start=(cc == 0), stop=(cc == 3)), [Bs, yb[b][1]], [Bp2])
                    if b == 0:
                        mul("dve", SC[j // 4][0][:, j % 4, 0:T], ps2[:], gs_[:], [Bp2, Bgs], [SC[j // 4][1]])
                    else:
                        tm_, Btm = tmpm[0]
                        mul("dve", tm_[:], ps2[:], gs_[:], [Bp2, Bgs], [Btm])
                        if b < 3:
                            tt("dve", SC[j // 4][0][:, j % 4, 0:T], SC[j // 4][0][:, j % 4, 0:T], tm_[:], ALU.add, [SC[j // 4][1], Btm], [SC[j // 4][1]])
                        else:
                            tt("dve", mrgT[:, j, :], SC[j // 4][0][:, j % 4, 0:T], tm_[:], ALU.add, [SC[j // 4][1], Btm], [BmrgT])
        wo = []
        for hf in range(2):
            slot, Bs = load_w([(0, 8, 512, wo_d[l, :, hf * 512:(hf + 1) * 512].rearrange("(k p) c -> p k c", p=128))])
            wo.append((slot[:, 0:4096].rearrange("p (k c) -> p k c", k=8), Bs))
        for tb in range(4):
            pss = []
            for hf in range(2):
                ps, Bp = nextps()
                for j in range(8):
                    E("pe", lambda e, j=j, ps=ps, hf=hf: e.matmul(ps[:], mrgT[:, j, tb * 128:(tb + 1) * 128], wo[hf][0][:, j, :], start=(j == 0), stop=(j == 7)), [BmrgT, wo[hf][1]], [Bp])
                E("act", lambda e, ps=ps, hf=hf: e.activation(out=junk[:, 0:512], in_=ps[:], func=AF.Square, accum_out=st4[:, 12 + hf:13 + hf]), [Bp], [Bjunk, Bst4])
                pss.append((ps, Bp))
            tt("dve", st4[:, 14:15], st4[:, 12:13], st4[:, 13:14], ALU.add, [Bst4], [Bst4])
            E("dve", lambda e: e.tensor_scalar(out=st4[:, 14:15], in0=st4[:, 14:15], scalar1=1.0 / D, scalar2=EPS, op0=ALU.mult, op1=ALU.add), [Bst4], [Bst4])
            E("act", lambda e: e.activation(out=st4[:, 14:15], in_=st4[:, 14:15], func=AF.Sqrt), [Bst4], [Bst4])
            E("dve", lambda e: e.reciprocal(out=st4[:, 15:16], in_=st4[:, 14:15]), [Bst4], [Bst4])
            for hf in range(2):
                ps, Bp = pss[hf]
                E("dve", lambda e, ps=ps, hf=hf: e.scalar_tensor_tensor(out=junk[:, hf * 512:(hf + 1) * 512], in0=ps[:], scalar=st4[:, 15:16], in1=ppw[:, hf * 512:(hf + 1) * 512], op0=ALU.mult, op1=ALU.mult),
                  [Bp, Bst4, Bppw], [Bjunk])
            tt("dve", xtile[:, tb, :], xtile[:, tb, :], junk[:], ALU.add, [Bx, Bjunk], [Bx])
        if store_x:
            return kb.dma("sp", ods, lambda q: q.dma_start(out=dst_d[s, t * T:(t + 1) * T, :].rearrange("(tb p) d -> p tb d", p=128), in_=xtile[:]), reads=[Bx], writes=[dbuf(dkey)])
        return None

    last = None
    for l in range(L):
        if l > 0:
            kb.new_epoch("_%d" % l)
        layer_prep(l)
        for s in range(NSEQ):
            seq_reset()
            for t in range(NT):
                last = tile_layer(l, s, t, 0, True, True)
    kb._wait("sp", [Tok(ods.sem, ods.cnt)])
    es.close()
    return nc, kb


_CACHE = {}
WNAMES = ["norm_pre_w", "norm_post_w", "w_in", "s5_A_re", "s5_A_im", "s5_log_dt", "s5_B_re", "s5_B_im", "s5_C_re", "s5_C_im",
          "s5_D", "s5_w_glu", "pool_w", "pool_scale", "sconv_w", "mlstm_conv_w", "mlstm_b_i", "mlstm_b_f", "mlstm_norm_w",
          "w_branch", "w_out"]


def run_layers(x, weights, ncores, en=(1, 1, 1, 1), dbg=False):
    B, S, _ = x.shape
    nseq = B // ncores
    NT = S // T
    Lw = weights["w_in"].shape[0]
    key = (nseq, NT, en, dbg)
    consts = host_consts()
    cur = np.ascontiguousarray(x, dtype=np.float32)
    dbg_out = None
    for l in range(Lw):
        nc, _ = build(1, nseq, NT, en, dbg)
        in_maps = []
        for c in range(ncores):
            m = {"x": cur[c * nseq:(c + 1) * nseq]}
            for k in WNAMES:
                m[k] = np.ascontiguousarray(weights[k][l:l + 1], dtype=np.float32)
            m.update(consts)
            in_maps.append(m)
        res = run_bass_kernel_spmd(nc, in_maps, core_ids=list(range(ncores)))
        cur = np.concatenate([r["y"] for r in res.results], axis=0)
        if dbg:
            dbg_out = res.results[0]["dbg"]
    return (cur, dbg_out) if dbg else cur


def run_fused(x, weights, ncores, dbg=False):
    B, S, _ = x.shape
    nseq = B // ncores
    NT = S // T
    Lw = weights["w_in"].shape[0]
    consts = host_consts()
    nc, _ = build(Lw, nseq, NT, (1, 1, 1, 1), dbg)
    xs = np.ascontiguousarray(x, dtype=np.float32)
    wts = {k: np.ascontiguousarray(weights[k], dtype=np.float32) for k in WNAMES}
    in_maps = []
    for c in range(ncores):
        m = {"x": xs[c * nseq:(c + 1) * nseq]}
        m.update(wts)
        m.update(consts)
        in_maps.append(m)
    res = run_bass_kernel_spmd(nc, in_maps, core_ids=list(range(ncores)))
    return np.concatenate([r["y"] for r in res.results], axis=0)


def kernel(**inputs):
    x = inputs["x"]
    weights = {k: inputs[k] for k in WNAMES}
    return run_fused(x, weights, 8).astype(np.float32)
```

```python
import math
import contextlib
import numpy as np
import concourse.bass as bass
import concourse.mybir as mybir
from concourse.bass_utils import run_bass_kernel_spmd

F32 = mybir.dt.float32
BF16 = mybir.dt.bfloat16
I32 = mybir.dt.int32
AF = mybir.ActivationFunctionType
ALU = mybir.AluOpType

SAME_SYNC = True
D = 1024
W = 512
NIN = 10760
T = 512
EPS = 1e-6
C_S5U, C_S5Z, C_PU, C_PZ, C_SX, C_SB, C_SC, C_SZ = 0, 512, 1024, 1536, 2048, 2560, 3072, 3584
C_Q, C_K, C_V, C_O, C_IF, C_MZ, C_G = 4096, 4608, 5120, 5632, 6144, 6152, 6664
POOL_WINDOWS = (2, 4, 8, 16)


class Tok:
    __slots__ = ("sem", "val")

    def __init__(self, sem, val):
        self.sem = sem
        self.val = val


class Buf:
    def __init__(self, name=""):
        self.name = name
        self.w = None
        self.r = {}


class DSem:
    def __init__(self, sem):
        self.sem = sem
        self.cnt = 0


class KB:
    def __init__(self, nc, es):
        self.nc = nc
        self.es = es
        self.eng = {"pe": nc.tensor, "act": nc.scalar, "dve": nc.vector, "pool": nc.gpsimd, "sp": nc.sync}
        self.sem = {e: es.enter_context(nc.semaphore("s_" + e)) for e in self.eng}
        self.cnt = {e: 0 for e in self.eng}
        self.seen = {e: {} for e in self.eng}
        self.nins = 0
        self.uid = 0

    def dsem(self, name):
        return DSem(self.es.enter_context(self.nc.semaphore(name)))

    def new_epoch(self, tag):
        self.sem = {e: self.es.enter_context(self.nc.semaphore("s_" + e + tag)) for e in self.eng}
        self.cnt = {e: 0 for e in self.eng}

    def sb(self, name, shape, dt):
        return self.es.enter_context(self.nc.sbuf_tensor(name, list(shape), dt))

    def _wait(self, e, toks):
        need = {}
        for t in toks:
            if t is None:
                continue
            k = id(t.sem)
            if k not in need or need[k].val < t.val:
                need[k] = t
        for k, t in need.items():
            if t.sem is self.sem[e] and (e == "pe" or not SAME_SYNC):
                continue
            if self.seen[e].get(k, 0) >= t.val:
                continue
            self.eng[e].wait_ge(t.sem, t.val)
            self.seen[e][k] = t.val

    def _deps(self, reads, writes):
        toks = []
        for b in reads:
            toks.append(b.w)
        for b in writes:
            toks.append(b.w)
            toks.extend(b.r.values())
        return toks

    def _upd(self, tok, reads, writes):
        k = id(tok.sem)
        for b in reads:
            b.r[k] = tok
        for b in writes:
            b.w = tok
            b.r = {}

    def op(self, e, fn, reads=(), writes=()):
        self._wait(e, self._deps(reads, writes))
        ins = fn(self.eng[e])
        self.cnt[e] += 1
        ins.then_inc(self.sem[e], 1)
        tok = Tok(self.sem[e], self.cnt[e])
        self._upd(tok, reads, writes)
        self.nins += 1
        return tok

    def dma(self, e, ds, fn, reads=(), writes=()):
        self._wait(e, self._deps(reads, writes))
        ins = fn(self.eng[e])
        ds.cnt += 16
        ins.then_inc(ds.sem, 16)
        tok = Tok(ds.sem, ds.cnt)
        self._upd(tok, reads, writes)
        self.nins += 1
        return tok


def host_consts():
    ident = np.eye(128, dtype=np.float32)
    m = np.arange(128)[:, None]
    l = np.arange(128)[None, :]
    tri = (m <= l).astype(np.float32)
    ones = np.ones((128, 128), np.float32)
    band = np.zeros((3, 4, 128, 128), np.float32)
    for g, win in enumerate(POOL_WINDOWS):
        dlt = l - m
        inw = (dlt >= 0) & (dlt < win)
        band[0, g] = inw / win - ident
        dp = l + 128 - m
        band[1, g] = ((dp >= 0) & (dp < win)) / win
        band[2, g] = inw / np.minimum(l + 1, win) - ident
    return {"c_ident": ident, "c_tri": tri, "c_ones": ones,
            "c_band": np.ascontiguousarray(band.transpose(2, 0, 1, 3)).reshape(128, 12 * 128)}


def build(L, NSEQ, NT, en=(1, 1, 1, 1), dbg=False):
    S = NT * T
    nc = bass.Bass("TRN2", target_bir_lowering=False)
    es = contextlib.ExitStack()
    kb = KB(nc, es)

    def din(name, shape):
        return nc.dram_tensor(name, list(shape), F32, kind="ExternalInput").ap()

    x_d = din("x", [NSEQ, S, D])
    npw_d = din("norm_pre_w", [L, D]); ppw_d = din("norm_post_w", [L, D])
    win_d = din("w_in", [L, D, NIN])
    are_d = din("s5_A_re", [L, 32, 64]); aim_d = din("s5_A_im", [L, 32, 64]); ldt_d = din("s5_log_dt", [L, 32])
    bre_d = din("s5_B_re", [L, 32, 64, 16]); bim_d = din("s5_B_im", [L, 32, 64, 16])
    cre_d = din("s5_C_re", [L, 32, 16, 64]); cim_d = din("s5_C_im", [L, 32, 16, 64])
    s5d_d = din("s5_D", [L, W]); glu_d = din("s5_w_glu", [L, W, W])
    pw_d = din("pool_w", [L, 4, 128, 128]); psc_d = din("pool_scale", [L, W])
    scw_d = din("sconv_w", [L, 3, W]); mcw_d = din("mlstm_conv_w", [L, 4, 2 * W])
    mbi_d = din("mlstm_b_i", [L, 4]); mbf_d = din("mlstm_b_f", [L, 4]); mnw_d = din("mlstm_norm_w", [L, W])
    wbr_d = din("w_branch", [L, 4, W, D]); wo_d = din("w_out", [L, D, D])
    cid_d = din("c_ident", [128, 128]); ctri_d = din("c_tri", [128, 128]); cone_d = din("c_ones", [128, 128])
    cband_d = din("c_band", [128, 12 * 128])
    y_d = nc.dram_tensor("y", [NSEQ, S, D], F32, kind="ExternalOutput").ap()
    dbg_d = nc.dram_tensor("dbg", [4, 128, 4, T], F32, kind="ExternalOutput").ap() if dbg else None
    scr_d = [nc.dram_tensor("xscr%d" % i, [NSEQ, S, D], F32, kind="Internal").ap() for i in range(2)] if L > 1 else []
    dram_bufs = {}
    NIMG = 32
    SLW = 8 * 520
    wimg_d = [nc.dram_tensor("wimg%d" % l_, [NIMG, 128, SLW], BF16, kind="Internal").ap() for l_ in range(L)]
    Bimg = [Buf("wimg") for _ in range(L)]
    cvds = [None] * L
    img_index = {}

    def img_specs(l):
        sp = []
        for c0 in (C_S5U, C_S5Z, C_PU, C_PZ, C_SX, C_SB, C_SC, C_SZ, C_Q, C_K, C_V, C_O):
            sp.append((("win", c0), 8, 512, win_d[l, :, c0:c0 + 512].rearrange("(k p) c -> p k c", p=128)))
        sp.append((("win", C_IF), 8, 520, win_d[l, :, C_IF:C_IF + 520].rearrange("(k p) c -> p k c", p=128)))
        for i in range(8):
            c0 = C_G + i * 512
            sp.append((("win", c0), 8, 512, win_d[l, :, c0:c0 + 512].rearrange("(k p) c -> p k c", p=128)))
        sp.append((("glu",), 4, 512, glu_d[l].rearrange("(k p) c -> p k c", p=128)))
        for b in range(4):
            for hf in range(2):
                sp.append((("wbr", b, hf), 4, 512, wbr_d[l, b, :, hf * 512:(hf + 1) * 512].rearrange("(k p) c -> p k c", p=128)))
        for hf in range(2):
            sp.append((("wo", hf), 8, 512, wo_d[l, :, hf * 512:(hf + 1) * 512].rearrange("(k p) c -> p k c", p=128)))
        return sp

    def convert_layer(l):
        cvds[l] = kb.dsem("cvds%d" % l)
        for i, (key, k, c, src) in enumerate(img_specs(l)):
            img_index[(l,) + key] = (i, k, c)
            for kk in range(k):
                dst = wimg_d[l][i, :, kk * c:(kk + 1) * c]
                kb.dma("pool", cvds[l], lambda q, dst=dst, src=src, kk=kk: q.dma_start(out=dst, in_=src[:, kk, :]), writes=[Bimg[l]])
        Bimg[l].w = Tok(cvds[l].sem, cvds[l].cnt)

    def load_img(l, key):
        i, k, c = img_index[(l,) + key]
        si = wsi[0] % NSL
        wsi[0] += 1
        slot, Bs = wsl[si]
        kb.dma("sp", wds[si], lambda q: q.dma_start(out=slot[:, 0:k * c], in_=wimg_d[l][i, :, 0:k * c]), reads=[Bimg[l]], writes=[Bs])
        return slot[:, 0:k * c].rearrange("p (k c) -> p k c", k=k), Bs

    def dbuf(key):
        if key not in dram_bufs:
            dram_bufs[key] = Buf("dram")
        return dram_bufs[key]

    uid = [0]

    def sbt(shape, dt, name=None):
        uid[0] += 1
        return kb.sb((name or "t") + str(uid[0]), shape, dt), Buf(name or "t")
    sbt_global = sbt

    NPS = 8
    psb = [es.enter_context(nc.psum_tensor("ps%d" % i, [128, 512], F32)) for i in range(NPS)]
    psB = [Buf("ps%d" % i) for i in range(NPS)]
    psi = [0]

    def nextps():
        i = psi[0] % NPS
        psi[0] += 1
        return psb[i], psB[i]

    def E(e, fn, r=(), w=()):
        return kb.op(e, fn, r, w)

    cdsem = kb.dsem("cdsem")

    cbufs = []

    def cbarrier():
        for b_ in cbufs:
            b_.w = Tok(cdsem.sem, cdsem.cnt)
        del cbufs[:]

    def cload(e, out_ap, in_ap, wb, slow=False):
        cbufs.append(wb)
        return kb.dma(e, cdsem, lambda q: q.dma_start(out=out_ap, in_=in_ap, allow_slow_non_contiguous=True) if slow
                      else q.dma_start(out=out_ap, in_=in_ap), writes=[wb])

    identf, Bidf = sbt([128, 128], F32, "identf")
    identb, Bidb = sbt([128, 128], BF16, "identb")
    trif, Btrif = sbt([128, 128], F32, "trif")
    onesf, Bonesf = sbt([128, 128], F32, "onesf")
    band, Bband = sbt([128, 3, 4, 128], BF16, "band")
    cload("sp", identf[:], cid_d, Bidf)
    cload("pool", identb[:], cid_d, Bidb)
    cload("sp", trif[:], ctri_d, Btrif)
    cload("sp", onesf[:], cone_d, Bonesf)
    cload("pool", band[:].rearrange("p a g l -> p (a g l)"), cband_d, Bband)
    cbarrier()

    xt = [sbt([128, 4, D], F32, "xt") for _ in range(1)]
    xds = [kb.dsem("xds%d" % i) for i in range(1)]
    ods = kb.dsem("ods")
    htok, Bhtok = sbt([128, D], BF16, "htok")
    hT, BhT = sbt([128, 8, T], BF16, "hT")
    st4, Bst4 = sbt([128, 16], F32, "st4")
    NSL = 3
    SLW = 8 * 520
    wsl = [sbt([128, SLW], BF16, "wsl") for _ in range(NSL)]
    wds = [kb.dsem("wds%d" % i) for i in range(NSL)]
    wsi = [0]

    def load_w(views):
        i = wsi[0] % NSL
        wsi[0] += 1
        slot, Bs = wsl[i]
        for (off, kk, cc, src) in views:
            dst = slot[:, off:off + kk * cc].rearrange("p (k c) -> p k c", k=kk)
            kb.dma("pool", wds[i], lambda q, dst=dst, src=src: q.dma_start(out=dst, in_=src), writes=[Bs])
        return slot, Bs

    def win_view(l, c0, n):
        return win_d[l, :, c0:c0 + n].rearrange("(k p) c -> p k c", p=128)

    def load_win(l, c0, n=512):
        return load_img(l, ("win", c0))

    def proj_fm(wv, Bw, jb):
        ps, Bp = nextps()
        for kc in range(8):
            E("pe", lambda e, kc=kc: e.matmul(ps[:], wv[:, kc, jb * 128:(jb + 1) * 128], hT[:, kc, :], start=(kc == 0), stop=(kc == 7)),
              [Bw, BhT], [Bp])
        return ps, Bp

    def proj_tm(wv, Bw, tb, c0, n):
        ps, Bp = nextps()
        for kc in range(8):
            E("pe", lambda e, kc=kc: e.matmul(ps[:, 0:n], hT[:, kc, tb * 128:(tb + 1) * 128], wv[:, kc, c0:c0 + n], start=(kc == 0), stop=(kc == 7)),
              [Bw, BhT], [Bp])
        return ps, Bp

    yb = [sbt([128, 4, T], BF16, "yb") for _ in range(4)]
    SC = [sbt([128, 4, T + 4], F32, "scr") for _ in range(4)]

    def scflat(i):
        return SC[i][0][:].rearrange("p a b -> p (a b)")

    class _V0:
        def __init__(self, ap):
            self.ap = ap

        def __getitem__(self, k):
            return self.ap[k]
    junk, Bjunk = _V0(scflat(2)[:, 0:D]), SC[2][1]

    def sc_half(i, h):
        return scflat(i)[:, h * 1024:(h + 1) * 1024].rearrange("p (g c) -> p g c", g=16)
    qkT, BqkT = sbt([128, 8, T], BF16, "qkT")
    mrgT, BmrgT = qkT, BqkT
    gsb = [(_V0(SC[2][0][:, 0, 0:T]), SC[2][1])]
    tmpm = [(_V0(SC[3][0][:, 0, 0:T]), SC[3][1])]

    npw, Bnpw = sbt([128, D], F32, "npw"); ppw, Bppw = sbt([128, D], F32, "ppw")
    scw, Bscw = sbt([128, 3, 4], F32, "scw"); mcw, Bmcw = sbt([128, 4, 8], F32, "mcw")
    psc, Bpsc = sbt([128, 4], F32, "psc"); s5dd, Bs5dd = sbt([128, 4], F32, "s5dd")
    mnw, Bmnw = sbt([128, W], F32, "mnw"); mbi, Bmbi = sbt([128, 4], F32, "mbi"); mbf, Bmbf = sbt([128, 4], F32, "mbf")
    poolw, Bpoolw = sbt([128, 4, 128], BF16, "poolw")
    schalo, Bschalo = sbt([128, 4, 2], F32, "schalo")
    putok, Bputok = sbt([128, 5, W], BF16, "putok")
    mlhalo, Bmlhalo = sbt([128, 8, 3], F32, "mlhalo")
    C32, BC32 = sbt([128, 4, 129], F32, "C32"); Cbf, BCbf = sbt([128, 4, 129], BF16, "Cbf")
    car = [sbt([128, 16], F32, "car") for _ in range(2)]
    Min, BMin = sbt([128, 4, 8, 2, 128], BF16, "Min")
    Mout, BMout = sbt([128, 16, 8, 2, 32], BF16, "Mout")
    Ktoe, BKtoe = sbt([128, 4, 8, 128], BF16, "Ktoe")
    Etab = [sbt([128, 16, 64], F32, "Etab") for _ in range(2)]
    rho, Brho = sbt([128, 16], F32, "rho")

    class _V:
        def __init__(self, ap):
            self.ap = ap

        def __getitem__(self, k):
            return self.ap[k]
    Zpad = [(_V(scflat(i)[:, 0:2048].rearrange("p (g c) -> p g c", g=16)), SC[i][1]) for i in range(2)]
    Cpad = [(_V(scflat(i)[:, 0:2048].rearrange("p (g c) -> p g c", g=16)), SC[i][1]) for i in range(2, 4)]
    E("dve", lambda e: e.memset(Mout[:], 0.0), w=[BMout])
    qT, BqT = _V(qkT[:, 0:4, :]), BqkT; kT, BkT = _V(qkT[:, 4:8, :]), BqkT
    vtok, Bvtok = sbt([128, 4, 4, 129], BF16, "vtok")
    E("dve", lambda e: e.memset(vtok[:], 1.0), w=[Bvtok])
    ogt, Bogt = sbt([128, 4, W], BF16, "ogt"); zwt, Bzwt = sbt([128, 4, W], BF16, "zwt")
    gt, Bgt = sbt([128, 4, 40], F32, "gt")
    ktil, Bktil = sbt([128, 128], BF16, "ktil"); PT, BPT = sbt([128, 128], BF16, "PT")
    hh, Bhh = sbt([128, 4, 128], F32, "hh"); ydtok, Bydtok = sbt([128, W], BF16, "ydtok")
    sm, Bsm = sbt([128, 16], F32, "sm")
    uT, BuT = sbt([128, 4, T], BF16, "uT"); u32, Bu32 = uT, BuT
    s5t = [(_V(sc_half(0, h)), SC[0][1]) for h in range(2)]
    Hs = [(_V(sc_half(1, h)), SC[1][1]) for h in range(2)]
    Xr = [(_V(sc_half(2, h)), SC[2][1]) for h in range(2)]
    Hu = Xr
    HpT, BHpT = sbt([128, 2, 16, 64], BF16, "HpT")
    Hp = [(_V(HpT[:, c]), BHpT) for c in range(2)]
    pooledT, BpooledT = _V(HpT[:].rearrange("p a g c -> p (a g c)").rearrange("p (g t) -> p g t", g=4)), BHpT

    def mul(e, o, a, b, r, w):
        return E(e, lambda q: q.tensor_tensor(out=o, in0=a, in1=b, op=ALU.mult), r, w)

    def tt(e, o, a, b, op, r, w):
        return E(e, lambda q: q.tensor_tensor(out=o, in0=a, in1=b, op=op), r, w)

    prep_cache = {}

    def layer_prep(l):
        pidx = [0]

        def sbt(shape, dt, name=None):
            pidx[0] += 1
            k = (name, pidx[0])
            if k not in prep_cache:
                prep_cache[k] = sbt_global(shape, dt, name)
            return prep_cache[k]

        def bc(v):
            return v.partition_broadcast(128)
        cload("sp", npw[:], bc(npw_d[l]), Bnpw); cload("sp", ppw[:], bc(ppw_d[l]), Bppw)
        cload("sp", scw[:], scw_d[l].rearrange("k (cb p) -> p k cb", p=128), Bscw, slow=True)
        cload("sp", mcw[:], mcw_d[l].rearrange("k (cb p) -> p k cb", p=128), Bmcw, slow=True)
        cload("sp", psc[:], psc_d[l].rearrange("(cb p) -> p cb", p=128), Bpsc, slow=True)
        cload("sp", s5dd[:], s5d_d[l].rearrange("(cb p) -> p cb", p=128), Bs5dd, slow=True)
        cload("sp", mnw[:], bc(mnw_d[l]), Bmnw); cload("sp", mbi[:], bc(mbi_d[l]), Bmbi); cload("sp", mbf[:], bc(mbf_d[l]), Bmbf)
        cload("pool", poolw[:], pw_d[l].rearrange("g c d -> c g d"), Bpoolw)
        if not en[0]:
            cbarrier()
            return
        for tt_, bb_ in Zpad + Cpad:
            E("dve", lambda e, tt_=tt_: e.memset(tt_[:], 0.0), w=[bb_])
        def arena(i):
            return yb[i][0][:].rearrange("p a b -> p (a b)").bitcast(F32)

        def carve(i, off, shp):
            n = shp[0] * shp[1]
            return (_V(arena(i)[:, off:off + n].rearrange("p (a b) -> p a b", a=shp[0])), yb[i][1])
        A = [sbt([128, 16], F32, "A") for _ in range(2)]
        ldt, Bldt = sbt([128, 16], F32, "ldt")
        Bm = [carve(0, c * 256, (16, 16)) for c in range(2)]
        Cm = [carve(0, 512 + c * 256, (16, 16)) for c in range(2)]
        cload("sp", A[0][0][:], are_d[l].rearrange("(gp gl) n -> (gl n) gp", gl=2), A[0][1], slow=True)
        cload("sp", A[1][0][:], aim_d[l].rearrange("(gp gl) n -> (gl n) gp", gl=2), A[1][1], slow=True)
        for gl in range(2):
            cload("sp", ldt[gl * 64:(gl + 1) * 64, :], ldt_d[l, gl::2].partition_broadcast(64), Bldt, slow=True)
        for c, src in enumerate((bre_d, bim_d)):
            cload("sp", Bm[c][0][:], src[l].rearrange("(gp gl) n q -> (gl n) gp q", gl=2), Bm[c][1], slow=True)
        for c, src in enumerate((cre_d, cim_d)):
            for gl in range(2):
                for gp in range(16):
                    cload("sp", Cm[c][0][gl * 64:(gl + 1) * 64, gp, :], src[l, 2 * gp + gl].rearrange("p n -> n p"), Cm[c][1], slow=True)
        cbarrier()
        tn = [sbt([128, 16], F32, "tn") for _ in range(12)]

        def V(i):
            return tn[i][0][:]

        def Bv(i):
            return tn[i][1]
        Are, Aim = A[0][0][:], A[1][0][:]
        BA = [A[0][1], A[1][1]]
        E("act", lambda e: e.activation(out=ldt[:], in_=ldt[:], func=AF.Exp), [Bldt], [Bldt])
        mul("dve", V(0), Are, ldt[:], [BA[0], Bldt], [Bv(0)])
        mul("dve", V(1), Aim, ldt[:], [BA[1], Bldt], [Bv(1)])
        E("act", lambda e: e.activation(out=V(2), in_=V(0), func=AF.Exp), [Bv(0)], [Bv(2)])
        E("act", lambda e: e.activation(out=V(11), in_=V(0), func=AF.Exp, scale=8.0), [Bv(0)], [Bv(11)])
        ki, Bki = sbt([128, 16], I32, "ki")
        E("dve", lambda e: e.tensor_scalar(out=V(3), in0=V(1), scalar1=1.0 / (2 * math.pi), scalar2=None, op0=ALU.mult), [Bv(1)], [Bv(3)])
        E("dve", lambda e: e.tensor_copy(out=ki[:], in_=V(3)), [Bv(3)], [Bki])
        E("dve", lambda e: e.tensor_copy(out=V(3), in_=ki[:]), [Bki], [Bv(3)])
        E("dve", lambda e: e.scalar_tensor_tensor(out=V(4), in0=V(3), scalar=-2 * math.pi, in1=V(1), op0=ALU.mult, op1=ALU.add), [Bv(3), Bv(1)], [Bv(4)])
        E("act", lambda e: e.activation(out=V(5), in_=V(4), func=AF.Sin, scale=0.5), [Bv(4)], [Bv(5)])
        E("act", lambda e: e.activation(out=V(6), in_=V(4), func=AF.Sin, scale=0.25), [Bv(4)], [Bv(6)])
        mul("dve", V(7), V(6), V(6), [Bv(6)], [Bv(7)])
        E("dve", lambda e: e.tensor_scalar(out=V(7), in0=V(7), scalar1=-2.0, scalar2=1.0, op0=ALU.mult, op1=ALU.add), [Bv(7)], [Bv(7)])
        mul("dve", V(8), V(5), V(7), [Bv(5), Bv(7)], [Bv(8)])
        mul("dve", V(5), V(5), V(5), [Bv(5)], [Bv(5)])
        E("dve", lambda e: e.tensor_scalar(out=V(5), in0=V(5), scalar1=-2.0, scalar2=1.0, op0=ALU.mult, op1=ALU.add), [Bv(5)], [Bv(5)])
        Pw = [sbt([128, 9, 16], F32, "Pw") for _ in range(2)]
        Pr, Pi = Pw[0][0], Pw[1][0]
        BP = [Pw[0][1], Pw[1][1]]
        E("dve", lambda e: e.memset(Pr[:, 0, :], 1.0), w=[BP[0]])
        E("dve", lambda e: e.memset(Pi[:, 0, :], 0.0), w=[BP[1]])
        mul("dve", Pr[:, 1, :], V(2), V(5), [Bv(2), Bv(5)], [BP[0]])
        E("dve", lambda e: e.scalar_tensor_tensor(out=Pi[:, 1, :], in0=V(8), scalar=2.0, in1=V(2), op0=ALU.mult, op1=ALU.mult), [Bv(8), Bv(2)], [BP[1]])

        def cmul(o_r, o_i, xr, xi, yr, yi, r, w, t1, t2, Bt):
            mul("dve", t1, xr, yr, r, Bt); mul("dve", t2, xi, yi, r, Bt)
            tt("dve", o_r, t1, t2, ALU.subtract, Bt, [w[0]])
            mul("dve", t1, xr, yi, r, Bt); mul("dve", t2, xi, yr, r, Bt)
            tt("dve", o_i, t1, t2, ALU.add, Bt, [w[1]])
        for j in range(1, 8):
            cmul(Pr[:, j + 1, :], Pi[:, j + 1, :], Pr[:, j, :], Pi[:, j, :], Pr[:, 1, :], Pi[:, 1, :], BP, BP, V(9), V(10), [Bv(9), Bv(10)])
        E("dve", lambda e: e.reciprocal(out=V(0), in_=V(11)), [Bv(11)], [Bv(0)])
        wl = [sbt([128, 16], F32, "wl") for _ in range(4)]
        mul("dve", wl[0][0][:], Pr[:, 8, :], V(0), [BP[0], Bv(0)], [wl[0][1]])
        E("dve", lambda e: e.scalar_tensor_tensor(out=wl[1][0][:], in0=Pi[:, 8, :], scalar=-1.0, in1=V(0), op0=ALU.mult, op1=ALU.mult), [BP[1], Bv(0)], [wl[1][1]])
        Er, Ei = Etab[0][0], Etab[1][0]
        BE = [Etab[0][1], Etab[1][1]]
        E("dve", lambda e: e.tensor_copy(out=Er[:, :, 0], in_=wl[0][0][:]), [wl[0][1]], [BE[0]])
        E("dve", lambda e: e.tensor_copy(out=Ei[:, :, 0], in_=wl[1][0][:]), [wl[1][1]], [BE[1]])
        t3a, Bt3a = carve(2, 0, (16, 32)); t3b, Bt3b = carve(2, 512, (16, 32))
        cur = 0
        Lw = 1
        while Lw < 64:
            wr = wl[cur][0][:].unsqueeze(2).to_broadcast([128, 16, Lw]); wi = wl[cur + 1][0][:].unsqueeze(2).to_broadcast([128, 16, Lw])
            Bw = [wl[cur][1], wl[cur + 1][1]]
            cmul(Er[:, :, Lw:2 * Lw], Ei[:, :, Lw:2 * Lw], Er[:, :, 0:Lw], Ei[:, :, 0:Lw], wr, wi, BE + Bw, BE, t3a[:, :, 0:Lw], t3b[:, :, 0:Lw], [Bt3a, Bt3b])
            nxt = 2 - cur
            cmul(wl[nxt][0][:], wl[nxt + 1][0][:], wl[cur][0][:], wl[cur + 1][0][:], wl[cur][0][:], wl[cur + 1][0][:], Bw, [wl[nxt][1], wl[nxt + 1][1]], V(9), V(10), [Bv(9), Bv(10)])
            cur = nxt
            Lw *= 2
        E("dve", lambda e: e.tensor_copy(out=rho[:], in_=V(11)), [Bv(11)], [Brho])
        E("dve", lambda e: e.tensor_scalar(out=V(0), in0=Pr[:, 1, :], scalar1=-1.0, scalar2=None, op0=ALU.add), [BP[0]], [Bv(0)])
        mul("dve", V(1), Are, Are, [BA[0]], [Bv(1)]); mul("dve", V(2), Aim, Aim, [BA[1]], [Bv(2)])
        tt("dve", V(1), V(1), V(2), ALU.add, [Bv(1), Bv(2)], [Bv(1)])
        E("dve", lambda e: e.reciprocal(out=V(1), in_=V(1)), [Bv(1)], [Bv(1)])
        mul("dve", V(2), V(0), Are, [Bv(0), BA[0]], [Bv(2)]); mul("dve", V(3), Pi[:, 1, :], Aim, [BP[1], BA[1]], [Bv(3)])
        tt("dve", V(2), V(2), V(3), ALU.add, [Bv(2), Bv(3)], [Bv(2)]); mul("dve", V(4), V(2), V(1), [Bv(2), Bv(1)], [Bv(4)])
        mul("dve", V(2), Pi[:, 1, :], Are, [BP[1], BA[0]], [Bv(2)]); mul("dve", V(3), V(0), Aim, [Bv(0), BA[1]], [Bv(3)])
        tt("dve", V(2), V(2), V(3), ALU.subtract, [Bv(2), Bv(3)], [Bv(2)]); mul("dve", V(5), V(2), V(1), [Bv(2), Bv(1)], [Bv(5)])
        Bb = [carve(1, c * 256, (16, 16)) for c in range(2)]
        Zz = [carve(1, 512 + c * 256, (16, 16)) for c in range(2)]
        t4a, Bt4a = carve(3, 0, (16, 16)); t4b, Bt4b = carve(3, 256, (16, 16))

        def b16(v):
            return v.unsqueeze(2).to_broadcast([128, 16, 16])
        cmul(Bb[0][0][:], Bb[1][0][:], b16(V(4)), b16(V(5)), Bm[0][0][:], Bm[1][0][:], [Bv(4), Bv(5), Bm[0][1], Bm[1][1]], [Bb[0][1], Bb[1][1]], t4a[:], t4b[:], [Bt4a, Bt4b])
        E("dve", lambda e: e.tensor_scalar(out=t4a[:], in0=Cm[1][0][:], scalar1=-1.0, scalar2=None, op0=ALU.mult), [Cm[1][1]], [Bt4a])
        csrc = [(Cm[0][0], Cm[0][1]), (t4a, Bt4a)]

        def padfill(dst, Bd, src, Bs):
            d5 = dst[:].rearrange("p (gq j) (jj c) -> p gq j jj c", j=4, jj=4)
            s4 = src[:].rearrange("p (gq j) q -> p gq j q", j=4)
            for j in range(4):
                for gl in range(2):
                    E("act", lambda e, j=j, gl=gl: e.copy(out=d5[gl * 64:(gl + 1) * 64, :, j, j, gl * 16:(gl + 1) * 16], in_=s4[gl * 64:(gl + 1) * 64, :, j, :]), [Bs], [Bd])
        for c in range(2):
            padfill(Cpad[c][0], Cpad[c][1], csrc[c][0], csrc[c][1])
        for jp in range(8):
            cmul(Zz[0][0][:], Zz[1][0][:], b16(Pr[:, jp, :]), b16(Pi[:, jp, :]), Bb[0][0][:], Bb[1][0][:], BP + [Bb[0][1], Bb[1][1]], [Zz[0][1], Zz[1][1]], t4a[:], t4b[:], [Bt4a, Bt4b])
            for c in range(2):
                padfill(Zpad[c][0], Zpad[c][1], Zz[c][0], Zz[c][1])
            s = 7 - jp
            for gq in range(4):
                for c in range(2):
                    ps, Bp = nextps()
                    for j in range(4):
                        E("pe", lambda e, j=j, c=c, gq=gq: e.matmul(ps[:, 0:128], Zpad[c][0][:, 4 * gq + j, :], identf[:], start=(j == 0), stop=(j == 3)), [Zpad[c][1], Bidf], [Bp])
                    E("dve", lambda e, c=c, gq=gq, ps=ps: e.tensor_copy(out=Min[:, gq, s, c, :], in_=ps[:, 0:128]), [Bp], [BMin])
                ps, Bp = nextps()
                for j in range(4):
                    for c in range(2):
                        E("pe", lambda e, j=j, c=c, gq=gq: e.matmul(ps[:, 0:128], Zpad[c][0][:, 4 * gq + j, :], Cpad[c][0][:, 4 * gq + j, :], start=(j == 0 and c == 0), stop=(j == 3 and c == 1)),
                          [Zpad[c][1], Cpad[c][1]], [Bp])
                E("dve", lambda e, gq=gq, ps=ps: e.tensor_copy(out=Ktoe[:, gq, jp, :], in_=ps[:, 0:128]), [Bp], [BKtoe])
        for t in range(8):
            cmul(Zz[0][0][:], Zz[1][0][:], b16(Pr[:, t + 1, :]), b16(Pi[:, t + 1, :]), Cm[0][0][:], Cm[1][0][:], BP + [Cm[0][1], Cm[1][1]], [Zz[0][1], Zz[1][1]], t4a[:], t4b[:], [Bt4a, Bt4b])
            for gl in range(2):
                E("dve", lambda e, gl=gl, t=t: e.tensor_copy(out=Mout[gl * 64:(gl + 1) * 64, :, t, 0, gl * 16:(gl + 1) * 16], in_=Zz[0][0][gl * 64:(gl + 1) * 64]), [Zz[0][1]], [BMout])
                E("dve", lambda e, gl=gl, t=t: e.tensor_scalar(out=Mout[gl * 64:(gl + 1) * 64, :, t, 1, gl * 16:(gl + 1) * 16], in0=Zz[1][0][gl * 64:(gl + 1) * 64], scalar1=-1.0, scalar2=None, op0=ALU.mult), [Zz[1][1]], [BMout])

    def seq_reset():
        E("dve", lambda e: e.memset(schalo[:], 0.0), w=[Bschalo])
        E("dve", lambda e: e.memset(putok[:, 0, :], 0.0), w=[Bputok])
        E("dve", lambda e: e.memset(mlhalo[:], 0.0), w=[Bmlhalo])
        E("dve", lambda e: e.memset(C32[:], 0.0), w=[BC32])
        E("dve", lambda e: e.memset(Cbf[:], 0.0), w=[BCbf])
        for c in range(2):
            E("dve", lambda e, c=c: e.memset(car[c][0][:], 0.0), w=[car[c][1]])

    def branch_sconv(l):
        xs, Bxs = SC[0]; prod, Bprod = SC[1]; acc, Bacc = SC[2]; zs, Bzs = SC[3]
        wv, Bw = load_win(l, C_SX)
        for cb in range(4):
            ps, Bp = proj_fm(wv, Bw, cb)
            E("act", lambda e, cb=cb, ps=ps: e.copy(out=xs[:, cb, 0:T], in_=ps[:]), [Bp], [Bxs])
        E("dve", lambda e: e.tensor_copy(out=prod[:, :, 0:2], in_=schalo[:]), [Bschalo], [Bprod])
        wv, Bw = load_win(l, C_SC)
        for cb in range(4):
            ps, Bp = proj_fm(wv, Bw, cb)
            mul("dve", prod[:, cb, 2:T + 2], ps[:], xs[:, cb, 0:T], [Bp, Bxs], [Bprod])
        E("dve", lambda e: e.tensor_copy(out=schalo[:], in_=prod[:, :, T:T + 2]), [Bprod], [Bschalo])
        for cb in range(4):
            E("dve", lambda e, cb=cb: e.tensor_scalar(out=acc[:, cb, 0:T], in0=prod[:, cb, 2:T + 2], scalar1=scw[:, 2, cb:cb + 1], scalar2=None, op0=ALU.mult), [Bprod, Bscw], [Bacc])
            for k in (1, 0):
                E("dve", lambda e, cb=cb, k=k: e.scalar_tensor_tensor(out=acc[:, cb, 0:T], in0=prod[:, cb, k:k + T], scalar=scw[:, k, cb:cb + 1], in1=acc[:, cb, 0:T], op0=ALU.mult, op1=ALU.add),
                  [Bprod, Bscw, Bacc], [Bacc])
        wv, Bw = load_win(l, C_SB)
        for cb in range(4):
            ps, Bp = proj_fm(wv, Bw, cb)
            mul("dve", acc[:, cb, 0:T], ps[:], acc[:, cb, 0:T], [Bp, Bacc], [Bacc])
        wv, Bw = load_win(l, C_SZ)
        for cb in range(4):
            ps, Bp = proj_fm(wv, Bw, cb)
            E("act", lambda e, cb=cb, ps=ps: e.activation(out=zs[:, cb, 0:T], in_=ps[:], func=AF.Silu), [Bp], [Bzs])
            mul("dve", yb[2][0][:, cb, :], acc[:, cb, 0:T], zs[:, cb, 0:T], [Bacc, Bzs], [yb[2][1]])

    def branch_pool(l, first):
        import os
        stage = int(os.environ.get("POOLDBG", "9"))
        zs, Bzs = SC[3]
        wv, Bw = load_win(l, C_PU)
        for tb in range(4):
            if stage == -2:
                continue
            ps, Bp = proj_tm(wv, Bw, tb, 0, 512)
            if stage == -1:
                continue
            E("dve", lambda e, tb=tb, ps=ps: e.tensor_copy(out=putok[:, tb + 1, :], in_=ps[:]), [Bp], [Bputok])
        if stage < 1:
            E("dve", lambda e: e.memset(yb[1][0][:], 0.0), w=[yb[1][1]])
            return
        for g in range(4):
            ps, Bp = nextps()
            for tb in range(4):
                f0 = first and tb == 0
                E("pe", lambda e, g=g, tb=tb, ps=ps, f0=f0: e.matmul(ps[:, tb * 128:(tb + 1) * 128], putok[:, tb + 1, g * 128:(g + 1) * 128], band[:, 2 if f0 else 0, g, :], start=True, stop=f0),
                  [Bputok, Bband], [Bp])
                if not f0:
                    E("pe", lambda e, g=g, tb=tb, ps=ps: e.matmul(ps[:, tb * 128:(tb + 1) * 128], putok[:, tb, g * 128:(g + 1) * 128], band[:, 1, g, :], start=False, stop=True),
                      [Bputok, Bband], [Bp])
            E("dve", lambda e, g=g, ps=ps: e.tensor_copy(out=pooledT[:, g, :], in_=ps[:]), [Bp], [BpooledT])
        E("dve", lambda e: e.tensor_copy(out=putok[:, 0, :], in_=putok[:, 4, :]), [Bputok], [Bputok])
        if stage < 2:
            E("dve", lambda e: e.memset(yb[1][0][:], 0.0), w=[yb[1][1]])
            return
        wv, Bw = load_win(l, C_PZ)
        for g in range(4):
            ps, Bp = proj_fm(wv, Bw, g)
            E("act", lambda e, g=g, ps=ps: e.activation(out=zs[:, g, 0:T], in_=ps[:], func=AF.Silu), [Bp], [Bzs])
            ps2, Bp2 = nextps()
            E("pe", lambda e, g=g, ps2=ps2: e.matmul(ps2[:], poolw[:, g, :], pooledT[:, g, :], start=True, stop=True), [Bpoolw, BpooledT], [Bp2])
            E("dve", lambda e, g=g, ps2=ps2: e.scalar_tensor_tensor(out=yb[1][0][:, g, :], in0=ps2[:], scalar=psc[:, g:g + 1], in1=zs[:, g, 0:T], op0=ALU.mult, op1=ALU.mult),
              [Bp2, Bpsc, Bzs], [yb[1][1]])

    def branch_mlstm(l):
        cq, Bcq = SC[0]; ca, Bca = SC[1]
        for qi, (c0, dst, Bd) in enumerate(((C_Q, qT, BqT), (C_K, kT, BkT))):
            wv, Bw = load_win(l, c0)
            E("dve", lambda e, qi=qi: e.tensor_copy(out=cq[:, :, 0:3], in_=mlhalo[:, qi * 4:(qi + 1) * 4, :]), [Bmlhalo], [Bcq])
            for cb in range(4):
                ps, Bp = proj_fm(wv, Bw, cb)
                E("act", lambda e, cb=cb, ps=ps: e.copy(out=cq[:, cb, 3:T + 3], in_=ps[:]), [Bp], [Bcq])
            E("dve", lambda e, qi=qi: e.tensor_copy(out=mlhalo[:, qi * 4:(qi + 1) * 4, :], in_=cq[:, :, T:T + 3]), [Bcq], [Bmlhalo])
            for cb in range(4):
                ch = qi * 4 + cb
                E("dve", lambda e, cb=cb, ch=ch: e.tensor_scalar(out=ca[:, cb, 0:T], in0=cq[:, cb, 3:T + 3], scalar1=mcw[:, 3, ch:ch + 1], scalar2=None, op0=ALU.mult), [Bcq, Bmcw], [Bca])
                for k in (2, 1, 0):
                    E("dve", lambda e, cb=cb, ch=ch, k=k: e.scalar_tensor_tensor(out=ca[:, cb, 0:T], in0=cq[:, cb, k:k + T], scalar=mcw[:, k, ch:ch + 1], in1=ca[:, cb, 0:T], op0=ALU.mult, op1=ALU.add),
                      [Bcq, Bmcw, Bca], [Bca])
                E("act", lambda e, cb=cb, dst=dst: e.activation(out=dst[:, cb, :], in_=ca[:, cb, 0:T], func=AF.Silu), [Bca], [Bd])
        wv, Bw = load_win(l, C_V)
        for tb in range(4):
            ps, Bp = proj_tm(wv, Bw, tb, 0, 512)
            E("dve", lambda e, tb=tb, ps=ps: e.tensor_copy(out=vtok[:, tb, :, 0:128], in_=ps[:].rearrange("p (h d) -> p h d", h=4)), [Bp], [Bvtok])
        wv, Bw = load_win(l, C_O)
        for tb in range(4):
            ps, Bp = proj_tm(wv, Bw, tb, 0, 512)
            E("act", lambda e, tb=tb, ps=ps: e.activation(out=ogt[:, tb, :], in_=ps[:], func=AF.Sigmoid), [Bp], [Bogt])
        wv, Bw = load_win(l, C_IF, 520)
        for tb in range(4):
            ps, Bp = proj_tm(wv, Bw, tb, 8, 512)
            E("act", lambda e, tb=tb, ps=ps: e.activation(out=zwt[:, tb, :], in_=ps[:], func=AF.Silu), [Bp], [Bzwt])
            mul("dve", zwt[:, tb, :], zwt[:, tb, :], mnw[:], [Bzwt, Bmnw], [Bzwt])
            psg, Bpg = proj_tm(wv, Bw, tb, 0, 8)
            G = gt[:, tb, :]
            tt("dve", G[:, 0:4], psg[:, 0:4], mbi[:], ALU.add, [Bpg, Bmbi], [Bgt])
            tt("dve", G[:, 4:8], psg[:, 4:8], mbf[:], ALU.add, [Bpg, Bmbf], [Bgt])
            E("act", lambda e, G=G: e.activation(out=G[:, 4:8], in_=G[:, 4:8], func=AF.Exp, scale=-1.0), [Bgt], [Bgt])
            E("act", lambda e, G=G: e.activation(out=G[:, 4:8], in_=G[:, 4:8], func=AF.Ln, bias=1.0), [Bgt], [Bgt])
            ps2, Bp2 = nextps()
            E("pe", lambda e, G=G, ps2=ps2: e.matmul(ps2[:, 0:4], trif[:], G[:, 4:8], start=True, stop=True), [Btrif, Bgt], [Bp2])
            E("pe", lambda e, G=G, ps2=ps2: e.matmul(ps2[:, 4:8], onesf[:], G[:, 4:8], start=True, stop=True), [Bonesf, Bgt], [Bp2])
            E("act", lambda e, G=G, ps2=ps2: e.activation(out=G[:, 8:12], in_=ps2[:, 0:4], func=AF.Exp, scale=-1.0, bias=math.log(128 ** -0.5)), [Bp2], [Bgt])
            tt("dve", G[:, 24:28], G[:, 0:4], ps2[:, 0:4], ALU.add, [Bgt, Bp2], [Bgt])
            E("act", lambda e, G=G: e.activation(out=G[:, 12:16], in_=G[:, 24:28], func=AF.Exp), [Bgt], [Bgt])
            tt("dve", G[:, 24:28], G[:, 24:28], ps2[:, 4:8], ALU.subtract, [Bgt, Bp2], [Bgt])
            E("act", lambda e, G=G: e.activation(out=G[:, 16:20], in_=G[:, 24:28], func=AF.Exp), [Bgt], [Bgt])
            E("act", lambda e, G=G, ps2=ps2: e.activation(out=G[:, 20:24], in_=ps2[:, 4:8], func=AF.Exp, scale=-1.0), [Bp2], [Bgt])
        pbt = None
        for tb in range(4):
            G = gt[:, tb, :]
            sl = slice(tb * 128, (tb + 1) * 128)
            for hd in range(4):
                pk, Bpk = nextps()
                pkb = pk[:].bitcast(BF16)
                E("pe", lambda e, hd=hd, pkb=pkb: e.transpose(pkb[:, 0:128], kT[:, hd, sl], identb[:]), [BkT, Bidb], [Bpk])
                E("dve", lambda e, hd=hd, pkb=pkb, G=G: e.tensor_scalar(out=ktil[:], in0=pkb[:, 0:128], scalar1=G[:, 16 + hd:17 + hd], scalar2=None, op0=ALU.mult), [Bpk, Bgt], [Bktil])
                pS, BpS = nextps()
                E("pe", lambda e, hd=hd, pS=pS: e.matmul(pS[:, 0:128], kT[:, hd, sl], qT[:, hd, sl], start=True, stop=True), [BkT, BqT], [BpS])
                E("dve", lambda e, hd=hd, pS=pS, G=G: e.scalar_tensor_tensor(out=PT[:], in0=pS[:, 0:128], scalar=G[:, 12 + hd:13 + hd], in1=trif[:], op0=ALU.mult, op1=ALU.mult), [BpS, Bgt, Btrif], [BPT])
                pN, BpN = nextps()
                E("pe", lambda e, hd=hd, pN=pN: e.matmul(pN[:, 0:129], PT[:], vtok[:, tb, hd, :], start=True, stop=False), [BPT, Bvtok], [BpN])
                E("pe", lambda e, hd=hd, pN=pN: e.matmul(pN[:, 0:129], qT[:, hd, sl], Cbf[:, hd, :], start=False, stop=True), [BqT, BCbf], [BpN])
                pC, BpC = nextps()
                E("pe", lambda e, hd=hd, pC=pC: e.matmul(pC[:, 0:129], ktil[:], vtok[:, tb, hd, :], start=True, stop=True), [Bktil, Bvtok], [BpC])
                E("dve", lambda e, hd=hd, pC=pC, G=G: e.scalar_tensor_tensor(out=C32[:, hd, :], in0=C32[:, hd, :], scalar=G[:, 20 + hd:21 + hd], in1=pC[:, 0:129], op0=ALU.mult, op1=ALU.add), [BC32, Bgt, BpC], [BC32])
                E("dve", lambda e, hd=hd: e.tensor_copy(out=Cbf[:, hd, :], in_=C32[:, hd, :]), [BC32], [BCbf])
                E("act", lambda e, hd=hd, pN=pN, G=G: e.activation(out=sm[:, 0:1], in_=pN[:, 128:129], func=AF.Abs, scale=G[:, 8 + hd:9 + hd]), [BpN, Bgt], [Bsm])
                E("dve", lambda e: e.tensor_scalar(out=sm[:, 0:1], in0=sm[:, 0:1], scalar1=1.0, scalar2=None, op0=ALU.max), [Bsm], [Bsm])
                E("dve", lambda e: e.reciprocal(out=sm[:, 1:2], in_=sm[:, 0:1]), [Bsm], [Bsm])
                mul("dve", sm[:, 2:3], sm[:, 1:2], G[:, 8 + hd:9 + hd], [Bsm, Bgt], [Bsm])
                E("dve", lambda e, hd=hd, pN=pN: e.scalar_tensor_tensor(out=hh[:, hd, :], in0=pN[:, 0:128], scalar=sm[:, 2:3], in1=ogt[:, tb, hd * 128:(hd + 1) * 128], op0=ALU.mult, op1=ALU.mult), [BpN, Bsm, Bogt], [Bhh])
                E("act", lambda e, hd=hd: e.activation(out=junk[:, 0:128], in_=hh[:, hd, :], func=AF.Square, accum_out=sm[:, 4 + hd:5 + hd]), [Bhh], [Bjunk, Bsm])
            E("dve", lambda e: e.tensor_scalar(out=sm[:, 8:12], in0=sm[:, 4:8], scalar1=1.0 / 128, scalar2=EPS, op0=ALU.mult, op1=ALU.add), [Bsm], [Bsm])
            E("act", lambda e: e.activation(out=sm[:, 8:12], in_=sm[:, 8:12], func=AF.Sqrt), [Bsm], [Bsm])
            E("dve", lambda e: e.reciprocal(out=sm[:, 12:16], in_=sm[:, 8:12]), [Bsm], [Bsm])
            for hd in range(4):
                E("dve", lambda e, hd=hd: e.scalar_tensor_tensor(out=ydtok[:, hd * 128:(hd + 1) * 128], in0=hh[:, hd, :], scalar=sm[:, 12 + hd:13 + hd], in1=zwt[:, tb, hd * 128:(hd + 1) * 128], op0=ALU.mult, op1=ALU.mult),
                  [Bhh, Bsm, Bzwt], [Bydtok])
            pt, Bpt = nextps()
            ptb = pt[:].bitcast(BF16)
            for cb in range(4):
                E("pe", lambda e, cb=cb, ptb=ptb: e.transpose(ptb[:, cb * 128:(cb + 1) * 128], ydtok[:, cb * 128:(cb + 1) * 128], identb[:]), [Bydtok, Bidb], [Bpt])
            E("act", lambda e, ptb=ptb: e.copy(out=yb[3][0][:, :, sl], in_=ptb[:, 0:512].rearrange("p (c t) -> p c t", c=4)), [Bpt], [yb[3][1]])

    def branch_s5(l):
        zs, Bzs = SC[3]; ya, Bya = SC[1]; yg, Byg = SC[2]
        wv, Bw = load_win(l, C_S5U)
        for cb in range(4):
            ps, Bp = proj_fm(wv, Bw, cb)
            E("dve", lambda e, cb=cb, ps=ps: e.tensor_copy(out=uT[:, cb, :], in_=ps[:]), [Bp], [BuT])
        uS = uT[:].rearrange("p g (c s) -> p g s c", s=8)
        Er, Ei = Etab[0][0], Etab[1][0]
        BE = [Etab[0][1], Etab[1][1]]
        Xps = {}
        for c in range(2):
            for hf in range(2):
                Xps[(c, hf)] = nextps()
        for gp in range(16):
            gq, j = divmod(gp, 4)
            for c in range(2):
                ps, Bp = Xps[(c, gp // 8)]
                pv = ps[:].rearrange("p (g c) -> p g c", g=8)
                for s in range(8):
                    E("pe", lambda e, pv=pv, gp=gp, gq=gq, j=j, c=c, s=s: e.matmul(pv[:, gp % 8, :], Min[32 * j:32 * j + 32, gq, s, c, :], uS[32 * j:32 * j + 32, gq, s, :],
                                                                        start=(s == 0), stop=(s == 7), tile_position=(32 * j, 0)), [BMin, BuT], [Bp])
        for hf in range(2):
            gs = slice(hf * 8, hf * 8 + 8)
            xr, Bxr = Xps[(0, hf)]; xi, Bxi = Xps[(1, hf)]
            xrv = xr[:].rearrange("p (g c) -> p g c", g=8); xiv = xi[:].rearrange("p (g c) -> p g c", g=8)
            t1, Bt1 = s5t[0]; t2, Bt2 = s5t[1]
            mul("dve", t1[:, gs, :], xrv, Er[:, gs, :], [Bxr, BE[0]], [Bt1]); mul("dve", t2[:, gs, :], xiv, Ei[:, gs, :], [Bxi, BE[1]], [Bt2])
            tt("dve", Xr[0][0][:, gs, :], t1[:, gs, :], t2[:, gs, :], ALU.subtract, [Bt1, Bt2], [Xr[0][1]])
            mul("dve", t1[:, gs, :], xiv, Er[:, gs, :], [Bxi, BE[0]], [Bt1]); mul("dve", t2[:, gs, :], xrv, Ei[:, gs, :], [Bxr, BE[1]], [Bt2])
            tt("dve", Xr[1][0][:, gs, :], t1[:, gs, :], t2[:, gs, :], ALU.add, [Bt1, Bt2], [Xr[1][1]])
        for c in range(2):
            for gp in range(16):
                E("dve", lambda e, c=c, gp=gp: e.tensor_tensor_scan(out=Hs[c][0][:, gp, :], data0=rho[:, gp:gp + 1].to_broadcast([128, 64]), data1=Xr[c][0][:, gp, :], initial=car[c][0][:, gp:gp + 1], op0=ALU.mult, op1=ALU.add),
                  [Brho, Xr[c][1], car[c][1]], [Hs[c][1]])
        t1, Bt1 = s5t[0]; t2, Bt2 = s5t[1]
        mul("dve", t1[:], Er[:], Hs[0][0][:], [BE[0], Hs[0][1]], [Bt1]); mul("dve", t2[:], Ei[:], Hs[1][0][:], [BE[1], Hs[1][1]], [Bt2])
        tt("dve", Hu[0][0][:], t1[:], t2[:], ALU.add, [Bt1, Bt2], [Hu[0][1]])
        mul("dve", t1[:], Er[:], Hs[1][0][:], [BE[0], Hs[1][1]], [Bt1]); mul("dve", t2[:], Ei[:], Hs[0][0][:], [BE[1], Hs[0][1]], [Bt2])
        tt("dve", Hu[1][0][:], t1[:], t2[:], ALU.subtract, [Bt1, Bt2], [Hu[1][1]])
        for c in range(2):
            E("dve", lambda e, c=c: e.tensor_copy(out=Hp[c][0][:, :, 0], in_=car[c][0][:]), [car[c][1]], [Hp[c][1]])
            E("dve", lambda e, c=c: e.tensor_copy(out=Hp[c][0][:, :, 1:64], in_=Hu[c][0][:, :, 0:63]), [Hu[c][1]], [Hp[c][1]])
            E("dve", lambda e, c=c: e.tensor_copy(out=car[c][0][:], in_=Hu[c][0][:, :, 63]), [Hu[c][1]], [car[c][1]])
        for gq in range(4):
            ps, Bp = nextps()
            pv = ps[:].rearrange("p (t c) -> p t c", t=8)
            for t in range(8):
                for s in range(t + 1):
                    E("pe", lambda e, pv=pv, gq=gq, t=t, s=s: e.matmul(pv[:, t, :], Ktoe[:, gq, t - s, :], uS[:, gq, s, :], start=(s == 0), stop=False), [BKtoe, BuT], [Bp])
                for j in range(4):
                    gp = 4 * gq + j
                    for c in range(2):
                        E("pe", lambda e, pv=pv, gp=gp, j=j, t=t, c=c: e.matmul(pv[32 * j:32 * j + 32, t, :], Mout[:, gp, t, c, :], Hp[c][0][:, gp, :], start=False, stop=(c == 1), tile_position=(0, 32 * j)),
                          [BMout, Hp[c][1]], [Bp])
            E("dve", lambda e, gq=gq, pv=pv: e.scalar_tensor_tensor(out=ya[:, gq, 0:T].rearrange("p (c t) -> p c t", t=8), in0=u32[:, gq, :].rearrange("p (c t) -> p c t", t=8), scalar=s5dd[:, gq:gq + 1],
                                                                    in1=pv.rearrange("p t c -> p c t"), op0=ALU.mult, op1=ALU.add), [Bu32, Bs5dd, Bp], [Bya])
        t1, Bt1 = SC[0]
        for cb in range(4):
            Y = ya[:, cb, 0:T]
            mul("dve", t1[:, cb, 0:T], Y, Y, [Bya], [Bt1])
            E("dve", lambda e, cb=cb: e.tensor_scalar(out=t1[:, cb, 0:T], in0=t1[:, cb, 0:T], scalar1=0.044715, scalar2=1.0, op0=ALU.mult, op1=ALU.add), [Bt1], [Bt1])
            mul("dve", t1[:, cb, 0:T], t1[:, cb, 0:T], Y, [Bt1, Bya], [Bt1])
            E("act", lambda e, cb=cb: e.activation(out=t1[:, cb, 0:T], in_=t1[:, cb, 0:T], func=AF.Sigmoid, scale=2.0 * math.sqrt(2.0 / math.pi)), [Bt1], [Bt1])
            mul("dve", yg[:, cb, 0:T], t1[:, cb, 0:T], Y, [Bt1, Bya], [Byg])
            E("dve", lambda e, cb=cb: e.tensor_copy(out=uT[:, cb, :], in_=yg[:, cb, 0:T]), [Byg], [BuT])
        gv, Bs = load_img(l, ("glu",))
        wv, Bw = load_win(l, C_S5Z)
        for cb in range(4):
            ps, Bp = nextps()
            for kc in range(4):
                E("pe", lambda e, cb=cb, kc=kc, ps=ps: e.matmul(ps[:], gv[:, kc, cb * 128:(cb + 1) * 128], uT[:, kc, :], start=(kc == 0), stop=(kc == 3)), [Bs, BuT], [Bp])
            E("act", lambda e, cb=cb, ps=ps: e.activation(out=t1[:, cb, 0:T], in_=ps[:], func=AF.Sigmoid), [Bp], [Bt1])
            mul("dve", yg[:, cb, 0:T], yg[:, cb, 0:T], t1[:, cb, 0:T], [Byg, Bt1], [Byg])
            ps, Bp = proj_fm(wv, Bw, cb)
            E("act", lambda e, cb=cb, ps=ps: e.activation(out=zs[:, cb, 0:T], in_=ps[:], func=AF.Silu), [Bp], [Bzs])
            mul("dve", yb[0][0][:, cb, :], yg[:, cb, 0:T], zs[:, cb, 0:T], [Byg, Bzs], [yb[0][1]])

    def tile_layer(l, s, t, xb, load_x, store_x):
        xtile, Bx = xt[xb]
        if L == 1:
            src_d, dst_d, skey, dkey = x_d, y_d, ("x", s, t), ("y", s, t)
        else:
            src_d = x_d if l == 0 else scr_d[(l - 1) % 2]
            dst_d = y_d if l == L - 1 else scr_d[l % 2]
            skey = ("x", s, t) if l == 0 else ((l - 1) % 2, s, t)
            dkey = ("y", s, t) if l == L - 1 else (l % 2, s, t)
        if load_x:
            kb.dma("sp", xds[xb], lambda q: q.dma_start(out=xtile[:], in_=src_d[s, t * T:(t + 1) * T, :].rearrange("(tb p) d -> p tb d", p=128)), reads=[dbuf(skey)], writes=[Bx])
        for tb in range(4):
            E("act", lambda e, tb=tb: e.activation(out=junk[:], in_=xtile[:, tb, :], func=AF.Square, accum_out=st4[:, tb:tb + 1]), [Bx], [Bjunk, Bst4])
        E("dve", lambda e: e.tensor_scalar(out=st4[:, 4:8], in0=st4[:, 0:4], scalar1=1.0 / D, scalar2=EPS, op0=ALU.mult, op1=ALU.add), [Bst4], [Bst4])
        E("act", lambda e: e.activation(out=st4[:, 4:8], in_=st4[:, 4:8], func=AF.Sqrt), [Bst4], [Bst4])
        E("dve", lambda e: e.reciprocal(out=st4[:, 8:12], in_=st4[:, 4:8]), [Bst4], [Bst4])
        for tb in range(4):
            E("dve", lambda e, tb=tb: e.scalar_tensor_tensor(out=htok[:], in0=xtile[:, tb, :], scalar=st4[:, 8 + tb:9 + tb], in1=npw[:], op0=ALU.mult, op1=ALU.mult), [Bx, Bst4, Bnpw], [Bhtok])
            pt, Bpt = nextps()
            ptb = pt[:].bitcast(BF16)
            for kc in range(8):
                E("pe", lambda e, kc=kc, ptb=ptb: e.transpose(ptb[:, kc * 128:(kc + 1) * 128], htok[:, kc * 128:(kc + 1) * 128], identb[:]), [Bhtok, Bidb], [Bpt])
            E("act", lambda e, tb=tb, ptb=ptb: e.copy(out=hT[:, :, tb * 128:(tb + 1) * 128], in_=ptb.rearrange("p (k c) -> p k c", k=8)), [Bpt], [BhT])
        for b in range(4):
            if not en[b]:
                E("dve", lambda e, b=b: e.memset(yb[b][0][:], 0.0), w=[yb[b][1]])
        if en[2]:
            branch_sconv(l)
        if en[1]:
            branch_pool(l, t == 0)
        if en[3]:
            branch_mlstm(l)
        if en[0]:
            branch_s5(l)
        if dbg and t == NT - 1 and s == 0 and l == L - 1:
            for b in range(4):
                dt_, Bdt = SC[0]
                E("act", lambda e, b=b: e.copy(out=dt_[:, :, 0:T], in_=yb[b][0][:]), [yb[b][1]], [Bdt])
                kb.dma("sp", ods, lambda q, b=b: q.dma_start(out=dbg_d[b], in_=dt_[:, :, 0:T]), reads=[Bdt])
        for b in range(4):
            for hf in range(2):
                wbv, Bs = load_img(l, ("wbr", b, hf))
                gvw, Bg = load_win(l, C_G + b * 1024 + hf * 512)
                for jj in range(4):
                    j = hf * 4 + jj
                    ps, Bp = proj_fm(gvw, Bg, jj)
                    gs_, Bgs = gsb[0]
                    E("act", lambda e, ps=ps, gs_=gs_: e.activation(out=gs_[:], in_=ps[:], func=AF.Sigmoid), [Bp], [Bgs])
                    ps2, Bp2 = nextps()
                    for cc in range(4):
                        E("pe", lambda e, cc=cc, jj=jj, ps2=ps2, b=b: e.matmul(ps2[:], wbv[:, cc, jj * 128:(jj + 1) * 128], yb[b][0][:, cc, :], start=(cc == 0), stop=(cc == 3)), [Bs, yb[b][1]], [Bp2])
                    if b == 0:
                        mul("dve", SC[j // 4][0][:, j % 4, 0:T], ps2[:], gs_[:], [Bp2, Bgs], [SC[j // 4][1]])
                    else:
                        tm_, Btm = tmpm[0]
                        mul("dve", tm_[:], ps2[:], gs_[:], [Bp2, Bgs], [Btm])
                        if b < 3:
                            tt("dve", SC[j // 4][0][:, j % 4, 0:T], SC[j // 4][0][:, j % 4, 0:T], tm_[:], ALU.add, [SC[j // 4][1], Btm], [SC[j // 4][1]])
                        else:
                            tt("dve", mrgT[:, j, :], SC[j // 4][0][:, j % 4, 0:T], tm_[:], ALU.add, [SC[j // 4][1], Btm], [BmrgT])
        wo = []
        for hf in range(2):
            wo.append(load_img(l, ("wo", hf)))
        for tb in range(4):
            pss = []
            for hf in range(2):
                ps, Bp = nextps()
                for j in range(8):
                    E("pe", lambda e, j=j, ps=ps, hf=hf: e.matmul(ps[:], mrgT[:, j, tb * 128:(tb + 1) * 128], wo[hf][0][:, j, :], start=(j == 0), stop=(j == 7)), [BmrgT, wo[hf][1]], [Bp])
                E("act", lambda e, ps=ps, hf=hf: e.activation(out=junk[:, 0:512], in_=ps[:], func=AF.Square, accum_out=st4[:, 12 + hf:13 + hf]), [Bp], [Bjunk, Bst4])
                pss.append((ps, Bp))
            tt("dve", st4[:, 14:15], st4[:, 12:13], st4[:, 13:14], ALU.add, [Bst4], [Bst4])
            E("dve", lambda e: e.tensor_scalar(out=st4[:, 14:15], in0=st4[:, 14:15], scalar1=1.0 / D, scalar2=EPS, op0=ALU.mult, op1=ALU.add), [Bst4], [Bst4])
            E("act", lambda e: e.activation(out=st4[:, 14:15], in_=st4[:, 14:15], func=AF.Sqrt), [Bst4], [Bst4])
            E("dve", lambda e: e.reciprocal(out=st4[:, 15:16], in_=st4[:, 14:15]), [Bst4], [Bst4])
            for hf in range(2):
                ps, Bp = pss[hf]
                E("dve", lambda e, ps=ps, hf=hf: e.scalar_tensor_tensor(out=junk[:, hf * 512:(hf + 1) * 512], in0=ps[:], scalar=st4[:, 15:16], in1=ppw[:, hf * 512:(hf + 1) * 512], op0=ALU.mult, op1=ALU.mult),
                  [Bp, Bst4, Bppw], [Bjunk])
            tt("dve", xtile[:, tb, :], xtile[:, tb, :], junk[:], ALU.add, [Bx, Bjunk], [Bx])
        if store_x:
            return kb.dma("sp", ods, lambda q: q.dma_start(out=dst_d[s, t * T:(t + 1) * T, :].rearrange("(tb p) d -> p tb d", p=128), in_=xtile[:]), reads=[Bx], writes=[dbuf(dkey)])
        return None

    last = None
    convert_layer(0)
    for l in range(L):
        if l > 0:
            kb.new_epoch("_%d" % l)
        layer_prep(l)
        if l + 1 < L:
            convert_layer(l + 1)
        for s in range(NSEQ):
            seq_reset()
            for t in range(NT):
                last = tile_layer(l, s, t, 0, True, True)
    kb._wait("sp", [Tok(ods.sem, ods.cnt)])
    es.close()
    return nc, kb


_CACHE = {}
WNAMES = ["norm_pre_w", "norm_post_w", "w_in", "s5_A_re", "s5_A_im", "s5_log_dt", "s5_B_re", "s5_B_im", "s5_C_re", "s5_C_im",
          "s5_D", "s5_w_glu", "pool_w", "pool_scale", "sconv_w", "mlstm_conv_w", "mlstm_b_i", "mlstm_b_f", "mlstm_norm_w",
          "w_branch", "w_out"]


def run_layers(x, weights, ncores, en=(1, 1, 1, 1), dbg=False):
    B, S, _ = x.shape
    nseq = B // ncores
    NT = S // T
    Lw = weights["w_in"].shape[0]
    key = (nseq, NT, en, dbg)
    consts = host_consts()
    cur = np.ascontiguousarray(x, dtype=np.float32)
    dbg_out = None
    for l in range(Lw):
        nc, _ = build(1, nseq, NT, en, dbg)
        in_maps = []
        for c in range(ncores):
            m = {"x": cur[c * nseq:(c + 1) * nseq]}
            for k in WNAMES:
                m[k] = np.ascontiguousarray(weights[k][l:l + 1], dtype=np.float32)
            m.update(consts)
            in_maps.append(m)
        res = run_bass_kernel_spmd(nc, in_maps, core_ids=list(range(ncores)))
        cur = np.concatenate([r["y"] for r in res.results], axis=0)
        if dbg:
            dbg_out = res.results[0]["dbg"]
    return (cur, dbg_out) if dbg else cur


def run_fused(x, weights, ncores, dbg=False):
    B, S, _ = x.shape
    nseq = B // ncores
    NT = S // T
    Lw = weights["w_in"].shape[0]
    consts = host_consts()
    nc, _ = build(Lw, nseq, NT, (1, 1, 1, 1), dbg)
    xs = np.ascontiguousarray(x, dtype=np.float32)
    wts = {k: np.ascontiguousarray(weights[k], dtype=np.float32) for k in WNAMES}
    in_maps = []
    for c in range(ncores):
        m = {"x": xs[c * nseq:(c + 1) * nseq]}
        m.update(wts)
        m.update(consts)
        in_maps.append(m)
    res = run_bass_kernel_spmd(nc, in_maps, core_ids=list(range(ncores)))
    return np.concatenate([r["y"] for r in res.results], axis=0)


def kernel(**inputs):
    x = inputs["x"]
    weights = {k: inputs[k] for k in WNAMES}
    return run_fused(x, weights, 8).astype(np.float32)
```

```python
import math
import contextlib
import numpy as np
import concourse.bass as bass
import concourse.mybir as mybir
from concourse.bass_utils import run_bass_kernel_spmd

F32 = mybir.dt.float32
BF16 = mybir.dt.bfloat16
I32 = mybir.dt.int32
AF = mybir.ActivationFunctionType
ALU = mybir.AluOpType

SAME_SYNC = True
import os as _os
PIPE = int(_os.environ.get('KPIPE', '1'))
D = 1024
W = 512
NIN = 10760
T = 512
EPS = 1e-6
C_S5U, C_S5Z, C_PU, C_PZ, C_SX, C_SB, C_SC, C_SZ = 0, 512, 1024, 1536, 2048, 2560, 3072, 3584
C_Q, C_K, C_V, C_O, C_IF, C_MZ, C_G = 4096, 4608, 5120, 5632, 6144, 6152, 6664
POOL_WINDOWS = (2, 4, 8, 16)


class Tok:
    __slots__ = ("sem", "val")

    def __init__(self, sem, val):
        self.sem = sem
        self.val = val


class Buf:
    def __init__(self, name=""):
        self.name = name
        self.w = None
        self.r = {}


class DSem:
    def __init__(self, sem):
        self.sem = sem
        self.cnt = 0


class KB:
    def __init__(self, nc, es):
        self.nc = nc
        self.es = es
        self.eng = {"pe": nc.tensor, "act": nc.scalar, "dve": nc.vector, "pool": nc.gpsimd, "sp": nc.sync}
        self.sem = {e: es.enter_context(nc.semaphore("s_" + e)) for e in self.eng}
        self.cnt = {e: 0 for e in self.eng}
        self.seen = {e: {} for e in self.eng}
        self.nins = 0
        self.uid = 0

    def dsem(self, name):
        return DSem(self.es.enter_context(self.nc.semaphore(name)))

    def new_epoch(self, tag):
        self._keep = getattr(self, '_keep', []) + [self.sem]
        self.sem = {e: self.es.enter_context(self.nc.semaphore("s_" + e + tag)) for e in self.eng}
        self.cnt = {e: 0 for e in self.eng}

    def sb(self, name, shape, dt):
        return self.es.enter_context(self.nc.sbuf_tensor(name, list(shape), dt))

    def _wait(self, e, toks):
        need = {}
        for t in toks:
            if t is None:
                continue
            k = id(t.sem)
            if k not in need or need[k].val < t.val:
                need[k] = t
        for k, t in need.items():
            if t.sem is self.sem[e] and (e == "pe" or not SAME_SYNC):
                continue
            if self.seen[e].get(k, 0) >= t.val:
                continue
            self.eng[e].wait_ge(t.sem, t.val)
            self.seen[e][k] = t.val

    def _deps(self, reads, writes):
        toks = []
        for b in reads:
            toks.append(b.w)
        for b in writes:
            toks.append(b.w)
            toks.extend(b.r.values())
        return toks

    def _upd(self, tok, reads, writes):
        k = id(tok.sem)
        for b in reads:
            b.r[k] = tok
        for b in writes:
            b.w = tok
            b.r = {}

    def op(self, e, fn, reads=(), writes=()):
        self._wait(e, self._deps(reads, writes))
        ins = fn(self.eng[e])
        self.cnt[e] += 1
        ins.then_inc(self.sem[e], 1)
        tok = Tok(self.sem[e], self.cnt[e])
        self._upd(tok, reads, writes)
        self.nins += 1
        return tok

    def dma(self, e, ds, fn, reads=(), writes=()):
        self._wait(e, self._deps(reads, writes))
        ins = fn(self.eng[e])
        ds.cnt += 16
        ins.then_inc(ds.sem, 16)
        tok = Tok(ds.sem, ds.cnt)
        self._upd(tok, reads, writes)
        self.nins += 1
        return tok


def host_consts():
    ident = np.eye(128, dtype=np.float32)
    m = np.arange(128)[:, None]
    l = np.arange(128)[None, :]
    tri = (m <= l).astype(np.float32)
    ones = np.ones((128, 128), np.float32)
    band = np.zeros((3, 4, 128, 128), np.float32)
    for g, win in enumerate(POOL_WINDOWS):
        dlt = l - m
        inw = (dlt >= 0) & (dlt < win)
        band[0, g] = inw / win - ident
        dp = l + 128 - m
        band[1, g] = ((dp >= 0) & (dp < win)) / win
        band[2, g] = inw / np.minimum(l + 1, win) - ident
    return {"c_ident": ident, "c_tri": tri, "c_ones": ones,
            "c_band": np.ascontiguousarray(band.transpose(2, 0, 1, 3)).reshape(128, 12 * 128)}


def build(L, NSEQ, NT, en=(1, 1, 1, 1), dbg=False):
    S = NT * T
    nc = bass.Bass("TRN2", target_bir_lowering=False)
    es = contextlib.ExitStack()
    kb = KB(nc, es)

    def din(name, shape):
        return nc.dram_tensor(name, list(shape), F32, kind="ExternalInput").ap()

    x_d = din("x", [NSEQ, S, D])
    npw_d = din("norm_pre_w", [L, D]); ppw_d = din("norm_post_w", [L, D])
    win_d = din("w_in", [L, D, NIN])
    are_d = din("s5_A_re", [L, 32, 64]); aim_d = din("s5_A_im", [L, 32, 64]); ldt_d = din("s5_log_dt", [L, 32])
    bre_d = din("s5_B_re", [L, 32, 64, 16]); bim_d = din("s5_B_im", [L, 32, 64, 16])
    cre_d = din("s5_C_re", [L, 32, 16, 64]); cim_d = din("s5_C_im", [L, 32, 16, 64])
    s5d_d = din("s5_D", [L, W]); glu_d = din("s5_w_glu", [L, W, W])
    pw_d = din("pool_w", [L, 4, 128, 128]); psc_d = din("pool_scale", [L, W])
    scw_d = din("sconv_w", [L, 3, W]); mcw_d = din("mlstm_conv_w", [L, 4, 2 * W])
    mbi_d = din("mlstm_b_i", [L, 4]); mbf_d = din("mlstm_b_f", [L, 4]); mnw_d = din("mlstm_norm_w", [L, W])
    wbr_d = din("w_branch", [L, 4, W, D]); wo_d = din("w_out", [L, D, D])
    cid_d = din("c_ident", [128, 128]); ctri_d = din("c_tri", [128, 128]); cone_d = din("c_ones", [128, 128])
    cband_d = din("c_band", [128, 12 * 128])
    y_d = nc.dram_tensor("y", [NSEQ, S, D], F32, kind="ExternalOutput").ap()
    dbg_d = nc.dram_tensor("dbg", [4, 128, 4, T], F32, kind="ExternalOutput").ap() if dbg else None
    scr_d = [nc.dram_tensor("xscr%d" % i, [NSEQ, S, D], F32, kind="Internal").ap() for i in range(2)] if L > 1 else []
    dram_bufs = {}
    NIMG = 55
    SLW = 8 * 264
    wimg_d = [nc.dram_tensor("wimg%d" % l_, [NIMG, 128, SLW], BF16, kind="Internal").ap() for l_ in range(L)]
    Bimg = [Buf("wimg") for _ in range(L)]
    cvds = [None] * L
    img_index = {}

    def img_specs(l):
        sp = []

        def wv_(c0, n):
            return win_d[l, :, c0:c0 + n].rearrange("(k p) c -> p k c", p=128)
        for c0 in (C_S5U, C_S5Z, C_PU, C_PZ, C_SX, C_SB, C_SC, C_SZ, C_Q, C_K, C_V, C_O) + tuple(C_G + i * 512 for i in range(8)):
            for h in range(2):
                sp.append((("win", c0, h), 8, 256, wv_(c0 + 256 * h, 256)))
        sp.append((("win", C_IF, 0), 8, 264, wv_(C_IF, 264)))
        sp.append((("win", C_IF, 1), 8, 256, wv_(C_IF + 264, 256)))
        sp.append((("glu",), 4, 512, glu_d[l].rearrange("(k p) c -> p k c", p=128)))
        for b in range(4):
            for hf in range(2):
                sp.append((("wbr", b, hf), 4, 512, wbr_d[l, b, :, hf * 512:(hf + 1) * 512].rearrange("(k p) c -> p k c", p=128)))
        for hf in range(2):
            for q2 in range(2):
                c0 = hf * 512 + q2 * 256
                sp.append((("wo", hf, q2), 8, 256, wo_d[l, :, c0:c0 + 256].rearrange("(k p) c -> p k c", p=128)))
        return sp

    def convert_layer(l):
        cvds[l] = kb.dsem("cvds%d" % l)
        for i, (key, k, c, src) in enumerate(img_specs(l)):
            img_index[(l,) + key] = (i, k, c)
            for kk in range(k):
                dst = wimg_d[l][i, :, kk * c:(kk + 1) * c]
                kb.dma("pool", cvds[l], lambda q, dst=dst, src=src, kk=kk: q.dma_start(out=dst, in_=src[:, kk, :]), writes=[Bimg[l]])
        Bimg[l].w = Tok(cvds[l].sem, cvds[l].cnt)

    def load_img(l, key):
        i, k, c = img_index[(l,) + key]
        si = wsi[0] % NSL
        wsi[0] += 1
        slot, Bs = wsl[si]
        kb.dma("sp", wds[si], lambda q: q.dma_start(out=slot[:, 0:k * c], in_=wimg_d[l][i, :, 0:k * c]), reads=[Bimg[l]], writes=[Bs])
        return slot[:, 0:k * c].rearrange("p (k c) -> p k c", k=k), Bs

    def dbuf(key):
        if key not in dram_bufs:
            dram_bufs[key] = Buf("dram")
        return dram_bufs[key]

    uid = [0]

    def sbt(shape, dt, name=None):
        uid[0] += 1
        return kb.sb((name or "t") + str(uid[0]), shape, dt), Buf(name or "t")
    sbt_global = sbt

    NPS = 8
    psb = [es.enter_context(nc.psum_tensor("ps%d" % i, [128, 512], F32)) for i in range(NPS)]
    psB = [Buf("ps%d" % i) for i in range(NPS)]
    psi = [0]

    def nextps():
        i = psi[0] % NPS
        psi[0] += 1
        return psb[i], psB[i]

    def E(e, fn, r=(), w=()):
        return kb.op(e, fn, r, w)

    cdsem = kb.dsem("cdsem")

    cbufs = []

    def cbarrier():
        for b_ in cbufs:
            b_.w = Tok(cdsem.sem, cdsem.cnt)
        del cbufs[:]

    def cload(e, out_ap, in_ap, wb, slow=False):
        cbufs.append(wb)
        return kb.dma(e, cdsem, lambda q: q.dma_start(out=out_ap, in_=in_ap, allow_slow_non_contiguous=True) if slow
                      else q.dma_start(out=out_ap, in_=in_ap), writes=[wb])

    identf, Bidf = sbt([128, 128], F32, "identf")
    identb, Bidb = sbt([128, 128], BF16, "identb")
    trif, Btrif = sbt([128, 128], F32, "trif")
    onesf, Bonesf = sbt([128, 128], F32, "onesf")
    band, Bband = sbt([128, 3, 4, 128], BF16, "band")
    cload("sp", identf[:], cid_d, Bidf)
    cload("pool", identb[:], cid_d, Bidb)
    cload("sp", trif[:], ctri_d, Btrif)
    cload("sp", onesf[:], cone_d, Bonesf)
    cload("pool", band[:].rearrange("p a g l -> p (a g l)"), cband_d, Bband)
    cbarrier()

    xt = [sbt([128, 4, D], F32, "xt") for _ in range(1)]
    xds = [kb.dsem("xds%d" % i) for i in range(1)]
    ods = kb.dsem("ods")
    htok, Bhtok = sbt([128, D], BF16, "htok")
    hT, BhT = sbt([128, 8, T], BF16, "hT")
    st4, Bst4 = sbt([128, 16], F32, "st4")
    NSL = 6
    wsl = [sbt([128, SLW], BF16, "wsl") for _ in range(NSL)]
    wds = [kb.dsem("wds%d" % i) for i in range(NSL)]
    wsi = [0]

    def load_w(views):
        i = wsi[0] % NSL
        wsi[0] += 1
        slot, Bs = wsl[i]
        for (off, kk, cc, src) in views:
            dst = slot[:, off:off + kk * cc].rearrange("p (k c) -> p k c", k=kk)
            kb.dma("pool", wds[i], lambda q, dst=dst, src=src: q.dma_start(out=dst, in_=src), writes=[Bs])
        return slot, Bs

    def win_view(l, c0, n):
        return win_d[l, :, c0:c0 + n].rearrange("(k p) c -> p k c", p=128)

    def load_win(l, c0, n=512):
        return [load_img(l, ("win", c0, h)) for h in range(2)], None

    def proj_fm(wg, _, jb):
        wv, Bw = wg[jb // 2]
        jj_ = jb % 2
        ps, Bp = nextps()
        for kc in range(8):
            E("pe", lambda e, kc=kc: e.matmul(ps[:], wv[:, kc, jj_ * 128:(jj_ + 1) * 128], hT[:, kc, :], start=(kc == 0), stop=(kc == 7)),
              [Bw, BhT], [Bp])
        return ps, Bp

    def proj_tm(wg, _, tb, c0, n):
        ps, Bp = nextps()
        if n == 8:
            parts = [(0, 0, 8, 0)]
        elif c0 == 8:
            parts = [(0, 8, 256, 0), (1, 0, 256, 256)]
        else:
            parts = [(0, 0, 256, 0), (1, 0, 256, 256)]
        for (h, wc, nn, pc) in parts:
            wv, Bw = wg[h]
            for kc in range(8):
                E("pe", lambda e, kc=kc, wv=wv: e.matmul(ps[:, pc:pc + nn], hT[:, kc, tb * 128:(tb + 1) * 128], wv[:, kc, wc:wc + nn], start=(kc == 0), stop=(kc == 7)),
                  [Bw, BhT], [Bp])
        return ps, Bp

    yb = [sbt([128, 4, T], BF16, "yb") for _ in range(4)]
    SC = [sbt([128, 4, T + 4], F32, "scr") for _ in range(4)]

    def scflat(i):
        return SC[i][0][:].rearrange("p a b -> p (a b)")

    class _V0:
        def __init__(self, ap):
            self.ap = ap

        def __getitem__(self, k):
            return self.ap[k]
    junk, Bjunk = _V0(scflat(2)[:, 0:D]), SC[2][1]

    def sc_half(i, h):
        return scflat(i)[:, h * 1024:(h + 1) * 1024].rearrange("p (g c) -> p g c", g=16)
    qkT, BqkT = sbt([128, 8, T], BF16, "qkT")
    mrgT, BmrgT = qkT, BqkT
    gsb = [(_V0(SC[3][0][:, 0, 0:T]), SC[3][1])]
    tmpm = [(_V0(SC[3][0][:, 1, 0:T]), SC[3][1])]

    npw, Bnpw = sbt([128, D], F32, "npw"); ppw, Bppw = sbt([128, D], F32, "ppw")
    scw, Bscw = sbt([128, 3, 4], F32, "scw"); mcw, Bmcw = sbt([128, 4, 8], F32, "mcw")
    psc, Bpsc = sbt([128, 4], F32, "psc"); s5dd, Bs5dd = sbt([128, 4], F32, "s5dd")
    mnw, Bmnw = sbt([128, W], F32, "mnw"); mbi, Bmbi = sbt([128, 4], F32, "mbi"); mbf, Bmbf = sbt([128, 4], F32, "mbf")
    poolw, Bpoolw = sbt([128, 4, 128], BF16, "poolw")
    schalo, Bschalo = sbt([128, 4, 2], F32, "schalo")
    putok, Bputok = sbt([128, 5, W], BF16, "putok")
    mlhalo, Bmlhalo = sbt([128, 8, 3], F32, "mlhalo")
    C32, BC32 = sbt([128, 4, 129], F32, "C32"); Cbf, BCbf = sbt([128, 4, 129], BF16, "Cbf")
    car = [sbt([128, 16], F32, "car") for _ in range(2)]
    Min, BMin = sbt([128, 4, 8, 2, 128], BF16, "Min")
    Mout, BMout = sbt([128, 16, 8, 2, 32], BF16, "Mout")
    Ktoe, BKtoe = sbt([128, 4, 8, 128], BF16, "Ktoe")
    Etab = [sbt([128, 16, 64], F32, "Etab") for _ in range(2)]
    rho, Brho = sbt([128, 16], F32, "rho")

    class _V:
        def __init__(self, ap):
            self.ap = ap

        def __getitem__(self, k):
            return self.ap[k]
    Zpad = [(_V(scflat(i)[:, 0:2048].rearrange("p (g c) -> p g c", g=16)), SC[i][1]) for i in range(2)]
    Cpad = [(_V(scflat(i)[:, 0:2048].rearrange("p (g c) -> p g c", g=16)), SC[i][1]) for i in range(2, 4)]
    E("dve", lambda e: e.memset(Mout[:], 0.0), w=[BMout])
    qT, BqT = _V(qkT[:, 0:4, :]), BqkT; kT, BkT = _V(qkT[:, 4:8, :]), BqkT
    vtok, Bvtok = sbt([128, 4, 4, 129], BF16, "vtok")
    E("dve", lambda e: e.memset(vtok[:], 1.0), w=[Bvtok])
    ogt, Bogt = sbt([128, 4, W], BF16, "ogt"); zwt, Bzwt = sbt([128, 4, W], BF16, "zwt")
    gt, Bgt = sbt([128, 4, 40], F32, "gt")
    ktil, Bktil = sbt([128, 128], BF16, "ktil"); PT, BPT = sbt([128, 128], BF16, "PT")
    hh, Bhh = sbt([128, 4, 128], F32, "hh"); ydtok, Bydtok = sbt([128, W], BF16, "ydtok")
    sm, Bsm = sbt([128, 16], F32, "sm")
    uT, BuT = sbt([128, 4, T], BF16, "uT"); u32, Bu32 = uT, BuT
    s5t = [(_V(sc_half(0, h)), SC[0][1]) for h in range(2)]
    Hs = [(_V(sc_half(1, h)), SC[1][1]) for h in range(2)]
    Xr = [(_V(sc_half(2, h)), SC[2][1]) for h in range(2)]
    Hu = Xr
    HpT, BHpT = sbt([128, 2, 16, 64], BF16, "HpT")
    Hp = [(_V(HpT[:, c]), BHpT) for c in range(2)]
    pooledT, BpooledT = _V(HpT[:].rearrange("p a g c -> p (a g c)").rearrange("p (g t) -> p g t", g=4)), BHpT

    def mul(e, o, a, b, r, w):
        return E(e, lambda q: q.tensor_tensor(out=o, in0=a, in1=b, op=ALU.mult), r, w)

    def tt(e, o, a, b, op, r, w):
        return E(e, lambda q: q.tensor_tensor(out=o, in0=a, in1=b, op=op), r, w)

    prep_cache = {}

    def layer_prep(l):
        pidx = [0]

        def sbt(shape, dt, name=None):
            pidx[0] += 1
            k = (name, pidx[0])
            if k not in prep_cache:
                prep_cache[k] = sbt_global(shape, dt, name)
            return prep_cache[k]

        def bc(v):
            return v.partition_broadcast(128)
        cload("sp", npw[:], bc(npw_d[l]), Bnpw); cload("sp", ppw[:], bc(ppw_d[l]), Bppw)
        cload("sp", scw[:], scw_d[l].rearrange("k (cb p) -> p k cb", p=128), Bscw, slow=True)
        cload("sp", mcw[:], mcw_d[l].rearrange("k (cb p) -> p k cb", p=128), Bmcw, slow=True)
        cload("sp", psc[:], psc_d[l].rearrange("(cb p) -> p cb", p=128), Bpsc, slow=True)
        cload("sp", s5dd[:], s5d_d[l].rearrange("(cb p) -> p cb", p=128), Bs5dd, slow=True)
        cload("sp", mnw[:], bc(mnw_d[l]), Bmnw); cload("sp", mbi[:], bc(mbi_d[l]), Bmbi); cload("sp", mbf[:], bc(mbf_d[l]), Bmbf)
        cload("pool", poolw[:], pw_d[l].rearrange("g c d -> c g d"), Bpoolw)
        if not en[0]:
            cbarrier()
            return
        for tt_, bb_ in Zpad + Cpad:
            E("dve", lambda e, tt_=tt_: e.memset(tt_[:], 0.0), w=[bb_])
        def arena(i):
            return yb[i][0][:].rearrange("p a b -> p (a b)").bitcast(F32)

        def carve(i, off, shp):
            n = shp[0] * shp[1]
            return (_V(arena(i)[:, off:off + n].rearrange("p (a b) -> p a b", a=shp[0])), yb[i][1])
        A = [sbt([128, 16], F32, "A") for _ in range(2)]
        ldt, Bldt = sbt([128, 16], F32, "ldt")
        Bm = [carve(0, c * 256, (16, 16)) for c in range(2)]
        Cm = [carve(0, 512 + c * 256, (16, 16)) for c in range(2)]
        cload("sp", A[0][0][:], are_d[l].rearrange("(gp gl) n -> (gl n) gp", gl=2), A[0][1], slow=True)
        cload("sp", A[1][0][:], aim_d[l].rearrange("(gp gl) n -> (gl n) gp", gl=2), A[1][1], slow=True)
        for gl in range(2):
            cload("sp", ldt[gl * 64:(gl + 1) * 64, :], ldt_d[l, gl::2].partition_broadcast(64), Bldt, slow=True)
        for c, src in enumerate((bre_d, bim_d)):
            cload("sp", Bm[c][0][:], src[l].rearrange("(gp gl) n q -> (gl n) gp q", gl=2), Bm[c][1], slow=True)
        for c, src in enumerate((cre_d, cim_d)):
            for gl in range(2):
                for gp in range(16):
                    cload("sp", Cm[c][0][gl * 64:(gl + 1) * 64, gp, :], src[l, 2 * gp + gl].rearrange("p n -> n p"), Cm[c][1], slow=True)
        cbarrier()
        tn = [sbt([128, 16], F32, "tn") for _ in range(12)]

        def V(i):
            return tn[i][0][:]

        def Bv(i):
            return tn[i][1]
        Are, Aim = A[0][0][:], A[1][0][:]
        BA = [A[0][1], A[1][1]]
        E("act", lambda e: e.activation(out=ldt[:], in_=ldt[:], func=AF.Exp), [Bldt], [Bldt])
        mul("dve", V(0), Are, ldt[:], [BA[0], Bldt], [Bv(0)])
        mul("dve", V(1), Aim, ldt[:], [BA[1], Bldt], [Bv(1)])
        E("act", lambda e: e.activation(out=V(2), in_=V(0), func=AF.Exp), [Bv(0)], [Bv(2)])
        E("act", lambda e: e.activation(out=V(11), in_=V(0), func=AF.Exp, scale=8.0), [Bv(0)], [Bv(11)])
        ki, Bki = sbt([128, 16], I32, "ki")
        E("dve", lambda e: e.tensor_scalar(out=V(3), in0=V(1), scalar1=1.0 / (2 * math.pi), scalar2=None, op0=ALU.mult), [Bv(1)], [Bv(3)])
        E("dve", lambda e: e.tensor_copy(out=ki[:], in_=V(3)), [Bv(3)], [Bki])
        E("dve", lambda e: e.tensor_copy(out=V(3), in_=ki[:]), [Bki], [Bv(3)])
        E("dve", lambda e: e.scalar_tensor_tensor(out=V(4), in0=V(3), scalar=-2 * math.pi, in1=V(1), op0=ALU.mult, op1=ALU.add), [Bv(3), Bv(1)], [Bv(4)])
        E("act", lambda e: e.activation(out=V(5), in_=V(4), func=AF.Sin, scale=0.5), [Bv(4)], [Bv(5)])
        E("act", lambda e: e.activation(out=V(6), in_=V(4), func=AF.Sin, scale=0.25), [Bv(4)], [Bv(6)])
        mul("dve", V(7), V(6), V(6), [Bv(6)], [Bv(7)])
        E("dve", lambda e: e.tensor_scalar(out=V(7), in0=V(7), scalar1=-2.0, scalar2=1.0, op0=ALU.mult, op1=ALU.add), [Bv(7)], [Bv(7)])
        mul("dve", V(8), V(5), V(7), [Bv(5), Bv(7)], [Bv(8)])
        mul("dve", V(5), V(5), V(5), [Bv(5)], [Bv(5)])
        E("dve", lambda e: e.tensor_scalar(out=V(5), in0=V(5), scalar1=-2.0, scalar2=1.0, op0=ALU.mult, op1=ALU.add), [Bv(5)], [Bv(5)])
        Pw = [sbt([128, 9, 16], F32, "Pw") for _ in range(2)]
        Pr, Pi = Pw[0][0], Pw[1][0]
        BP = [Pw[0][1], Pw[1][1]]
        E("dve", lambda e: e.memset(Pr[:, 0, :], 1.0), w=[BP[0]])
        E("dve", lambda e: e.memset(Pi[:, 0, :], 0.0), w=[BP[1]])
        mul("dve", Pr[:, 1, :], V(2), V(5), [Bv(2), Bv(5)], [BP[0]])
        E("dve", lambda e: e.scalar_tensor_tensor(out=Pi[:, 1, :], in0=V(8), scalar=2.0, in1=V(2), op0=ALU.mult, op1=ALU.mult), [Bv(8), Bv(2)], [BP[1]])

        def cmul(o_r, o_i, xr, xi, yr, yi, r, w, t1, t2, Bt):
            mul("dve", t1, xr, yr, r, Bt); mul("dve", t2, xi, yi, r, Bt)
            tt("dve", o_r, t1, t2, ALU.subtract, Bt, [w[0]])
            mul("dve", t1, xr, yi, r, Bt); mul("dve", t2, xi, yr, r, Bt)
            tt("dve", o_i, t1, t2, ALU.add, Bt, [w[1]])
        for j in range(1, 8):
            cmul(Pr[:, j + 1, :], Pi[:, j + 1, :], Pr[:, j, :], Pi[:, j, :], Pr[:, 1, :], Pi[:, 1, :], BP, BP, V(9), V(10), [Bv(9), Bv(10)])
        E("dve", lambda e: e.reciprocal(out=V(0), in_=V(11)), [Bv(11)], [Bv(0)])
        wl = [sbt([128, 16], F32, "wl") for _ in range(4)]
        mul("dve", wl[0][0][:], Pr[:, 8, :], V(0), [BP[0], Bv(0)], [wl[0][1]])
        E("dve", lambda e: e.scalar_tensor_tensor(out=wl[1][0][:], in0=Pi[:, 8, :], scalar=-1.0, in1=V(0), op0=ALU.mult, op1=ALU.mult), [BP[1], Bv(0)], [wl[1][1]])
        Er, Ei = Etab[0][0], Etab[1][0]
        BE = [Etab[0][1], Etab[1][1]]
        E("dve", lambda e: e.tensor_copy(out=Er[:, :, 0], in_=wl[0][0][:]), [wl[0][1]], [BE[0]])
        E("dve", lambda e: e.tensor_copy(out=Ei[:, :, 0], in_=wl[1][0][:]), [wl[1][1]], [BE[1]])
        t3a, Bt3a = carve(2, 0, (16, 32)); t3b, Bt3b = carve(2, 512, (16, 32))
        cur = 0
        Lw = 1
        while Lw < 64:
            wr = wl[cur][0][:].unsqueeze(2).to_broadcast([128, 16, Lw]); wi = wl[cur + 1][0][:].unsqueeze(2).to_broadcast([128, 16, Lw])
            Bw = [wl[cur][1], wl[cur + 1][1]]
            cmul(Er[:, :, Lw:2 * Lw], Ei[:, :, Lw:2 * Lw], Er[:, :, 0:Lw], Ei[:, :, 0:Lw], wr, wi, BE + Bw, BE, t3a[:, :, 0:Lw], t3b[:, :, 0:Lw], [Bt3a, Bt3b])
            nxt = 2 - cur
            cmul(wl[nxt][0][:], wl[nxt + 1][0][:], wl[cur][0][:], wl[cur + 1][0][:], wl[cur][0][:], wl[cur + 1][0][:], Bw, [wl[nxt][1], wl[nxt + 1][1]], V(9), V(10), [Bv(9), Bv(10)])
            cur = nxt
            Lw *= 2
        E("dve", lambda e: e.tensor_copy(out=rho[:], in_=V(11)), [Bv(11)], [Brho])
        E("dve", lambda e: e.tensor_scalar(out=V(0), in0=Pr[:, 1, :], scalar1=-1.0, scalar2=None, op0=ALU.add), [BP[0]], [Bv(0)])
        mul("dve", V(1), Are, Are, [BA[0]], [Bv(1)]); mul("dve", V(2), Aim, Aim, [BA[1]], [Bv(2)])
        tt("dve", V(1), V(1), V(2), ALU.add, [Bv(1), Bv(2)], [Bv(1)])
        E("dve", lambda e: e.reciprocal(out=V(1), in_=V(1)), [Bv(1)], [Bv(1)])
        mul("dve", V(2), V(0), Are, [Bv(0), BA[0]], [Bv(2)]); mul("dve", V(3), Pi[:, 1, :], Aim, [BP[1], BA[1]], [Bv(3)])
        tt("dve", V(2), V(2), V(3), ALU.add, [Bv(2), Bv(3)], [Bv(2)]); mul("dve", V(4), V(2), V(1), [Bv(2), Bv(1)], [Bv(4)])
        mul("dve", V(2), Pi[:, 1, :], Are, [BP[1], BA[0]], [Bv(2)]); mul("dve", V(3), V(0), Aim, [Bv(0), BA[1]], [Bv(3)])
        tt("dve", V(2), V(2), V(3), ALU.subtract, [Bv(2), Bv(3)], [Bv(2)]); mul("dve", V(5), V(2), V(1), [Bv(2), Bv(1)], [Bv(5)])
        Bb = [carve(1, c * 256, (16, 16)) for c in range(2)]
        Zz = [carve(1, 512 + c * 256, (16, 16)) for c in range(2)]
        t4a, Bt4a = carve(3, 0, (16, 16)); t4b, Bt4b = carve(3, 256, (16, 16))

        def b16(v):
            return v.unsqueeze(2).to_broadcast([128, 16, 16])
        cmul(Bb[0][0][:], Bb[1][0][:], b16(V(4)), b16(V(5)), Bm[0][0][:], Bm[1][0][:], [Bv(4), Bv(5), Bm[0][1], Bm[1][1]], [Bb[0][1], Bb[1][1]], t4a[:], t4b[:], [Bt4a, Bt4b])
        E("dve", lambda e: e.tensor_scalar(out=t4a[:], in0=Cm[1][0][:], scalar1=-1.0, scalar2=None, op0=ALU.mult), [Cm[1][1]], [Bt4a])
        csrc = [(Cm[0][0], Cm[0][1]), (t4a, Bt4a)]

        def padfill(dst, Bd, src, Bs):
            d5 = dst[:].rearrange("p (gq j) (jj c) -> p gq j jj c", j=4, jj=4)
            s4 = src[:].rearrange("p (gq j) q -> p gq j q", j=4)
            for j in range(4):
                for gl in range(2):
                    E("act", lambda e, j=j, gl=gl: e.copy(out=d5[gl * 64:(gl + 1) * 64, :, j, j, gl * 16:(gl + 1) * 16], in_=s4[gl * 64:(gl + 1) * 64, :, j, :]), [Bs], [Bd])
        for c in range(2):
            padfill(Cpad[c][0], Cpad[c][1], csrc[c][0], csrc[c][1])
        for jp in range(8):
            cmul(Zz[0][0][:], Zz[1][0][:], b16(Pr[:, jp, :]), b16(Pi[:, jp, :]), Bb[0][0][:], Bb[1][0][:], BP + [Bb[0][1], Bb[1][1]], [Zz[0][1], Zz[1][1]], t4a[:], t4b[:], [Bt4a, Bt4b])
            for c in range(2):
                padfill(Zpad[c][0], Zpad[c][1], Zz[c][0], Zz[c][1])
            s = 7 - jp
            for gq in range(4):
                for c in range(2):
                    ps, Bp = nextps()
                    for j in range(4):
                        E("pe", lambda e, j=j, c=c, gq=gq: e.matmul(ps[:, 0:128], Zpad[c][0][:, 4 * gq + j, :], identf[:], start=(j == 0), stop=(j == 3)), [Zpad[c][1], Bidf], [Bp])
                    E("dve", lambda e, c=c, gq=gq, ps=ps: e.tensor_copy(out=Min[:, gq, s, c, :], in_=ps[:, 0:128]), [Bp], [BMin])
                ps, Bp = nextps()
                for j in range(4):
                    for c in range(2):
                        E("pe", lambda e, j=j, c=c, gq=gq: e.matmul(ps[:, 0:128], Zpad[c][0][:, 4 * gq + j, :], Cpad[c][0][:, 4 * gq + j, :], start=(j == 0 and c == 0), stop=(j == 3 and c == 1)),
                          [Zpad[c][1], Cpad[c][1]], [Bp])
                E("dve", lambda e, gq=gq, ps=ps: e.tensor_copy(out=Ktoe[:, gq, jp, :], in_=ps[:, 0:128]), [Bp], [BKtoe])
        for t in range(8):
            cmul(Zz[0][0][:], Zz[1][0][:], b16(Pr[:, t + 1, :]), b16(Pi[:, t + 1, :]), Cm[0][0][:], Cm[1][0][:], BP + [Cm[0][1], Cm[1][1]], [Zz[0][1], Zz[1][1]], t4a[:], t4b[:], [Bt4a, Bt4b])
            for gl in range(2):
                E("dve", lambda e, gl=gl, t=t: e.tensor_copy(out=Mout[gl * 64:(gl + 1) * 64, :, t, 0, gl * 16:(gl + 1) * 16], in_=Zz[0][0][gl * 64:(gl + 1) * 64]), [Zz[0][1]], [BMout])
                E("dve", lambda e, gl=gl, t=t: e.tensor_scalar(out=Mout[gl * 64:(gl + 1) * 64, :, t, 1, gl * 16:(gl + 1) * 16], in0=Zz[1][0][gl * 64:(gl + 1) * 64], scalar1=-1.0, scalar2=None, op0=ALU.mult), [Zz[1][1]], [BMout])

    def seq_reset():
        E("dve", lambda e: e.memset(schalo[:], 0.0), w=[Bschalo])
        E("dve", lambda e: e.memset(putok[:, 0, :], 0.0), w=[Bputok])
        E("dve", lambda e: e.memset(mlhalo[:], 0.0), w=[Bmlhalo])
        E("dve", lambda e: e.memset(C32[:], 0.0), w=[BC32])
        E("dve", lambda e: e.memset(Cbf[:], 0.0), w=[BCbf])
        for c in range(2):
            E("dve", lambda e, c=c: e.memset(car[c][0][:], 0.0), w=[car[c][1]])

    def branch_sconv(l):
        xs, Bxs = SC[0]; prod, Bprod = SC[1]; acc, Bacc = SC[2]; zs, Bzs = SC[3]
        wv, Bw = load_win(l, C_SX)
        for cb in range(4):
            ps, Bp = proj_fm(wv, Bw, cb)
            E("act", lambda e, cb=cb, ps=ps: e.copy(out=xs[:, cb, 0:T], in_=ps[:]), [Bp], [Bxs])
        E("dve", lambda e: e.tensor_copy(out=prod[:, :, 0:2], in_=schalo[:]), [Bschalo], [Bprod])
        wv, Bw = load_win(l, C_SC)
        for cb in range(4):
            ps, Bp = proj_fm(wv, Bw, cb)
            mul("dve", prod[:, cb, 2:T + 2], ps[:], xs[:, cb, 0:T], [Bp, Bxs], [Bprod])
        E("dve", lambda e: e.tensor_copy(out=schalo[:], in_=prod[:, :, T:T + 2]), [Bprod], [Bschalo])
        for cb in range(4):
            E("dve", lambda e, cb=cb: e.tensor_scalar(out=acc[:, cb, 0:T], in0=prod[:, cb, 2:T + 2], scalar1=scw[:, 2, cb:cb + 1], scalar2=None, op0=ALU.mult), [Bprod, Bscw], [Bacc])
            for k in (1, 0):
                E("dve", lambda e, cb=cb, k=k: e.scalar_tensor_tensor(out=acc[:, cb, 0:T], in0=prod[:, cb, k:k + T], scalar=scw[:, k, cb:cb + 1], in1=acc[:, cb, 0:T], op0=ALU.mult, op1=ALU.add),
                  [Bprod, Bscw, Bacc], [Bacc])
        wv, Bw = load_win(l, C_SB)
        for cb in range(4):
            ps, Bp = proj_fm(wv, Bw, cb)
            mul("dve", acc[:, cb, 0:T], ps[:], acc[:, cb, 0:T], [Bp, Bacc], [Bacc])
        wv, Bw = load_win(l, C_SZ)
        for cb in range(4):
            ps, Bp = proj_fm(wv, Bw, cb)
            E("act", lambda e, cb=cb, ps=ps: e.activation(out=zs[:, cb, 0:T], in_=ps[:], func=AF.Silu), [Bp], [Bzs])
            mul("dve", yb[2][0][:, cb, :], acc[:, cb, 0:T], zs[:, cb, 0:T], [Bacc, Bzs], [yb[2][1]])

    def branch_pool(l, first):
        import os
        stage = int(os.environ.get("POOLDBG", "9"))
        zs, Bzs = SC[3]
        wv, Bw = load_win(l, C_PU)
        for tb in range(4):
            if stage == -2:
                continue
            ps, Bp = proj_tm(wv, Bw, tb, 0, 512)
            if stage == -1:
                continue
            E("dve", lambda e, tb=tb, ps=ps: e.tensor_copy(out=putok[:, tb + 1, :], in_=ps[:]), [Bp], [Bputok])
        if stage < 1:
            E("dve", lambda e: e.memset(yb[1][0][:], 0.0), w=[yb[1][1]])
            return
        for g in range(4):
            ps, Bp = nextps()
            for tb in range(4):
                f0 = first and tb == 0
                E("pe", lambda e, g=g, tb=tb, ps=ps, f0=f0: e.matmul(ps[:, tb * 128:(tb + 1) * 128], putok[:, tb + 1, g * 128:(g + 1) * 128], band[:, 2 if f0 else 0, g, :], start=True, stop=f0),
                  [Bputok, Bband], [Bp])
                if not f0:
                    E("pe", lambda e, g=g, tb=tb, ps=ps: e.matmul(ps[:, tb * 128:(tb + 1) * 128], putok[:, tb, g * 128:(g + 1) * 128], band[:, 1, g, :], start=False, stop=True),
                      [Bputok, Bband], [Bp])
            E("dve", lambda e, g=g, ps=ps: e.tensor_copy(out=pooledT[:, g, :], in_=ps[:]), [Bp], [BpooledT])
        E("dve", lambda e: e.tensor_copy(out=putok[:, 0, :], in_=putok[:, 4, :]), [Bputok], [Bputok])
        if stage < 2:
            E("dve", lambda e: e.memset(yb[1][0][:], 0.0), w=[yb[1][1]])
            return
        wv, Bw = load_win(l, C_PZ)
        for g in range(4):
            ps, Bp = proj_fm(wv, Bw, g)
            E("act", lambda e, g=g, ps=ps: e.activation(out=zs[:, g, 0:T], in_=ps[:], func=AF.Silu), [Bp], [Bzs])
            ps2, Bp2 = nextps()
            E("pe", lambda e, g=g, ps2=ps2: e.matmul(ps2[:], poolw[:, g, :], pooledT[:, g, :], start=True, stop=True), [Bpoolw, BpooledT], [Bp2])
            E("dve", lambda e, g=g, ps2=ps2: e.scalar_tensor_tensor(out=yb[1][0][:, g, :], in0=ps2[:], scalar=psc[:, g:g + 1], in1=zs[:, g, 0:T], op0=ALU.mult, op1=ALU.mult),
              [Bp2, Bpsc, Bzs], [yb[1][1]])

    def branch_mlstm(l):
        cq, Bcq = SC[0]; ca, Bca = SC[1]
        for qi, (c0, dst, Bd) in enumerate(((C_Q, qT, BqT), (C_K, kT, BkT))):
            wv, Bw = load_win(l, c0)
            E("dve", lambda e, qi=qi: e.tensor_copy(out=cq[:, :, 0:3], in_=mlhalo[:, qi * 4:(qi + 1) * 4, :]), [Bmlhalo], [Bcq])
            for cb in range(4):
                ps, Bp = proj_fm(wv, Bw, cb)
                E("act", lambda e, cb=cb, ps=ps: e.copy(out=cq[:, cb, 3:T + 3], in_=ps[:]), [Bp], [Bcq])
            E("dve", lambda e, qi=qi: e.tensor_copy(out=mlhalo[:, qi * 4:(qi + 1) * 4, :], in_=cq[:, :, T:T + 3]), [Bcq], [Bmlhalo])
            for cb in range(4):
                ch = qi * 4 + cb
                E("dve", lambda e, cb=cb, ch=ch: e.tensor_scalar(out=ca[:, cb, 0:T], in0=cq[:, cb, 3:T + 3], scalar1=mcw[:, 3, ch:ch + 1], scalar2=None, op0=ALU.mult), [Bcq, Bmcw], [Bca])
                for k in (2, 1, 0):
                    E("dve", lambda e, cb=cb, ch=ch, k=k: e.scalar_tensor_tensor(out=ca[:, cb, 0:T], in0=cq[:, cb, k:k + T], scalar=mcw[:, k, ch:ch + 1], in1=ca[:, cb, 0:T], op0=ALU.mult, op1=ALU.add),
                      [Bcq, Bmcw, Bca], [Bca])
                E("act", lambda e, cb=cb, dst=dst: e.activation(out=dst[:, cb, :], in_=ca[:, cb, 0:T], func=AF.Silu), [Bca], [Bd])
        wv, Bw = load_win(l, C_V)
        for tb in range(4):
            ps, Bp = proj_tm(wv, Bw, tb, 0, 512)
            E("dve", lambda e, tb=tb, ps=ps: e.tensor_copy(out=vtok[:, tb, :, 0:128], in_=ps[:].rearrange("p (h d) -> p h d", h=4)), [Bp], [Bvtok])
        wv, Bw = load_win(l, C_O)
        for tb in range(4):
            ps, Bp = proj_tm(wv, Bw, tb, 0, 512)
            E("act", lambda e, tb=tb, ps=ps: e.activation(out=ogt[:, tb, :], in_=ps[:], func=AF.Sigmoid), [Bp], [Bogt])
        wv, Bw = load_win(l, C_IF, 520)
        for tb in range(4):
            ps, Bp = proj_tm(wv, Bw, tb, 8, 512)
            E("act", lambda e, tb=tb, ps=ps: e.activation(out=zwt[:, tb, :], in_=ps[:], func=AF.Silu), [Bp], [Bzwt])
            mul("dve", zwt[:, tb, :], zwt[:, tb, :], mnw[:], [Bzwt, Bmnw], [Bzwt])
            psg, Bpg = proj_tm(wv, Bw, tb, 0, 8)
            G = gt[:, tb, :]
            tt("dve", G[:, 0:4], psg[:, 0:4], mbi[:], ALU.add, [Bpg, Bmbi], [Bgt])
            tt("dve", G[:, 4:8], psg[:, 4:8], mbf[:], ALU.add, [Bpg, Bmbf], [Bgt])
            E("act", lambda e, G=G: e.activation(out=G[:, 4:8], in_=G[:, 4:8], func=AF.Exp, scale=-1.0), [Bgt], [Bgt])
            E("act", lambda e, G=G: e.activation(out=G[:, 4:8], in_=G[:, 4:8], func=AF.Ln, bias=1.0), [Bgt], [Bgt])
            ps2, Bp2 = nextps()
            E("pe", lambda e, G=G, ps2=ps2: e.matmul(ps2[:, 0:4], trif[:], G[:, 4:8], start=True, stop=True), [Btrif, Bgt], [Bp2])
            E("pe", lambda e, G=G, ps2=ps2: e.matmul(ps2[:, 4:8], onesf[:], G[:, 4:8], start=True, stop=True), [Bonesf, Bgt], [Bp2])
            E("act", lambda e, G=G, ps2=ps2: e.activation(out=G[:, 8:12], in_=ps2[:, 0:4], func=AF.Exp, scale=-1.0, bias=math.log(128 ** -0.5)), [Bp2], [Bgt])
            tt("dve", G[:, 24:28], G[:, 0:4], ps2[:, 0:4], ALU.add, [Bgt, Bp2], [Bgt])
            E("act", lambda e, G=G: e.activation(out=G[:, 12:16], in_=G[:, 24:28], func=AF.Exp), [Bgt], [Bgt])
            tt("dve", G[:, 24:28], G[:, 24:28], ps2[:, 4:8], ALU.subtract, [Bgt, Bp2], [Bgt])
            E("act", lambda e, G=G: e.activation(out=G[:, 16:20], in_=G[:, 24:28], func=AF.Exp), [Bgt], [Bgt])
            E("act", lambda e, G=G, ps2=ps2: e.activation(out=G[:, 20:24], in_=ps2[:, 4:8], func=AF.Exp, scale=-1.0), [Bp2], [Bgt])
        pbt = None
        for tb in range(4):
            G = gt[:, tb, :]
            sl = slice(tb * 128, (tb + 1) * 128)
            for hd in range(4):
                pk, Bpk = nextps()
                pkb = pk[:].bitcast(BF16)
                E("pe", lambda e, hd=hd, pkb=pkb: e.transpose(pkb[:, 0:128], kT[:, hd, sl], identb[:]), [BkT, Bidb], [Bpk])
                E("dve", lambda e, hd=hd, pkb=pkb, G=G: e.tensor_scalar(out=ktil[:], in0=pkb[:, 0:128], scalar1=G[:, 16 + hd:17 + hd], scalar2=None, op0=ALU.mult), [Bpk, Bgt], [Bktil])
                pS, BpS = nextps()
                E("pe", lambda e, hd=hd, pS=pS: e.matmul(pS[:, 0:128], kT[:, hd, sl], qT[:, hd, sl], start=True, stop=True), [BkT, BqT], [BpS])
                E("dve", lambda e, hd=hd, pS=pS, G=G: e.scalar_tensor_tensor(out=PT[:], in0=pS[:, 0:128], scalar=G[:, 12 + hd:13 + hd], in1=trif[:], op0=ALU.mult, op1=ALU.mult), [BpS, Bgt, Btrif], [BPT])
                pN, BpN = nextps()
                E("pe", lambda e, hd=hd, pN=pN: e.matmul(pN[:, 0:129], PT[:], vtok[:, tb, hd, :], start=True, stop=False), [BPT, Bvtok], [BpN])
                E("pe", lambda e, hd=hd, pN=pN: e.matmul(pN[:, 0:129], qT[:, hd, sl], Cbf[:, hd, :], start=False, stop=True), [BqT, BCbf], [BpN])
                pC, BpC = nextps()
                E("pe", lambda e, hd=hd, pC=pC: e.matmul(pC[:, 0:129], ktil[:], vtok[:, tb, hd, :], start=True, stop=True), [Bktil, Bvtok], [BpC])
                E("dve", lambda e, hd=hd, pC=pC, G=G: e.scalar_tensor_tensor(out=C32[:, hd, :], in0=C32[:, hd, :], scalar=G[:, 20 + hd:21 + hd], in1=pC[:, 0:129], op0=ALU.mult, op1=ALU.add), [BC32, Bgt, BpC], [BC32])
                E("dve", lambda e, hd=hd: e.tensor_copy(out=Cbf[:, hd, :], in_=C32[:, hd, :]), [BC32], [BCbf])
                E("act", lambda e, hd=hd, pN=pN, G=G: e.activation(out=sm[:, 0:1], in_=pN[:, 128:129], func=AF.Abs, scale=G[:, 8 + hd:9 + hd]), [BpN, Bgt], [Bsm])
                E("dve", lambda e: e.tensor_scalar(out=sm[:, 0:1], in0=sm[:, 0:1], scalar1=1.0, scalar2=None, op0=ALU.max), [Bsm], [Bsm])
                E("dve", lambda e: e.reciprocal(out=sm[:, 1:2], in_=sm[:, 0:1]), [Bsm], [Bsm])
                mul("dve", sm[:, 2:3], sm[:, 1:2], G[:, 8 + hd:9 + hd], [Bsm, Bgt], [Bsm])
                E("dve", lambda e, hd=hd, pN=pN: e.scalar_tensor_tensor(out=hh[:, hd, :], in0=pN[:, 0:128], scalar=sm[:, 2:3], in1=ogt[:, tb, hd * 128:(hd + 1) * 128], op0=ALU.mult, op1=ALU.mult), [BpN, Bsm, Bogt], [Bhh])
                E("act", lambda e, hd=hd: e.activation(out=junk[:, 0:128], in_=hh[:, hd, :], func=AF.Square, accum_out=sm[:, 4 + hd:5 + hd]), [Bhh], [Bjunk, Bsm])
            E("dve", lambda e: e.tensor_scalar(out=sm[:, 8:12], in0=sm[:, 4:8], scalar1=1.0 / 128, scalar2=EPS, op0=ALU.mult, op1=ALU.add), [Bsm], [Bsm])
            E("act", lambda e: e.activation(out=sm[:, 8:12], in_=sm[:, 8:12], func=AF.Sqrt), [Bsm], [Bsm])
            E("dve", lambda e: e.reciprocal(out=sm[:, 12:16], in_=sm[:, 8:12]), [Bsm], [Bsm])
            for hd in range(4):
                E("dve", lambda e, hd=hd: e.scalar_tensor_tensor(out=ydtok[:, hd * 128:(hd + 1) * 128], in0=hh[:, hd, :], scalar=sm[:, 12 + hd:13 + hd], in1=zwt[:, tb, hd * 128:(hd + 1) * 128], op0=ALU.mult, op1=ALU.mult),
                  [Bhh, Bsm, Bzwt], [Bydtok])
            pt, Bpt = nextps()
            ptb = pt[:].bitcast(BF16)
            for cb in range(4):
                E("pe", lambda e, cb=cb, ptb=ptb: e.transpose(ptb[:, cb * 128:(cb + 1) * 128], ydtok[:, cb * 128:(cb + 1) * 128], identb[:]), [Bydtok, Bidb], [Bpt])
            E("act", lambda e, ptb=ptb: e.copy(out=yb[3][0][:, :, sl], in_=ptb[:, 0:512].rearrange("p (c t) -> p c t", c=4)), [Bpt], [yb[3][1]])

    def s5_p1(l):
        wv, Bw = load_win(l, C_S5U)
        for cb in range(4):
            ps, Bp = proj_fm(wv, Bw, cb)
            E("dve", lambda e, cb=cb, ps=ps: e.tensor_copy(out=uT[:, cb, :], in_=ps[:]), [Bp], [BuT])
        uS = uT[:].rearrange("p g (c s) -> p g s c", s=8)
        Er, Ei = Etab[0][0], Etab[1][0]
        BE = [Etab[0][1], Etab[1][1]]
        Xps = {}
        for c in range(2):
            for hf in range(2):
                Xps[(c, hf)] = nextps()
        for gp in range(16):
            gq, j = divmod(gp, 4)
            for c in range(2):
                ps, Bp = Xps[(c, gp // 8)]
                pv = ps[:].rearrange("p (g c) -> p g c", g=8)
                for s in range(8):
                    E("pe", lambda e, pv=pv, gp=gp, gq=gq, j=j, c=c, s=s: e.matmul(pv[:, gp % 8, :], Min[32 * j:32 * j + 32, gq, s, c, :], uS[32 * j:32 * j + 32, gq, s, :],
                                                                        start=(s == 0), stop=(s == 7), tile_position=(32 * j, 0)), [BMin, BuT], [Bp])
        for hf in range(2):
            gs = slice(hf * 8, hf * 8 + 8)
            xr, Bxr = Xps[(0, hf)]; xi, Bxi = Xps[(1, hf)]
            xrv = xr[:].rearrange("p (g c) -> p g c", g=8); xiv = xi[:].rearrange("p (g c) -> p g c", g=8)
            t1, Bt1 = s5t[0]; t2, Bt2 = s5t[1]
            mul("dve", t1[:, gs, :], xrv, Er[:, gs, :], [Bxr, BE[0]], [Bt1]); mul("dve", t2[:, gs, :], xiv, Ei[:, gs, :], [Bxi, BE[1]], [Bt2])
            tt("dve", Xr[0][0][:, gs, :], t1[:, gs, :], t2[:, gs, :], ALU.subtract, [Bt1, Bt2], [Xr[0][1]])
            mul("dve", t1[:, gs, :], xiv, Er[:, gs, :], [Bxi, BE[0]], [Bt1]); mul("dve", t2[:, gs, :], xrv, Ei[:, gs, :], [Bxr, BE[1]], [Bt2])
            tt("dve", Xr[1][0][:, gs, :], t1[:, gs, :], t2[:, gs, :], ALU.add, [Bt1, Bt2], [Xr[1][1]])
        for c in range(2):
            for gp in range(16):
                E("dve", lambda e, c=c, gp=gp: e.tensor_tensor_scan(out=Hs[c][0][:, gp, :], data0=rho[:, gp:gp + 1].to_broadcast([128, 64]), data1=Xr[c][0][:, gp, :], initial=car[c][0][:, gp:gp + 1], op0=ALU.mult, op1=ALU.add),
                  [Brho, Xr[c][1], car[c][1]], [Hs[c][1]])
        t1, Bt1 = s5t[0]; t2, Bt2 = s5t[1]
        mul("dve", t1[:], Er[:], Hs[0][0][:], [BE[0], Hs[0][1]], [Bt1]); mul("dve", t2[:], Ei[:], Hs[1][0][:], [BE[1], Hs[1][1]], [Bt2])
        tt("dve", Hu[0][0][:], t1[:], t2[:], ALU.add, [Bt1, Bt2], [Hu[0][1]])
        mul("dve", t1[:], Er[:], Hs[1][0][:], [BE[0], Hs[1][1]], [Bt1]); mul("dve", t2[:], Ei[:], Hs[0][0][:], [BE[1], Hs[0][1]], [Bt2])
        tt("dve", Hu[1][0][:], t1[:], t2[:], ALU.subtract, [Bt1, Bt2], [Hu[1][1]])
        for c in range(2):
            E("dve", lambda e, c=c: e.tensor_copy(out=Hp[c][0][:, :, 0], in_=car[c][0][:]), [car[c][1]], [Hp[c][1]])
            E("dve", lambda e, c=c: e.tensor_copy(out=Hp[c][0][:, :, 1:64], in_=Hu[c][0][:, :, 0:63]), [Hu[c][1]], [Hp[c][1]])
            E("dve", lambda e, c=c: e.tensor_copy(out=car[c][0][:], in_=Hu[c][0][:, :, 63]), [Hu[c][1]], [car[c][1]])

    def s5_p2a(l):
        ya, Bya = SC[1]; yg, Byg = SC[2]
        uS = uT[:].rearrange("p g (c s) -> p g s c", s=8)
        for gq in range(4):
            ps, Bp = nextps()
            pv = ps[:].rearrange("p (t c) -> p t c", t=8)
            for t in range(8):
                for s in range(t + 1):
                    E("pe", lambda e, pv=pv, gq=gq, t=t, s=s: e.matmul(pv[:, t, :], Ktoe[:, gq, t - s, :], uS[:, gq, s, :], start=(s == 0), stop=False), [BKtoe, BuT], [Bp])
                for j in range(4):
                    gp = 4 * gq + j
                    for c in range(2):
                        E("pe", lambda e, pv=pv, gp=gp, j=j, t=t, c=c: e.matmul(pv[32 * j:32 * j + 32, t, :], Mout[:, gp, t, c, :], Hp[c][0][:, gp, :], start=False, stop=(c == 1), tile_position=(0, 32 * j)),
                          [BMout, Hp[c][1]], [Bp])
            E("dve", lambda e, gq=gq, pv=pv: e.scalar_tensor_tensor(out=ya[:, gq, 0:T].rearrange("p (c t) -> p c t", t=8), in0=u32[:, gq, :].rearrange("p (c t) -> p c t", t=8), scalar=s5dd[:, gq:gq + 1],
                                                                    in1=pv.rearrange("p t c -> p c t"), op0=ALU.mult, op1=ALU.add), [Bu32, Bs5dd, Bp], [Bya])
        t1, Bt1 = SC[0]
        for cb in range(4):
            Y = ya[:, cb, 0:T]
            mul("dve", t1[:, cb, 0:T], Y, Y, [Bya], [Bt1])
            E("dve", lambda e, cb=cb: e.tensor_scalar(out=t1[:, cb, 0:T], in0=t1[:, cb, 0:T], scalar1=0.044715, scalar2=1.0, op0=ALU.mult, op1=ALU.add), [Bt1], [Bt1])
            mul("dve", t1[:, cb, 0:T], t1[:, cb, 0:T], Y, [Bt1, Bya], [Bt1])
            E("act", lambda e, cb=cb: e.activation(out=t1[:, cb, 0:T], in_=t1[:, cb, 0:T], func=AF.Sigmoid, scale=2.0 * math.sqrt(2.0 / math.pi)), [Bt1], [Bt1])
            mul("dve", yg[:, cb, 0:T], t1[:, cb, 0:T], Y, [Bt1, Bya], [Byg])
            E("dve", lambda e, cb=cb: e.tensor_copy(out=uT[:, cb, :], in_=yg[:, cb, 0:T]), [Byg], [BuT])

    def s5_p2b(l):
        yg, Byg = SC[2]
        B3 = SC[3][1]
        t1v = SC[3][0][:, 2, 0:T]
        zsv = SC[3][0][:, 3, 0:T]
        gv, Bs = load_img(l, ("glu",))
        wv, Bw = load_win(l, C_S5Z)
        for cb in range(4):
            ps, Bp = nextps()
            for kc in range(4):
                E("pe", lambda e, cb=cb, kc=kc, ps=ps: e.matmul(ps[:], gv[:, kc, cb * 128:(cb + 1) * 128], uT[:, kc, :], start=(kc == 0), stop=(kc == 3)), [Bs, BuT], [Bp])
            E("act", lambda e, cb=cb, ps=ps: e.activation(out=t1v, in_=ps[:], func=AF.Sigmoid), [Bp], [B3])
            mul("dve", yg[:, cb, 0:T], yg[:, cb, 0:T], t1v, [Byg, B3], [Byg])
            ps, Bp = proj_fm(wv, Bw, cb)
            E("act", lambda e, cb=cb, ps=ps: e.activation(out=zsv, in_=ps[:], func=AF.Silu), [Bp], [B3])
            mul("dve", yb[0][0][:, cb, :], yg[:, cb, 0:T], zsv, [Byg, B3], [yb[0][1]])

    Bx4 = [Buf("x%d" % i) for i in range(4)]
    xds4 = [kb.dsem("xds4_%d" % i) for i in range(4)]
    ods4 = [kb.dsem("ods4_%d" % i) for i in range(4)]
    stp, Bstp = sbt([128, 16], F32, "stp")

    def dram_io(l, s, t):
        if L == 1:
            return x_d, y_d, ("x", s, t), ("y", s, t)
        src_d = x_d if l == 0 else scr_d[(l - 1) % 2]
        dst_d = y_d if l == L - 1 else scr_d[l % 2]
        skey = ("x", s, t) if l == 0 else ((l - 1) % 2, s, t)
        dkey = ("y", s, t) if l == L - 1 else (l % 2, s, t)
        return src_d, dst_d, skey, dkey

    def emit_load_prenorm(l, s, t, tb):
        xtile = xt[0][0]
        Bx = Bx4[tb]
        src_d, _, skey, _ = dram_io(l, s, t)
        kb.dma("sp", xds4[tb], lambda q: q.dma_start(out=xtile[:, tb, :], in_=src_d[s, t * T + tb * 128:t * T + (tb + 1) * 128, :]), reads=[dbuf(skey)], writes=[Bx])
        E("act", lambda e: e.activation(out=junk[:], in_=xtile[:, tb, :], func=AF.Square, accum_out=stp[:, tb:tb + 1]), [Bx], [Bjunk, Bstp])
        E("dve", lambda e: e.tensor_scalar(out=stp[:, 4 + tb:5 + tb], in0=stp[:, tb:tb + 1], scalar1=1.0 / D, scalar2=EPS, op0=ALU.mult, op1=ALU.add), [Bstp], [Bstp])
        E("act", lambda e: e.activation(out=stp[:, 4 + tb:5 + tb], in_=stp[:, 4 + tb:5 + tb], func=AF.Sqrt), [Bstp], [Bstp])
        E("dve", lambda e: e.reciprocal(out=stp[:, 8 + tb:9 + tb], in_=stp[:, 4 + tb:5 + tb]), [Bstp], [Bstp])
        E("dve", lambda e: e.scalar_tensor_tensor(out=htok[:], in0=xtile[:, tb, :], scalar=stp[:, 8 + tb:9 + tb], in1=npw[:], op0=ALU.mult, op1=ALU.mult), [Bx, Bstp, Bnpw], [Bhtok])
        pt, Bpt = nextps()
        ptb = pt[:].bitcast(BF16)
        for kc in range(8):
            E("pe", lambda e, kc=kc, ptb=ptb: e.transpose(ptb[:, kc * 128:(kc + 1) * 128], htok[:, kc * 128:(kc + 1) * 128], identb[:]), [Bhtok, Bidb], [Bpt])
        E("act", lambda e, ptb=ptb: e.copy(out=hT[:, :, tb * 128:(tb + 1) * 128], in_=ptb.rearrange("p (k c) -> p k c", k=8)), [Bpt], [BhT])

    def tile_layer(l, s, t, prenorm_done, nxt):
        xtile = xt[0][0]
        _, dst_d, _, dkey = dram_io(l, s, t)
        if not prenorm_done:
            for tb in range(4):
                emit_load_prenorm(l, s, t, tb)
        for b in range(4):
            if not en[b]:
                E("dve", lambda e, b=b: e.memset(yb[b][0][:], 0.0), w=[yb[b][1]])
        if en[2]:
            branch_sconv(l)
        if en[1]:
            branch_pool(l, t == 0)
        if en[0]:
            s5_p1(l)
        if en[3]:
            branch_mlstm(l)
        if en[0]:
            s5_p2a(l)
        merge_pass(l, [1, 2, 3], True, False)
        if en[0]:
            s5_p2b(l)
        if dbg and t == NT - 1 and s == 0 and l == L - 1:
            for b in range(4):
                dt_, Bdt = SC[2]
                E("act", lambda e, b=b: e.copy(out=dt_[:, :, 0:T], in_=yb[b][0][:]), [yb[b][1]], [Bdt])
                kb.dma("sp", ods, lambda q, b=b: q.dma_start(out=dbg_d[b], in_=dt_[:, :, 0:T]), reads=[Bdt])
        merge_pass(l, [0], False, True)
        out_proj(l, s, t, nxt)

    def merge_pass(l, blist, init, final):
        for bi, b in enumerate(blist):
            first = init and bi == 0
            lastb = final and bi == len(blist) - 1
            for hf in range(2):
                wbv, Bs = load_img(l, ("wbr", b, hf))
                gvw, Bg = load_win(l, C_G + b * 1024 + hf * 512)
                for jj in range(4):
                    j = hf * 4 + jj
                    ps, Bp = proj_fm(gvw, Bg, jj)
                    gs_, Bgs = gsb[0]
                    E("act", lambda e, ps=ps, gs_=gs_: e.activation(out=gs_[:], in_=ps[:], func=AF.Sigmoid), [Bp], [Bgs])
                    ps2, Bp2 = nextps()
                    for cc in range(4):
                        E("pe", lambda e, cc=cc, jj=jj, ps2=ps2, b=b: e.matmul(ps2[:], wbv[:, cc, jj * 128:(jj + 1) * 128], yb[b][0][:, cc, :], start=(cc == 0), stop=(cc == 3)), [Bs, yb[b][1]], [Bp2])
                    if first:
                        mul("dve", SC[j // 4][0][:, j % 4, 0:T], ps2[:], gs_[:], [Bp2, Bgs], [SC[j // 4][1]])
                    else:
                        tm_, Btm = tmpm[0]
                        mul("dve", tm_[:], ps2[:], gs_[:], [Bp2, Bgs], [Btm])
                        if not lastb:
                            tt("dve", SC[j // 4][0][:, j % 4, 0:T], SC[j // 4][0][:, j % 4, 0:T], tm_[:], ALU.add, [SC[j // 4][1], Btm], [SC[j // 4][1]])
                        else:
                            tt("dve", mrgT[:, j, :], SC[j // 4][0][:, j % 4, 0:T], tm_[:], ALU.add, [SC[j // 4][1], Btm], [BmrgT])

    def out_proj(l, s, t, nxt):
        xtile = xt[0][0]
        _, dst_d, _, dkey = dram_io(l, s, t)
        wo = []
        for hf in range(2):
            wo.append([load_img(l, ("wo", hf, q2)) for q2 in range(2)])
        for tb in range(4):
            pss = []
            for hf in range(2):
                ps, Bp = nextps()
                for q2 in range(2):
                    for j in range(8):
                        E("pe", lambda e, j=j, ps=ps, hf=hf, q2=q2: e.matmul(ps[:, q2 * 256:(q2 + 1) * 256], mrgT[:, j, tb * 128:(tb + 1) * 128], wo[hf][q2][0][:, j, :], start=(j == 0), stop=(j == 7)),
                          [BmrgT, wo[hf][q2][1]], [Bp])
                E("act", lambda e, ps=ps, hf=hf: e.activation(out=junk[:, 0:512], in_=ps[:], func=AF.Square, accum_out=st4[:, 12 + hf:13 + hf]), [Bp], [Bjunk, Bst4])
                pss.append((ps, Bp))
            tt("dve", st4[:, 14:15], st4[:, 12:13], st4[:, 13:14], ALU.add, [Bst4], [Bst4])
            E("dve", lambda e: e.tensor_scalar(out=st4[:, 14:15], in0=st4[:, 14:15], scalar1=1.0 / D, scalar2=EPS, op0=ALU.mult, op1=ALU.add), [Bst4], [Bst4])
            E("act", lambda e: e.activation(out=st4[:, 14:15], in_=st4[:, 14:15], func=AF.Sqrt), [Bst4], [Bst4])
            E("dve", lambda e: e.reciprocal(out=st4[:, 15:16], in_=st4[:, 14:15]), [Bst4], [Bst4])
            for hf in range(2):
                ps, Bp = pss[hf]
                E("dve", lambda e, ps=ps, hf=hf: e.scalar_tensor_tensor(out=junk[:, hf * 512:(hf + 1) * 512], in0=ps[:], scalar=st4[:, 15:16], in1=ppw[:, hf * 512:(hf + 1) * 512], op0=ALU.mult, op1=ALU.mult),
                  [Bp, Bst4, Bppw], [Bjunk])
            tt("dve", xtile[:, tb, :], xtile[:, tb, :], junk[:], ALU.add, [Bx4[tb], Bjunk], [Bx4[tb]])
            kb.dma("sp", ods4[tb], lambda q, tb=tb: q.dma_start(out=dst_d[s, t * T + tb * 128:t * T + (tb + 1) * 128, :], in_=xtile[:, tb, :]), reads=[Bx4[tb]], writes=[dbuf(dkey)])
            if nxt is not None:
                emit_load_prenorm(nxt[0], nxt[1], nxt[2], tb)
        return None

    last = None
    convert_layer(0)
    for l in range(L):
        if l > 0:
            kb.new_epoch("_%d" % l)
        layer_prep(l)
        if l + 1 < L:
            convert_layer(l + 1)
        order = [(s, t) for s in range(NSEQ) for t in range(NT)]
        for i, (s, t) in enumerate(order):
            if t == 0:
                seq_reset()
            nxt = (l, order[i + 1][0], order[i + 1][1]) if i + 1 < len(order) else None
            if not PIPE:
                nxt = None
            tile_layer(l, s, t, (i > 0) and PIPE, nxt)
    kb._wait("sp", [Tok(d_.sem, d_.cnt) for d_ in ods4] + [Tok(ods.sem, ods.cnt)])
    es.close()
    return nc, kb


_CACHE = {}
WNAMES = ["norm_pre_w", "norm_post_w", "w_in", "s5_A_re", "s5_A_im", "s5_log_dt", "s5_B_re", "s5_B_im", "s5_C_re", "s5_C_im",
          "s5_D", "s5_w_glu", "pool_w", "pool_scale", "sconv_w", "mlstm_conv_w", "mlstm_b_i", "mlstm_b_f", "mlstm_norm_w",
          "w_branch", "w_out"]


def run_layers(x, weights, ncores, en=(1, 1, 1, 1), dbg=False):
    B, S, _ = x.shape
    nseq = B // ncores
    NT = S // T
    Lw = weights["w_in"].shape[0]
    key = (nseq, NT, en, dbg)
    consts = host_consts()
    cur = np.ascontiguousarray(x, dtype=np.float32)
    dbg_out = None
    for l in range(Lw):
        nc, _ = build(1, nseq, NT, en, dbg)
        in_maps = []
        for c in range(ncores):
            m = {"x": cur[c * nseq:(c + 1) * nseq]}
            for k in WNAMES:
                m[k] = np.ascontiguousarray(weights[k][l:l + 1], dtype=np.float32)
            m.update(consts)
            in_maps.append(m)
        res = run_bass_kernel_spmd(nc, in_maps, core_ids=list(range(ncores)))
        cur = np.concatenate([r["y"] for r in res.results], axis=0)
        if dbg:
            dbg_out = res.results[0]["dbg"]
    return (cur, dbg_out) if dbg else cur


def run_fused(x, weights, ncores, dbg=False):
    B, S, _ = x.shape
    nseq = B // ncores
    NT = S // T
    Lw = weights["w_in"].shape[0]
    consts = host_consts()
    nc, _ = build(Lw, nseq, NT, (1, 1, 1, 1), dbg)
    xs = np.ascontiguousarray(x, dtype=np.float32)
    wts = {k: np.ascontiguousarray(weights[k], dtype=np.float32) for k in WNAMES}
    in_maps = []
    for c in range(ncores):
        m = {"x": xs[c * nseq:(c + 1) * nseq]}
        m.update(wts)
        m.update(consts)
        in_maps.append(m)
    res = run_bass_kernel_spmd(nc, in_maps, core_ids=list(range(ncores)))
    return np.concatenate([r["y"] for r in res.results], axis=0)


def kernel(**inputs):
    x = inputs["x"]
    weights = {k: inputs[k] for k in WNAMES}
    return run_fused(x, weights, 8).astype(np.float32)
```

```python
import math
import contextlib
import numpy as np
import concourse.bass as bass
import concourse.mybir as mybir
from concourse.bass_utils import run_bass_kernel_spmd

F32 = mybir.dt.float32
BF16 = mybir.dt.bfloat16
I32 = mybir.dt.int32
AF = mybir.ActivationFunctionType
ALU = mybir.AluOpType

SAME_SYNC = True
import os as _os
PIPE = int(_os.environ.get('KPIPE', '1'))
D = 1024
W = 512
NIN = 10760
T = 512
EPS = 1e-6
C_S5U, C_S5Z, C_PU, C_PZ, C_SX, C_SB, C_SC, C_SZ = 0, 512, 1024, 1536, 2048, 2560, 3072, 3584
C_Q, C_K, C_V, C_O, C_IF, C_MZ, C_G = 4096, 4608, 5120, 5632, 6144, 6152, 6664
POOL_WINDOWS = (2, 4, 8, 16)


class Tok:
    __slots__ = ("sem", "val")

    def __init__(self, sem, val):
        self.sem = sem
        self.val = val


class Buf:
    def __init__(self, name=""):
        self.name = name
        self.w = None
        self.r = {}


class DSem:
    def __init__(self, sem):
        self.sem = sem
        self.cnt = 0


class KB:
    def __init__(self, nc, es):
        self.nc = nc
        self.es = es
        self.eng = {"pe": nc.tensor, "act": nc.scalar, "dve": nc.vector, "pool": nc.gpsimd, "sp": nc.sync}
        self.sem = {e: es.enter_context(nc.semaphore("s_" + e)) for e in self.eng}
        self.cnt = {e: 0 for e in self.eng}
        self.seen = {e: {} for e in self.eng}
        self.nins = 0
        self.uid = 0

    def dsem(self, name):
        return DSem(self.es.enter_context(self.nc.semaphore(name)))

    def new_epoch(self, tag):
        self._keep = getattr(self, '_keep', []) + [self.sem]
        self.sem = {e: self.es.enter_context(self.nc.semaphore("s_" + e + tag)) for e in self.eng}
        self.cnt = {e: 0 for e in self.eng}

    def sb(self, name, shape, dt):
        return self.es.enter_context(self.nc.sbuf_tensor(name, list(shape), dt))

    def _wait(self, e, toks):
        need = {}
        for t in toks:
            if t is None:
                continue
            k = id(t.sem)
            if k not in need or need[k].val < t.val:
                need[k] = t
        for k, t in need.items():
            if t.sem is self.sem[e] and (e == "pe" or not SAME_SYNC):
                continue
            if self.seen[e].get(k, 0) >= t.val:
                continue
            self.eng[e].wait_ge(t.sem, t.val)
            self.seen[e][k] = t.val

    @staticmethod
    def _flat(lst):
        out = []
        for b in lst:
            if isinstance(b, (list, tuple)):
                out.extend(KB._flat(b))
            else:
                out.append(b)
        return out

    def _deps(self, reads, writes):
        reads = self._flat(reads); writes = self._flat(writes)
        toks = []
        for b in reads:
            toks.append(b.w)
        for b in writes:
            toks.append(b.w)
            toks.extend(b.r.values())
        return toks

    def _upd(self, tok, reads, writes):
        reads = self._flat(reads); writes = self._flat(writes)
        k = id(tok.sem)
        for b in reads:
            b.r[k] = tok
        for b in writes:
            b.w = tok
            b.r = {}

    def op(self, e, fn, reads=(), writes=()):
        self._wait(e, self._deps(reads, writes))
        ins = fn(self.eng[e])
        self.cnt[e] += 1
        ins.then_inc(self.sem[e], 1)
        tok = Tok(self.sem[e], self.cnt[e])
        self._upd(tok, reads, writes)
        self.nins += 1
        return tok

    def dma(self, e, ds, fn, reads=(), writes=()):
        self._wait(e, self._deps(reads, writes))
        ins = fn(self.eng[e])
        ds.cnt += 16
        ins.then_inc(ds.sem, 16)
        tok = Tok(ds.sem, ds.cnt)
        self._upd(tok, reads, writes)
        self.nins += 1
        return tok


def host_consts():
    ident = np.eye(128, dtype=np.float32)
    m = np.arange(128)[:, None]
    l = np.arange(128)[None, :]
    tri = (m <= l).astype(np.float32)
    ones = np.ones((128, 128), np.float32)
    band = np.zeros((3, 4, 128, 128), np.float32)
    for g, win in enumerate(POOL_WINDOWS):
        dlt = l - m
        inw = (dlt >= 0) & (dlt < win)
        band[0, g] = inw / win - ident
        dp = l + 128 - m
        band[1, g] = ((dp >= 0) & (dp < win)) / win
        band[2, g] = inw / np.minimum(l + 1, win) - ident
    return {"c_ident": ident, "c_tri": tri, "c_ones": ones,
            "c_band": np.ascontiguousarray(band.transpose(2, 0, 1, 3)).reshape(128, 12 * 128)}


def build(L, NSEQ, NT, en=(1, 1, 1, 1), dbg=False):
    S = NT * T
    nc = bass.Bass("TRN2", target_bir_lowering=False)
    es = contextlib.ExitStack()
    kb = KB(nc, es)

    def din(name, shape):
        return nc.dram_tensor(name, list(shape), F32, kind="ExternalInput").ap()

    x_d = din("x", [NSEQ, S, D])
    npw_d = din("norm_pre_w", [L, D]); ppw_d = din("norm_post_w", [L, D])
    win_d = din("w_in", [L, D, NIN])
    are_d = din("s5_A_re", [L, 32, 64]); aim_d = din("s5_A_im", [L, 32, 64]); ldt_d = din("s5_log_dt", [L, 32])
    bre_d = din("s5_B_re", [L, 32, 64, 16]); bim_d = din("s5_B_im", [L, 32, 64, 16])
    cre_d = din("s5_C_re", [L, 32, 16, 64]); cim_d = din("s5_C_im", [L, 32, 16, 64])
    s5d_d = din("s5_D", [L, W]); glu_d = din("s5_w_glu", [L, W, W])
    pw_d = din("pool_w", [L, 4, 128, 128]); psc_d = din("pool_scale", [L, W])
    scw_d = din("sconv_w", [L, 3, W]); mcw_d = din("mlstm_conv_w", [L, 4, 2 * W])
    mbi_d = din("mlstm_b_i", [L, 4]); mbf_d = din("mlstm_b_f", [L, 4]); mnw_d = din("mlstm_norm_w", [L, W])
    wbr_d = din("w_branch", [L, 4, W, D]); wo_d = din("w_out", [L, D, D])
    cid_d = din("c_ident", [128, 128]); ctri_d = din("c_tri", [128, 128]); cone_d = din("c_ones", [128, 128])
    cband_d = din("c_band", [128, 12 * 128])
    y_d = nc.dram_tensor("y", [NSEQ, S, D], F32, kind="ExternalOutput").ap()
    dbg_d = nc.dram_tensor("dbg", [4, 128, 4, T], F32, kind="ExternalOutput").ap() if dbg else None
    scr_d = [nc.dram_tensor("xscr%d" % i, [NSEQ, S, D], F32, kind="Internal").ap() for i in range(2)] if L > 1 else []
    dram_bufs = {}
    NIMG = 55
    SLW = 8 * 264
    wimg_d = [nc.dram_tensor("wimg%d" % l_, [NIMG, 128, SLW], BF16, kind="Internal").ap() for l_ in range(L)]
    Bimg = [Buf("wimg") for _ in range(L)]
    cvds = [None] * L
    img_index = {}

    def img_specs(l):
        sp = []

        def wv_(c0, n):
            return win_d[l, :, c0:c0 + n].rearrange("(k p) c -> p k c", p=128)
        for c0 in (C_S5U, C_S5Z, C_PU, C_PZ, C_SX, C_SB, C_SC, C_SZ, C_Q, C_K, C_V, C_O) + tuple(C_G + i * 512 for i in range(8)):
            for h in range(2):
                sp.append((("win", c0, h), 8, 256, wv_(c0 + 256 * h, 256)))
        sp.append((("win", C_IF, 0), 8, 264, wv_(C_IF, 264)))
        sp.append((("win", C_IF, 1), 8, 256, wv_(C_IF + 264, 256)))
        sp.append((("glu",), 4, 512, glu_d[l].rearrange("(k p) c -> p k c", p=128)))
        for b in range(4):
            for hf in range(2):
                sp.append((("wbr", b, hf), 4, 512, wbr_d[l, b, :, hf * 512:(hf + 1) * 512].rearrange("(k p) c -> p k c", p=128)))
        for hf in range(2):
            for q2 in range(2):
                c0 = hf * 512 + q2 * 256
                sp.append((("wo", hf, q2), 8, 256, wo_d[l, :, c0:c0 + 256].rearrange("(k p) c -> p k c", p=128)))
        return sp

    def convert_layer(l):
        cvds[l] = kb.dsem("cvds%d" % l)
        for i, (key, k, c, src) in enumerate(img_specs(l)):
            img_index[(l,) + key] = (i, k, c)
            for kk in range(k):
                dst = wimg_d[l][i, :, kk * c:(kk + 1) * c]
                kb.dma("pool", cvds[l], lambda q, dst=dst, src=src, kk=kk: q.dma_start(out=dst, in_=src[:, kk, :]), writes=[Bimg[l]])
        Bimg[l].w = Tok(cvds[l].sem, cvds[l].cnt)

    def load_img(l, key):
        i, k, c = img_index[(l,) + key]
        si = wsi[0] % NSL
        wsi[0] += 1
        slot, Bs = wsl[si]
        kb.dma("sp", wds[si], lambda q: q.dma_start(out=slot[:, 0:k * c], in_=wimg_d[l][i, :, 0:k * c]), reads=[Bimg[l]], writes=[Bs])
        return slot[:, 0:k * c].rearrange("p (k c) -> p k c", k=k), Bs

    def dbuf(key):
        if key not in dram_bufs:
            dram_bufs[key] = Buf("dram")
        return dram_bufs[key]

    uid = [0]

    def sbt(shape, dt, name=None):
        uid[0] += 1
        return kb.sb((name or "t") + str(uid[0]), shape, dt), Buf(name or "t")
    sbt_global = sbt

    NPS = 8
    psb = [es.enter_context(nc.psum_tensor("ps%d" % i, [128, 512], F32)) for i in range(NPS)]
    psB = [Buf("ps%d" % i) for i in range(NPS)]
    psi = [0]

    def nextps():
        i = psi[0] % NPS
        psi[0] += 1
        return psb[i], psB[i]

    def E(e, fn, r=(), w=()):
        return kb.op(e, fn, r, w)

    cdsem = kb.dsem("cdsem")

    cbufs = []

    def cbarrier():
        for b_ in cbufs:
            b_.w = Tok(cdsem.sem, cdsem.cnt)
        del cbufs[:]

    def cload(e, out_ap, in_ap, wb, slow=False):
        cbufs.append(wb)
        return kb.dma(e, cdsem, lambda q: q.dma_start(out=out_ap, in_=in_ap, allow_slow_non_contiguous=True) if slow
                      else q.dma_start(out=out_ap, in_=in_ap), writes=[wb])

    identf, Bidf = sbt([128, 128], F32, "identf")
    identb, Bidb = sbt([128, 128], BF16, "identb")
    trif, Btrif = sbt([128, 128], F32, "trif")
    onesf, Bonesf = sbt([128, 128], F32, "onesf")
    band, Bband = sbt([128, 3, 4, 128], BF16, "band")
    cload("sp", identf[:], cid_d, Bidf)
    cload("pool", identb[:], cid_d, Bidb)
    cload("sp", trif[:], ctri_d, Btrif)
    cload("sp", onesf[:], cone_d, Bonesf)
    cload("pool", band[:].rearrange("p a g l -> p (a g l)"), cband_d, Bband)
    cbarrier()

    xt = [sbt([128, 4, D], F32, "xt") for _ in range(1)]
    xds = [kb.dsem("xds%d" % i) for i in range(1)]
    ods = kb.dsem("ods")
    hT, BhT = sbt([128, 8, T], BF16, "hT")
    st4, Bst4 = sbt([128, 16], F32, "st4")
    NSL = 6
    wsl = [sbt([128, SLW], BF16, "wsl") for _ in range(NSL)]
    wds = [kb.dsem("wds%d" % i) for i in range(NSL)]
    wsi = [0]

    def load_w(views):
        i = wsi[0] % NSL
        wsi[0] += 1
        slot, Bs = wsl[i]
        for (off, kk, cc, src) in views:
            dst = slot[:, off:off + kk * cc].rearrange("p (k c) -> p k c", k=kk)
            kb.dma("pool", wds[i], lambda q, dst=dst, src=src: q.dma_start(out=dst, in_=src), writes=[Bs])
        return slot, Bs

    def win_view(l, c0, n):
        return win_d[l, :, c0:c0 + n].rearrange("(k p) c -> p k c", p=128)

    def load_win(l, c0, n=512):
        return [load_img(l, ("win", c0, h)) for h in range(2)], None

    def proj_fm(wg, _, jb):
        wv, Bw = wg[jb // 2]
        jj_ = jb % 2
        ps, Bp = nextps()
        for kc in range(8):
            E("pe", lambda e, kc=kc: e.matmul(ps[:], wv[:, kc, jj_ * 128:(jj_ + 1) * 128], hT[:, kc, :], start=(kc == 0), stop=(kc == 7)),
              [Bw, BhT], [Bp])
        return ps, Bp

    def proj_tm(wg, _, tb, c0, n):
        ps, Bp = nextps()
        if n == 8:
            parts = [(0, 0, 8, 0)]
        elif c0 == 8:
            parts = [(0, 8, 256, 0), (1, 0, 256, 256)]
        else:
            parts = [(0, 0, 256, 0), (1, 0, 256, 256)]
        for (h, wc, nn, pc) in parts:
            wv, Bw = wg[h]
            for kc in range(8):
                E("pe", lambda e, kc=kc, wv=wv: e.matmul(ps[:, pc:pc + nn], hT[:, kc, tb * 128:(tb + 1) * 128], wv[:, kc, wc:wc + nn], start=(kc == 0), stop=(kc == 7)),
                  [Bw, BhT], [Bp])
        return ps, Bp

    yb = [sbt([128, 4, T], BF16, "yb") for _ in range(4)]
    SC = [sbt([128, 4, T + 4], F32, "scr") for _ in range(4)]

    def scflat(i):
        return SC[i][0][:].rearrange("p a b -> p (a b)")

    class _V0:
        def __init__(self, ap):
            self.ap = ap

        def __getitem__(self, k):
            return self.ap[k]
    junk, Bjunk = _V0(scflat(2)[:, 0:D]), SC[2][1]

    def sc_half(i, h):
        return scflat(i)[:, h * 1024:(h + 1) * 1024].rearrange("p (g c) -> p g c", g=16)
    qkT, _bq = sbt([128, 8, T], BF16, "qkT")
    BqkT = [Buf("mrg%d" % i) for i in range(4)]
    mrgT, BmrgT = qkT, BqkT
    gsb = [(_V0(SC[3][0][:, 0, 0:T]), SC[3][1])]
    tmpm = [(_V0(SC[3][0][:, 1, 0:T]), SC[3][1])]

    npw, Bnpw = sbt([128, D], F32, "npw"); ppw, Bppw = sbt([128, D], F32, "ppw")
    scw, Bscw = sbt([128, 3, 4], F32, "scw"); mcw, Bmcw = sbt([128, 4, 8], F32, "mcw")
    psc, Bpsc = sbt([128, 4], F32, "psc"); s5dd, Bs5dd = sbt([128, 4], F32, "s5dd")
    mnw, Bmnw = sbt([128, W], F32, "mnw"); mbi, Bmbi = sbt([128, 4], F32, "mbi"); mbf, Bmbf = sbt([128, 4], F32, "mbf")
    poolw, Bpoolw = sbt([128, 4, 128], BF16, "poolw")
    schalo, Bschalo = sbt([128, 4, 2], F32, "schalo")
    putok, Bputok = sbt([128, 5, W], BF16, "putok")
    mlhalo, Bmlhalo = sbt([128, 8, 3], F32, "mlhalo")
    C32, BC32 = sbt([128, 4, 129], F32, "C32"); Cbf, BCbf = sbt([128, 4, 129], BF16, "Cbf")
    car = [sbt([128, 16], F32, "car") for _ in range(2)]
    Min, BMin = sbt([128, 4, 8, 2, 128], BF16, "Min")
    Mout, BMout = sbt([128, 16, 8, 2, 32], BF16, "Mout")
    Ktoe, BKtoe = sbt([128, 4, 8, 128], BF16, "Ktoe")
    Etab = [sbt([128, 16, 64], F32, "Etab") for _ in range(2)]
    rho, Brho = sbt([128, 16], F32, "rho")

    class _V:
        def __init__(self, ap):
            self.ap = ap

        def __getitem__(self, k):
            return self.ap[k]
    Zpad = [(_V(scflat(i)[:, 0:2048].rearrange("p (g c) -> p g c", g=16)), SC[i][1]) for i in range(2)]
    Cpad = [(_V(scflat(i)[:, 0:2048].rearrange("p (g c) -> p g c", g=16)), SC[i][1]) for i in range(2, 4)]
    E("dve", lambda e: e.memset(Mout[:], 0.0), w=[BMout])
    qT, BqT = _V(qkT[:, 0:4, :]), BqkT; kT, BkT = _V(qkT[:, 4:8, :]), BqkT
    vtok, Bvtok = sbt([128, 4, 4, 129], BF16, "vtok")
    E("dve", lambda e: e.memset(vtok[:], 1.0), w=[Bvtok])
    ogt, Bogt = sbt([128, 4, W], BF16, "ogt"); zwt, Bzwt = sbt([128, 4, W], BF16, "zwt")
    gt, Bgt = sbt([128, 4, 40], F32, "gt")
    ktil, Bktil = sbt([128, 128], BF16, "ktil"); PT, BPT = sbt([128, 128], BF16, "PT")
    hh, Bhh = sbt([128, 4, 128], F32, "hh"); ydtok, Bydtok = sbt([128, W], BF16, "ydtok")
    sm, Bsm = sbt([128, 16], F32, "sm")
    uT, BuT = sbt([128, 4, T], BF16, "uT"); u32, Bu32 = uT, BuT
    s5t = [(_V(sc_half(0, h)), SC[0][1]) for h in range(2)]
    Hs = [(_V(sc_half(1, h)), SC[1][1]) for h in range(2)]
    Xr = [(_V(sc_half(2, h)), SC[2][1]) for h in range(2)]
    Hu = Xr
    HpT, BHpT = sbt([128, 2, 16, 64], BF16, "HpT")
    Hp = [(_V(HpT[:, c]), BHpT) for c in range(2)]
    pooledT, BpooledT = _V(HpT[:].rearrange("p a g c -> p (a g c)").rearrange("p (g t) -> p g t", g=4)), BHpT

    def mul(e, o, a, b, r, w):
        return E(e, lambda q: q.tensor_tensor(out=o, in0=a, in1=b, op=ALU.mult), r, w)

    def tt(e, o, a, b, op, r, w):
        return E(e, lambda q: q.tensor_tensor(out=o, in0=a, in1=b, op=op), r, w)

    prep_cache = {}

    def layer_prep(l):
        pidx = [0]

        def sbt(shape, dt, name=None):
            pidx[0] += 1
            k = (name, pidx[0])
            if k not in prep_cache:
                prep_cache[k] = sbt_global(shape, dt, name)
            return prep_cache[k]

        def bc(v):
            return v.partition_broadcast(128)
        cload("sp", npw[:], bc(npw_d[l]), Bnpw); cload("sp", ppw[:], bc(ppw_d[l]), Bppw)
        cload("sp", scw[:], scw_d[l].rearrange("k (cb p) -> p k cb", p=128), Bscw, slow=True)
        cload("sp", mcw[:], mcw_d[l].rearrange("k (cb p) -> p k cb", p=128), Bmcw, slow=True)
        cload("sp", psc[:], psc_d[l].rearrange("(cb p) -> p cb", p=128), Bpsc, slow=True)
        cload("sp", s5dd[:], s5d_d[l].rearrange("(cb p) -> p cb", p=128), Bs5dd, slow=True)
        cload("sp", mnw[:], bc(mnw_d[l]), Bmnw); cload("sp", mbi[:], bc(mbi_d[l]), Bmbi); cload("sp", mbf[:], bc(mbf_d[l]), Bmbf)
        cload("pool", poolw[:], pw_d[l].rearrange("g c d -> c g d"), Bpoolw)
        if not en[0]:
            cbarrier()
            return
        for tt_, bb_ in Zpad + Cpad:
            E("dve", lambda e, tt_=tt_: e.memset(tt_[:], 0.0), w=[bb_])
        def arena(i):
            return yb[i][0][:].rearrange("p a b -> p (a b)").bitcast(F32)

        def carve(i, off, shp):
            n = shp[0] * shp[1]
            return (_V(arena(i)[:, off:off + n].rearrange("p (a b) -> p a b", a=shp[0])), yb[i][1])
        A = [sbt([128, 16], F32, "A") for _ in range(2)]
        ldt, Bldt = sbt([128, 16], F32, "ldt")
        Bm = [carve(0, c * 256, (16, 16)) for c in range(2)]
        Cm = [carve(0, 512 + c * 256, (16, 16)) for c in range(2)]
        cload("sp", A[0][0][:], are_d[l].rearrange("(gp gl) n -> (gl n) gp", gl=2), A[0][1], slow=True)
        cload("sp", A[1][0][:], aim_d[l].rearrange("(gp gl) n -> (gl n) gp", gl=2), A[1][1], slow=True)
        for gl in range(2):
            cload("sp", ldt[gl * 64:(gl + 1) * 64, :], ldt_d[l, gl::2].partition_broadcast(64), Bldt, slow=True)
        for c, src in enumerate((bre_d, bim_d)):
            cload("sp", Bm[c][0][:], src[l].rearrange("(gp gl) n q -> (gl n) gp q", gl=2), Bm[c][1], slow=True)
        for c, src in enumerate((cre_d, cim_d)):
            for gl in range(2):
                for gp in range(16):
                    cload("sp", Cm[c][0][gl * 64:(gl + 1) * 64, gp, :], src[l, 2 * gp + gl].rearrange("p n -> n p"), Cm[c][1], slow=True)
        cbarrier()
        tn = [sbt([128, 16], F32, "tn") for _ in range(12)]

        def V(i):
            return tn[i][0][:]

        def Bv(i):
            return tn[i][1]
        Are, Aim = A[0][0][:], A[1][0][:]
        BA = [A[0][1], A[1][1]]
        E("act", lambda e: e.activation(out=ldt[:], in_=ldt[:], func=AF.Exp), [Bldt], [Bldt])
        mul("dve", V(0), Are, ldt[:], [BA[0], Bldt], [Bv(0)])
        mul("dve", V(1), Aim, ldt[:], [BA[1], Bldt], [Bv(1)])
        E("act", lambda e: e.activation(out=V(2), in_=V(0), func=AF.Exp), [Bv(0)], [Bv(2)])
        E("act", lambda e: e.activation(out=V(11), in_=V(0), func=AF.Exp, scale=8.0), [Bv(0)], [Bv(11)])
        ki, Bki = sbt([128, 16], I32, "ki")
        E("dve", lambda e: e.tensor_scalar(out=V(3), in0=V(1), scalar1=1.0 / (2 * math.pi), scalar2=None, op0=ALU.mult), [Bv(1)], [Bv(3)])
        E("dve", lambda e: e.tensor_copy(out=ki[:], in_=V(3)), [Bv(3)], [Bki])
        E("dve", lambda e: e.tensor_copy(out=V(3), in_=ki[:]), [Bki], [Bv(3)])
        E("dve", lambda e: e.scalar_tensor_tensor(out=V(4), in0=V(3), scalar=-2 * math.pi, in1=V(1), op0=ALU.mult, op1=ALU.add), [Bv(3), Bv(1)], [Bv(4)])
        E("act", lambda e: e.activation(out=V(5), in_=V(4), func=AF.Sin, scale=0.5), [Bv(4)], [Bv(5)])
        E("act", lambda e: e.activation(out=V(6), in_=V(4), func=AF.Sin, scale=0.25), [Bv(4)], [Bv(6)])
        mul("dve", V(7), V(6), V(6), [Bv(6)], [Bv(7)])
        E("dve", lambda e: e.tensor_scalar(out=V(7), in0=V(7), scalar1=-2.0, scalar2=1.0, op0=ALU.mult, op1=ALU.add), [Bv(7)], [Bv(7)])
        mul("dve", V(8), V(5), V(7), [Bv(5), Bv(7)], [Bv(8)])
        mul("dve", V(5), V(5), V(5), [Bv(5)], [Bv(5)])
        E("dve", lambda e: e.tensor_scalar(out=V(5), in0=V(5), scalar1=-2.0, scalar2=1.0, op0=ALU.mult, op1=ALU.add), [Bv(5)], [Bv(5)])
        Pw = [sbt([128, 9, 16], F32, "Pw") for _ in range(2)]
        Pr, Pi = Pw[0][0], Pw[1][0]
        BP = [Pw[0][1], Pw[1][1]]
        E("dve", lambda e: e.memset(Pr[:, 0, :], 1.0), w=[BP[0]])
        E("dve", lambda e: e.memset(Pi[:, 0, :], 0.0), w=[BP[1]])
        mul("dve", Pr[:, 1, :], V(2), V(5), [Bv(2), Bv(5)], [BP[0]])
        E("dve", lambda e: e.scalar_tensor_tensor(out=Pi[:, 1, :], in0=V(8), scalar=2.0, in1=V(2), op0=ALU.mult, op1=ALU.mult), [Bv(8), Bv(2)], [BP[1]])

        def cmul(o_r, o_i, xr, xi, yr, yi, r, w, t1, t2, Bt):
            mul("dve", t1, xr, yr, r, Bt); mul("dve", t2, xi, yi, r, Bt)
            tt("dve", o_r, t1, t2, ALU.subtract, Bt, [w[0]])
            mul("dve", t1, xr, yi, r, Bt); mul("dve", t2, xi, yr, r, Bt)
            tt("dve", o_i, t1, t2, ALU.add, Bt, [w[1]])
        for j in range(1, 8):
            cmul(Pr[:, j + 1, :], Pi[:, j + 1, :], Pr[:, j, :], Pi[:, j, :], Pr[:, 1, :], Pi[:, 1, :], BP, BP, V(9), V(10), [Bv(9), Bv(10)])
        E("dve", lambda e: e.reciprocal(out=V(0), in_=V(11)), [Bv(11)], [Bv(0)])
        wl = [sbt([128, 16], F32, "wl") for _ in range(4)]
        mul("dve", wl[0][0][:], Pr[:, 8, :], V(0), [BP[0], Bv(0)], [wl[0][1]])
        E("dve", lambda e: e.scalar_tensor_tensor(out=wl[1][0][:], in0=Pi[:, 8, :], scalar=-1.0, in1=V(0), op0=ALU.mult, op1=ALU.mult), [BP[1], Bv(0)], [wl[1][1]])
        Er, Ei = Etab[0][0], Etab[1][0]
        BE = [Etab[0][1], Etab[1][1]]
        E("dve", lambda e: e.tensor_copy(out=Er[:, :, 0], in_=wl[0][0][:]), [wl[0][1]], [BE[0]])
        E("dve", lambda e: e.tensor_copy(out=Ei[:, :, 0], in_=wl[1][0][:]), [wl[1][1]], [BE[1]])
        t3a, Bt3a = carve(2, 0, (16, 32)); t3b, Bt3b = carve(2, 512, (16, 32))
        cur = 0
        Lw = 1
        while Lw < 64:
            wr = wl[cur][0][:].unsqueeze(2).to_broadcast([128, 16, Lw]); wi = wl[cur + 1][0][:].unsqueeze(2).to_broadcast([128, 16, Lw])
            Bw = [wl[cur][1], wl[cur + 1][1]]
            cmul(Er[:, :, Lw:2 * Lw], Ei[:, :, Lw:2 * Lw], Er[:, :, 0:Lw], Ei[:, :, 0:Lw], wr, wi, BE + Bw, BE, t3a[:, :, 0:Lw], t3b[:, :, 0:Lw], [Bt3a, Bt3b])
            nxt = 2 - cur
            cmul(wl[nxt][0][:], wl[nxt + 1][0][:], wl[cur][0][:], wl[cur + 1][0][:], wl[cur][0][:], wl[cur + 1][0][:], Bw, [wl[nxt][1], wl[nxt + 1][1]], V(9), V(10), [Bv(9), Bv(10)])
            cur = nxt
            Lw *= 2
        E("dve", lambda e: e.tensor_copy(out=rho[:], in_=V(11)), [Bv(11)], [Brho])
        E("dve", lambda e: e.tensor_scalar(out=V(0), in0=Pr[:, 1, :], scalar1=-1.0, scalar2=None, op0=ALU.add), [BP[0]], [Bv(0)])
        mul("dve", V(1), Are, Are, [BA[0]], [Bv(1)]); mul("dve", V(2), Aim, Aim, [BA[1]], [Bv(2)])
        tt("dve", V(1), V(1), V(2), ALU.add, [Bv(1), Bv(2)], [Bv(1)])
        E("dve", lambda e: e.reciprocal(out=V(1), in_=V(1)), [Bv(1)], [Bv(1)])
        mul("dve", V(2), V(0), Are, [Bv(0), BA[0]], [Bv(2)]); mul("dve", V(3), Pi[:, 1, :], Aim, [BP[1], BA[1]], [Bv(3)])
        tt("dve", V(2), V(2), V(3), ALU.add, [Bv(2), Bv(3)], [Bv(2)]); mul("dve", V(4), V(2), V(1), [Bv(2), Bv(1)], [Bv(4)])
        mul("dve", V(2), Pi[:, 1, :], Are, [BP[1], BA[0]], [Bv(2)]); mul("dve", V(3), V(0), Aim, [Bv(0), BA[1]], [Bv(3)])
        tt("dve", V(2), V(2), V(3), ALU.subtract, [Bv(2), Bv(3)], [Bv(2)]); mul("dve", V(5), V(2), V(1), [Bv(2), Bv(1)], [Bv(5)])
        Bb = [carve(1, c * 256, (16, 16)) for c in range(2)]
        Zz = [carve(1, 512 + c * 256, (16, 16)) for c in range(2)]
        t4a, Bt4a = carve(3, 0, (16, 16)); t4b, Bt4b = carve(3, 256, (16, 16))

        def b16(v):
            return v.unsqueeze(2).to_broadcast([128, 16, 16])
        cmul(Bb[0][0][:], Bb[1][0][:], b16(V(4)), b16(V(5)), Bm[0][0][:], Bm[1][0][:], [Bv(4), Bv(5), Bm[0][1], Bm[1][1]], [Bb[0][1], Bb[1][1]], t4a[:], t4b[:], [Bt4a, Bt4b])
        E("dve", lambda e: e.tensor_scalar(out=t4a[:], in0=Cm[1][0][:], scalar1=-1.0, scalar2=None, op0=ALU.mult), [Cm[1][1]], [Bt4a])
        csrc = [(Cm[0][0], Cm[0][1]), (t4a, Bt4a)]

        def padfill(dst, Bd, src, Bs):
            d5 = dst[:].rearrange("p (gq j) (jj c) -> p gq j jj c", j=4, jj=4)
            s4 = src[:].rearrange("p (gq j) q -> p gq j q", j=4)
            for j in range(4):
                for gl in range(2):
                    E("act", lambda e, j=j, gl=gl: e.copy(out=d5[gl * 64:(gl + 1) * 64, :, j, j, gl * 16:(gl + 1) * 16], in_=s4[gl * 64:(gl + 1) * 64, :, j, :]), [Bs], [Bd])
        for c in range(2):
            padfill(Cpad[c][0], Cpad[c][1], csrc[c][0], csrc[c][1])
        for jp in range(8):
            cmul(Zz[0][0][:], Zz[1][0][:], b16(Pr[:, jp, :]), b16(Pi[:, jp, :]), Bb[0][0][:], Bb[1][0][:], BP + [Bb[0][1], Bb[1][1]], [Zz[0][1], Zz[1][1]], t4a[:], t4b[:], [Bt4a, Bt4b])
            for c in range(2):
                padfill(Zpad[c][0], Zpad[c][1], Zz[c][0], Zz[c][1])
            s = 7 - jp
            for gq in range(4):
                for c in range(2):
                    ps, Bp = nextps()
                    for j in range(4):
                        E("pe", lambda e, j=j, c=c, gq=gq: e.matmul(ps[:, 0:128], Zpad[c][0][:, 4 * gq + j, :], identf[:], start=(j == 0), stop=(j == 3)), [Zpad[c][1], Bidf], [Bp])
                    E("dve", lambda e, c=c, gq=gq, ps=ps: e.tensor_copy(out=Min[:, gq, s, c, :], in_=ps[:, 0:128]), [Bp], [BMin])
                ps, Bp = nextps()
                for j in range(4):
                    for c in range(2):
                        E("pe", lambda e, j=j, c=c, gq=gq: e.matmul(ps[:, 0:128], Zpad[c][0][:, 4 * gq + j, :], Cpad[c][0][:, 4 * gq + j, :], start=(j == 0 and c == 0), stop=(j == 3 and c == 1)),
                          [Zpad[c][1], Cpad[c][1]], [Bp])
                E("dve", lambda e, gq=gq, ps=ps: e.tensor_copy(out=Ktoe[:, gq, jp, :], in_=ps[:, 0:128]), [Bp], [BKtoe])
        for t in range(8):
            cmul(Zz[0][0][:], Zz[1][0][:], b16(Pr[:, t + 1, :]), b16(Pi[:, t + 1, :]), Cm[0][0][:], Cm[1][0][:], BP + [Cm[0][1], Cm[1][1]], [Zz[0][1], Zz[1][1]], t4a[:], t4b[:], [Bt4a, Bt4b])
            for gl in range(2):
                E("dve", lambda e, gl=gl, t=t: e.tensor_copy(out=Mout[gl * 64:(gl + 1) * 64, :, t, 0, gl * 16:(gl + 1) * 16], in_=Zz[0][0][gl * 64:(gl + 1) * 64]), [Zz[0][1]], [BMout])
                E("dve", lambda e, gl=gl, t=t: e.tensor_scalar(out=Mout[gl * 64:(gl + 1) * 64, :, t, 1, gl * 16:(gl + 1) * 16], in0=Zz[1][0][gl * 64:(gl + 1) * 64], scalar1=-1.0, scalar2=None, op0=ALU.mult), [Zz[1][1]], [BMout])

    def seq_reset():
        E("dve", lambda e: e.memset(schalo[:], 0.0), w=[Bschalo])
        E("dve", lambda e: e.memset(putok[:, 0, :], 0.0), w=[Bputok])
        E("dve", lambda e: e.memset(mlhalo[:], 0.0), w=[Bmlhalo])
        E("dve", lambda e: e.memset(C32[:], 0.0), w=[BC32])
        E("dve", lambda e: e.memset(Cbf[:], 0.0), w=[BCbf])
        for c in range(2):
            E("dve", lambda e, c=c: e.memset(car[c][0][:], 0.0), w=[car[c][1]])

    def branch_sconv(l):
        xs, Bxs = SC[0]; prod, Bprod = SC[1]; acc, Bacc = SC[2]; zs, Bzs = SC[3]
        wv, Bw = load_win(l, C_SX)
        for cb in range(4):
            ps, Bp = proj_fm(wv, Bw, cb)
            E("act", lambda e, cb=cb, ps=ps: e.copy(out=xs[:, cb, 0:T], in_=ps[:]), [Bp], [Bxs])
        E("dve", lambda e: e.tensor_copy(out=prod[:, :, 0:2], in_=schalo[:]), [Bschalo], [Bprod])
        wv, Bw = load_win(l, C_SC)
        for cb in range(4):
            ps, Bp = proj_fm(wv, Bw, cb)
            mul("dve", prod[:, cb, 2:T + 2], ps[:], xs[:, cb, 0:T], [Bp, Bxs], [Bprod])
        E("dve", lambda e: e.tensor_copy(out=schalo[:], in_=prod[:, :, T:T + 2]), [Bprod], [Bschalo])
        for cb in range(4):
            E("dve", lambda e, cb=cb: e.tensor_scalar(out=acc[:, cb, 0:T], in0=prod[:, cb, 2:T + 2], scalar1=scw[:, 2, cb:cb + 1], scalar2=None, op0=ALU.mult), [Bprod, Bscw], [Bacc])
            for k in (1, 0):
                E("dve", lambda e, cb=cb, k=k: e.scalar_tensor_tensor(out=acc[:, cb, 0:T], in0=prod[:, cb, k:k + T], scalar=scw[:, k, cb:cb + 1], in1=acc[:, cb, 0:T], op0=ALU.mult, op1=ALU.add),
                  [Bprod, Bscw, Bacc], [Bacc])
        wv, Bw = load_win(l, C_SB)
        for cb in range(4):
            ps, Bp = proj_fm(wv, Bw, cb)
            mul("dve", acc[:, cb, 0:T], ps[:], acc[:, cb, 0:T], [Bp, Bacc], [Bacc])
        wv, Bw = load_win(l, C_SZ)
        for cb in range(4):
            ps, Bp = proj_fm(wv, Bw, cb)
            E("act", lambda e, cb=cb, ps=ps: e.activation(out=zs[:, cb, 0:T], in_=ps[:], func=AF.Silu), [Bp], [Bzs])
            mul("dve", yb[2][0][:, cb, :], acc[:, cb, 0:T], zs[:, cb, 0:T], [Bacc, Bzs], [yb[2][1]])

    def branch_pool(l, first):
        import os
        stage = int(os.environ.get("POOLDBG", "9"))
        zs, Bzs = SC[3]
        wv, Bw = load_win(l, C_PU)
        for tb in range(4):
            if stage == -2:
                continue
            ps, Bp = proj_tm(wv, Bw, tb, 0, 512)
            if stage == -1:
                continue
            E("act", lambda e, tb=tb, ps=ps: e.activation(out=putok[:, tb + 1, :], in_=ps[:], func=AF.Identity), [Bp], [Bputok])
        if stage < 1:
            E("dve", lambda e: e.memset(yb[1][0][:], 0.0), w=[yb[1][1]])
            return
        for g in range(4):
            ps, Bp = nextps()
            for tb in range(4):
                f0 = first and tb == 0
                E("pe", lambda e, g=g, tb=tb, ps=ps, f0=f0: e.matmul(ps[:, tb * 128:(tb + 1) * 128], putok[:, tb + 1, g * 128:(g + 1) * 128], band[:, 2 if f0 else 0, g, :], start=True, stop=f0),
                  [Bputok, Bband], [Bp])
                if not f0:
                    E("pe", lambda e, g=g, tb=tb, ps=ps: e.matmul(ps[:, tb * 128:(tb + 1) * 128], putok[:, tb, g * 128:(g + 1) * 128], band[:, 1, g, :], start=False, stop=True),
                      [Bputok, Bband], [Bp])
            E("dve", lambda e, g=g, ps=ps: e.tensor_copy(out=pooledT[:, g, :], in_=ps[:]), [Bp], [BpooledT])
        E("dve", lambda e: e.tensor_copy(out=putok[:, 0, :], in_=putok[:, 4, :]), [Bputok], [Bputok])
        if stage < 2:
            E("dve", lambda e: e.memset(yb[1][0][:], 0.0), w=[yb[1][1]])
            return
        wv, Bw = load_win(l, C_PZ)
        for g in range(4):
            ps, Bp = proj_fm(wv, Bw, g)
            E("act", lambda e, g=g, ps=ps: e.activation(out=zs[:, g, 0:T], in_=ps[:], func=AF.Silu), [Bp], [Bzs])
            ps2, Bp2 = nextps()
            E("pe", lambda e, g=g, ps2=ps2: e.matmul(ps2[:], poolw[:, g, :], pooledT[:, g, :], start=True, stop=True), [Bpoolw, BpooledT], [Bp2])
            E("dve", lambda e, g=g, ps2=ps2: e.scalar_tensor_tensor(out=yb[1][0][:, g, :], in0=ps2[:], scalar=psc[:, g:g + 1], in1=zs[:, g, 0:T], op0=ALU.mult, op1=ALU.mult),
              [Bp2, Bpsc, Bzs], [yb[1][1]])

    def branch_mlstm(l):
        cq, Bcq = SC[0]; ca, Bca = SC[1]
        for qi, (c0, dst, Bd) in enumerate(((C_Q, qT, BqT), (C_K, kT, BkT))):
            wv, Bw = load_win(l, c0)
            E("dve", lambda e, qi=qi: e.tensor_copy(out=cq[:, :, 0:3], in_=mlhalo[:, qi * 4:(qi + 1) * 4, :]), [Bmlhalo], [Bcq])
            for cb in range(4):
                ps, Bp = proj_fm(wv, Bw, cb)
                E("act", lambda e, cb=cb, ps=ps: e.copy(out=cq[:, cb, 3:T + 3], in_=ps[:]), [Bp], [Bcq])
            E("dve", lambda e, qi=qi: e.tensor_copy(out=mlhalo[:, qi * 4:(qi + 1) * 4, :], in_=cq[:, :, T:T + 3]), [Bcq], [Bmlhalo])
            for cb in range(4):
                ch = qi * 4 + cb
                E("dve", lambda e, cb=cb, ch=ch: e.tensor_scalar(out=ca[:, cb, 0:T], in0=cq[:, cb, 3:T + 3], scalar1=mcw[:, 3, ch:ch + 1], scalar2=None, op0=ALU.mult), [Bcq, Bmcw], [Bca])
                for k in (2, 1, 0):
                    E("dve", lambda e, cb=cb, ch=ch, k=k: e.scalar_tensor_tensor(out=ca[:, cb, 0:T], in0=cq[:, cb, k:k + T], scalar=mcw[:, k, ch:ch + 1], in1=ca[:, cb, 0:T], op0=ALU.mult, op1=ALU.add),
                      [Bcq, Bmcw, Bca], [Bca])
                E("act", lambda e, cb=cb, dst=dst: e.activation(out=dst[:, cb, :], in_=ca[:, cb, 0:T], func=AF.Silu), [Bca], [Bd])
        wv, Bw = load_win(l, C_V)
        for tb in range(4):
            ps, Bp = proj_tm(wv, Bw, tb, 0, 512)
            E("act", lambda e, tb=tb, ps=ps: e.activation(out=vtok[:, tb, :, 0:128], in_=ps[:].rearrange("p (h d) -> p h d", h=4), func=AF.Identity), [Bp], [Bvtok])
        wv, Bw = load_win(l, C_O)
        for tb in range(4):
            ps, Bp = proj_tm(wv, Bw, tb, 0, 512)
            E("act", lambda e, tb=tb, ps=ps: e.activation(out=ogt[:, tb, :], in_=ps[:], func=AF.Sigmoid), [Bp], [Bogt])
        wv, Bw = load_win(l, C_IF, 520)
        psgs = []
        for tb in range(4):
            psgs.append(proj_tm(wv, Bw, tb, 0, 8))
        for tb in range(4):
            ps, Bp = proj_tm(wv, Bw, tb, 8, 512)
            E("act", lambda e, tb=tb, ps=ps: e.activation(out=zwt[:, tb, :], in_=ps[:], func=AF.Silu), [Bp], [Bzwt])
            mul("dve", zwt[:, tb, :], zwt[:, tb, :], mnw[:], [Bzwt, Bmnw], [Bzwt])
        for tb in range(4):
            psg, Bpg = psgs[tb]
            G = gt[:, tb, :]
            tt("dve", G[:, 0:4], psg[:, 0:4], mbi[:], ALU.add, [Bpg, Bmbi], [Bgt])
            tt("dve", G[:, 4:8], psg[:, 4:8], mbf[:], ALU.add, [Bpg, Bmbf], [Bgt])
            E("act", lambda e, G=G: e.activation(out=G[:, 4:8], in_=G[:, 4:8], func=AF.Exp, scale=-1.0), [Bgt], [Bgt])
            E("act", lambda e, G=G: e.activation(out=G[:, 4:8], in_=G[:, 4:8], func=AF.Ln, bias=1.0), [Bgt], [Bgt])
            ps2, Bp2 = nextps()
            E("pe", lambda e, G=G, ps2=ps2: e.matmul(ps2[:, 0:4], trif[:], G[:, 4:8], start=True, stop=True), [Btrif, Bgt], [Bp2])
            E("pe", lambda e, G=G, ps2=ps2: e.matmul(ps2[:, 4:8], onesf[:], G[:, 4:8], start=True, stop=True), [Bonesf, Bgt], [Bp2])
            E("act", lambda e, G=G, ps2=ps2: e.activation(out=G[:, 8:12], in_=ps2[:, 0:4], func=AF.Exp, scale=-1.0, bias=math.log(128 ** -0.5)), [Bp2], [Bgt])
            tt("dve", G[:, 24:28], G[:, 0:4], ps2[:, 0:4], ALU.add, [Bgt, Bp2], [Bgt])
            E("act", lambda e, G=G: e.activation(out=G[:, 12:16], in_=G[:, 24:28], func=AF.Exp), [Bgt], [Bgt])
            tt("dve", G[:, 24:28], G[:, 24:28], ps2[:, 4:8], ALU.subtract, [Bgt, Bp2], [Bgt])
            E("act", lambda e, G=G: e.activation(out=G[:, 16:20], in_=G[:, 24:28], func=AF.Exp), [Bgt], [Bgt])
            E("act", lambda e, G=G, ps2=ps2: e.activation(out=G[:, 20:24], in_=ps2[:, 4:8], func=AF.Exp, scale=-1.0), [Bp2], [Bgt])
        pbt = None
        for tb in range(4):
            G = gt[:, tb, :]
            sl = slice(tb * 128, (tb + 1) * 128)
            for hd in range(4):
                pk, Bpk = nextps()
                pkb = pk[:].bitcast(BF16)
                E("pe", lambda e, hd=hd, pkb=pkb: e.transpose(pkb[:, 0:128], kT[:, hd, sl], identb[:]), [BkT, Bidb], [Bpk])
                E("dve", lambda e, hd=hd, pkb=pkb, G=G: e.tensor_scalar(out=ktil[:], in0=pkb[:, 0:128], scalar1=G[:, 16 + hd:17 + hd], scalar2=None, op0=ALU.mult), [Bpk, Bgt], [Bktil])
                pS, BpS = nextps()
                E("pe", lambda e, hd=hd, pS=pS: e.matmul(pS[:, 0:128], kT[:, hd, sl], qT[:, hd, sl], start=True, stop=True), [BkT, BqT], [BpS])
                E("dve", lambda e, hd=hd, pS=pS, G=G: e.scalar_tensor_tensor(out=PT[:], in0=pS[:, 0:128], scalar=G[:, 12 + hd:13 + hd], in1=trif[:], op0=ALU.mult, op1=ALU.mult), [BpS, Bgt, Btrif], [BPT])
                pN, BpN = nextps()
                E("pe", lambda e, hd=hd, pN=pN: e.matmul(pN[:, 0:129], PT[:], vtok[:, tb, hd, :], start=True, stop=False), [BPT, Bvtok], [BpN])
                E("pe", lambda e, hd=hd, pN=pN: e.matmul(pN[:, 0:129], qT[:, hd, sl], Cbf[:, hd, :], start=False, stop=True), [BqT, BCbf], [BpN])
                pC, BpC = nextps()
                E("pe", lambda e, hd=hd, pC=pC: e.matmul(pC[:, 0:129], ktil[:], vtok[:, tb, hd, :], start=True, stop=True), [Bktil, Bvtok], [BpC])
                E("dve", lambda e, hd=hd, pC=pC, G=G: e.scalar_tensor_tensor(out=C32[:, hd, :], in0=C32[:, hd, :], scalar=G[:, 20 + hd:21 + hd], in1=pC[:, 0:129], op0=ALU.mult, op1=ALU.add), [BC32, Bgt, BpC], [BC32])
                E("dve", lambda e, hd=hd: e.tensor_copy(out=Cbf[:, hd, :], in_=C32[:, hd, :]), [BC32], [BCbf])
                E("act", lambda e, hd=hd, pN=pN, G=G: e.activation(out=sm[:, 0:1], in_=pN[:, 128:129], func=AF.Abs, scale=G[:, 8 + hd:9 + hd]), [BpN, Bgt], [Bsm])
                E("dve", lambda e: e.tensor_scalar(out=sm[:, 0:1], in0=sm[:, 0:1], scalar1=1.0, scalar2=None, op0=ALU.max), [Bsm], [Bsm])
                E("dve", lambda e: e.reciprocal(out=sm[:, 1:2], in_=sm[:, 0:1]), [Bsm], [Bsm])
                mul("dve", sm[:, 2:3], sm[:, 1:2], G[:, 8 + hd:9 + hd], [Bsm, Bgt], [Bsm])
                E("dve", lambda e, hd=hd, pN=pN: e.scalar_tensor_tensor(out=hh[:, hd, :], in0=pN[:, 0:128], scalar=sm[:, 2:3], in1=ogt[:, tb, hd * 128:(hd + 1) * 128], op0=ALU.mult, op1=ALU.mult), [BpN, Bsm, Bogt], [Bhh])
                E("act", lambda e, hd=hd: e.activation(out=junk[:, 0:128], in_=hh[:, hd, :], func=AF.Square, accum_out=sm[:, 4 + hd:5 + hd]), [Bhh], [Bjunk, Bsm])
            E("dve", lambda e: e.tensor_scalar(out=sm[:, 8:12], in0=sm[:, 4:8], scalar1=1.0 / 128, scalar2=EPS, op0=ALU.mult, op1=ALU.add), [Bsm], [Bsm])
            E("act", lambda e: e.activation(out=sm[:, 8:12], in_=sm[:, 8:12], func=AF.Sqrt), [Bsm], [Bsm])
            E("dve", lambda e: e.reciprocal(out=sm[:, 12:16], in_=sm[:, 8:12]), [Bsm], [Bsm])
            for hd in range(4):
                E("dve", lambda e, hd=hd: e.scalar_tensor_tensor(out=ydtok[:, hd * 128:(hd + 1) * 128], in0=hh[:, hd, :], scalar=sm[:, 12 + hd:13 + hd], in1=zwt[:, tb, hd * 128:(hd + 1) * 128], op0=ALU.mult, op1=ALU.mult),
                  [Bhh, Bsm, Bzwt], [Bydtok])
            pt, Bpt = nextps()
            ptb = pt[:].bitcast(BF16)
            for cb in range(4):
                E("pe", lambda e, cb=cb, ptb=ptb: e.transpose(ptb[:, cb * 128:(cb + 1) * 128], ydtok[:, cb * 128:(cb + 1) * 128], identb[:]), [Bydtok, Bidb], [Bpt])
            E("act", lambda e, ptb=ptb: e.copy(out=yb[3][0][:, :, sl], in_=ptb[:, 0:512].rearrange("p (c t) -> p c t", c=4)), [Bpt], [yb[3][1]])

    def s5_p1(l):
        wv, Bw = load_win(l, C_S5U)
        for cb in range(4):
            ps, Bp = proj_fm(wv, Bw, cb)
            E("act", lambda e, cb=cb, ps=ps: e.activation(out=uT[:, cb, :], in_=ps[:], func=AF.Identity), [Bp], [BuT])
        uS = uT[:].rearrange("p g (c s) -> p g s c", s=8)
        Er, Ei = Etab[0][0], Etab[1][0]
        BE = [Etab[0][1], Etab[1][1]]
        Xps = {}
        for c in range(2):
            for hf in range(2):
                Xps[(c, hf)] = nextps()
        for gp in range(16):
            gq, j = divmod(gp, 4)
            for c in range(2):
                ps, Bp = Xps[(c, gp // 8)]
                pv = ps[:].rearrange("p (g c) -> p g c", g=8)
                for s in range(8):
                    E("pe", lambda e, pv=pv, gp=gp, gq=gq, j=j, c=c, s=s: e.matmul(pv[:, gp % 8, :], Min[32 * j:32 * j + 32, gq, s, c, :], uS[32 * j:32 * j + 32, gq, s, :],
                                                                        start=(s == 0), stop=(s == 7), tile_position=(32 * j, 0)), [BMin, BuT], [Bp])
        for hf in range(2):
            gs = slice(hf * 8, hf * 8 + 8)
            xr, Bxr = Xps[(0, hf)]; xi, Bxi = Xps[(1, hf)]
            xrv = xr[:].rearrange("p (g c) -> p g c", g=8); xiv = xi[:].rearrange("p (g c) -> p g c", g=8)
            t1, Bt1 = s5t[0]; t2, Bt2 = s5t[1]
            mul("dve", t1[:, gs, :], xrv, Er[:, gs, :], [Bxr, BE[0]], [Bt1]); mul("dve", t2[:, gs, :], xiv, Ei[:, gs, :], [Bxi, BE[1]], [Bt2])
            tt("dve", Xr[0][0][:, gs, :], t1[:, gs, :], t2[:, gs, :], ALU.subtract, [Bt1, Bt2], [Xr[0][1]])
            mul("dve", t1[:, gs, :], xiv, Er[:, gs, :], [Bxi, BE[0]], [Bt1]); mul("dve", t2[:, gs, :], xrv, Ei[:, gs, :], [Bxr, BE[1]], [Bt2])
            tt("dve", Xr[1][0][:, gs, :], t1[:, gs, :], t2[:, gs, :], ALU.add, [Bt1, Bt2], [Xr[1][1]])
        for c in range(2):
            for gp in range(16):
                E("dve", lambda e, c=c, gp=gp: e.tensor_tensor_scan(out=Hs[c][0][:, gp, :], data0=rho[:, gp:gp + 1].to_broadcast([128, 64]), data1=Xr[c][0][:, gp, :], initial=car[c][0][:, gp:gp + 1], op0=ALU.mult, op1=ALU.add),
                  [Brho, Xr[c][1], car[c][1]], [Hs[c][1]])
        t1, Bt1 = s5t[0]; t2, Bt2 = s5t[1]
        mul("dve", t1[:], Er[:], Hs[0][0][:], [BE[0], Hs[0][1]], [Bt1]); mul("dve", t2[:], Ei[:], Hs[1][0][:], [BE[1], Hs[1][1]], [Bt2])
        tt("dve", Hu[0][0][:], t1[:], t2[:], ALU.add, [Bt1, Bt2], [Hu[0][1]])
        mul("dve", t1[:], Er[:], Hs[1][0][:], [BE[0], Hs[1][1]], [Bt1]); mul("dve", t2[:], Ei[:], Hs[0][0][:], [BE[1], Hs[0][1]], [Bt2])
        tt("dve", Hu[1][0][:], t1[:], t2[:], ALU.subtract, [Bt1, Bt2], [Hu[1][1]])
        for c in range(2):
            E("dve", lambda e, c=c: e.tensor_copy(out=Hp[c][0][:, :, 0], in_=car[c][0][:]), [car[c][1]], [Hp[c][1]])
            E("dve", lambda e, c=c: e.tensor_copy(out=Hp[c][0][:, :, 1:64], in_=Hu[c][0][:, :, 0:63]), [Hu[c][1]], [Hp[c][1]])
            E("dve", lambda e, c=c: e.tensor_copy(out=car[c][0][:], in_=Hu[c][0][:, :, 63]), [Hu[c][1]], [car[c][1]])

    def s5_p2a(l):
        ya, Bya = SC[1]; yg, Byg = SC[2]
        uS = uT[:].rearrange("p g (c s) -> p g s c", s=8)
        for gq in range(4):
            ps, Bp = nextps()
            pv = ps[:].rearrange("p (t c) -> p t c", t=8)
            for t in range(8):
                for s in range(t + 1):
                    E("pe", lambda e, pv=pv, gq=gq, t=t, s=s: e.matmul(pv[:, t, :], Ktoe[:, gq, t - s, :], uS[:, gq, s, :], start=(s == 0), stop=False), [BKtoe, BuT], [Bp])
                for j in range(4):
                    gp = 4 * gq + j
                    for c in range(2):
                        E("pe", lambda e, pv=pv, gp=gp, j=j, t=t, c=c: e.matmul(pv[32 * j:32 * j + 32, t, :], Mout[:, gp, t, c, :], Hp[c][0][:, gp, :], start=False, stop=(c == 1), tile_position=(0, 32 * j)),
                          [BMout, Hp[c][1]], [Bp])
            E("dve", lambda e, gq=gq, pv=pv: e.scalar_tensor_tensor(out=ya[:, gq, 0:T].rearrange("p (c t) -> p c t", t=8), in0=u32[:, gq, :].rearrange("p (c t) -> p c t", t=8), scalar=s5dd[:, gq:gq + 1],
                                                                    in1=pv.rearrange("p t c -> p c t"), op0=ALU.mult, op1=ALU.add), [Bu32, Bs5dd, Bp], [Bya])
        t1, Bt1 = SC[0]
        for cb in range(4):
            Y = ya[:, cb, 0:T]
            mul("dve", t1[:, cb, 0:T], Y, Y, [Bya], [Bt1])
            E("dve", lambda e, cb=cb: e.tensor_scalar(out=t1[:, cb, 0:T], in0=t1[:, cb, 0:T], scalar1=0.044715, scalar2=1.0, op0=ALU.mult, op1=ALU.add), [Bt1], [Bt1])
            mul("dve", t1[:, cb, 0:T], t1[:, cb, 0:T], Y, [Bt1, Bya], [Bt1])
            E("act", lambda e, cb=cb: e.activation(out=t1[:, cb, 0:T], in_=t1[:, cb, 0:T], func=AF.Sigmoid, scale=2.0 * math.sqrt(2.0 / math.pi)), [Bt1], [Bt1])
            mul("dve", yg[:, cb, 0:T], t1[:, cb, 0:T], Y, [Bt1, Bya], [Byg])
            E("dve", lambda e, cb=cb: e.tensor_copy(out=uT[:, cb, :], in_=yg[:, cb, 0:T]), [Byg], [BuT])

    def s5_p2b(l):
        yg, Byg = SC[2]
        B3 = SC[3][1]
        t1v = SC[3][0][:, 2, 0:T]
        zsv = SC[3][0][:, 3, 0:T]
        gv, Bs = load_img(l, ("glu",))
        wv, Bw = load_win(l, C_S5Z)
        for cb in range(4):
            ps, Bp = nextps()
            for kc in range(4):
                E("pe", lambda e, cb=cb, kc=kc, ps=ps: e.matmul(ps[:], gv[:, kc, cb * 128:(cb + 1) * 128], uT[:, kc, :], start=(kc == 0), stop=(kc == 3)), [Bs, BuT], [Bp])
            E("act", lambda e, cb=cb, ps=ps: e.activation(out=t1v, in_=ps[:], func=AF.Sigmoid), [Bp], [B3])
            mul("dve", yg[:, cb, 0:T], yg[:, cb, 0:T], t1v, [Byg, B3], [Byg])
            ps, Bp = proj_fm(wv, Bw, cb)
            E("act", lambda e, cb=cb, ps=ps: e.activation(out=zsv, in_=ps[:], func=AF.Silu), [Bp], [B3])
            mul("dve", yb[0][0][:, cb, :], yg[:, cb, 0:T], zsv, [Byg, B3], [yb[0][1]])

    Bx4 = [Buf("x%d" % i) for i in range(4)]
    xds4 = [kb.dsem("xds4_%d" % i) for i in range(4)]
    ods4 = [kb.dsem("ods4_%d" % i) for i in range(4)]
    stp, Bstp = sbt([128, 16], F32, "stp")

    def dram_io(l, s, t):
        if L == 1:
            return x_d, y_d, ("x", s, t), ("y", s, t)
        src_d = x_d if l == 0 else scr_d[(l - 1) % 2]
        dst_d = y_d if l == L - 1 else scr_d[l % 2]
        skey = ("x", s, t) if l == 0 else ((l - 1) % 2, s, t)
        dkey = ("y", s, t) if l == L - 1 else (l % 2, s, t)
        return src_d, dst_d, skey, dkey

    def emit_load_prenorm(l, s, t, tb, do_b=True):
        xtile = xt[0][0]
        Bx = Bx4[tb]
        src_d, _, skey, _ = dram_io(l, s, t)
        hblk = mrgT[:, :, tb * 128:(tb + 1) * 128]
        kb.dma("sp", xds4[tb], lambda q: q.dma_start(out=xtile[:, tb, :], in_=src_d[s, t * T + tb * 128:t * T + (tb + 1) * 128, :]), reads=[dbuf(skey)], writes=[Bx])
        E("act", lambda e: e.activation(out=junk[:], in_=xtile[:, tb, :], func=AF.Square, accum_out=stp[:, tb:tb + 1]), [Bx], [Bjunk, Bstp])
        E("dve", lambda e: e.tensor_scalar(out=stp[:, 4 + tb:5 + tb], in0=stp[:, tb:tb + 1], scalar1=1.0 / D, scalar2=EPS, op0=ALU.mult, op1=ALU.add), [Bstp], [Bstp])
        E("act", lambda e: e.activation(out=stp[:, 4 + tb:5 + tb], in_=stp[:, 4 + tb:5 + tb], func=AF.Sqrt), [Bstp], [Bstp])
        E("dve", lambda e: e.reciprocal(out=stp[:, 8 + tb:9 + tb], in_=stp[:, 4 + tb:5 + tb]), [Bstp], [Bstp])
        E("dve", lambda e: e.scalar_tensor_tensor(out=hblk, in0=xtile[:, tb, :].rearrange("p (k c) -> p k c", k=8), scalar=stp[:, 8 + tb:9 + tb], in1=npw[:].rearrange("p (k c) -> p k c", k=8), op0=ALU.mult, op1=ALU.mult),
          [Bx, Bstp, Bnpw], [BmrgT[tb]])
        if do_b:
            emit_transposes(tb)

    def emit_transposes(tb):
        pt, Bpt = nextps()
        ptb = pt[:].bitcast(BF16)
        for kc in range(8):
            E("pe", lambda e, kc=kc, ptb=ptb: e.transpose(ptb[:, kc * 128:(kc + 1) * 128], mrgT[:, kc, tb * 128:(tb + 1) * 128], identb[:]), [BmrgT[tb], Bidb], [Bpt])
        E("act", lambda e, ptb=ptb: e.copy(out=hT[:, :, tb * 128:(tb + 1) * 128], in_=ptb.rearrange("p (k c) -> p k c", k=8)), [Bpt], [BhT])

    def tile_layer(l, s, t, prenorm_done, nxt):
        xtile = xt[0][0]
        _, dst_d, _, dkey = dram_io(l, s, t)
        if not prenorm_done:
            for tb in range(4):
                emit_load_prenorm(l, s, t, tb)
        for b in range(4):
            if not en[b]:
                E("dve", lambda e, b=b: e.memset(yb[b][0][:], 0.0), w=[yb[b][1]])
        if en[2]:
            branch_sconv(l)
        if en[1]:
            branch_pool(l, t == 0)
        if en[0]:
            s5_p1(l)
        if en[3]:
            branch_mlstm(l)
        if en[0]:
            s5_p2a(l)
        merge_pass(l, [1, 2, 3], True, False)
        if en[0]:
            s5_p2b(l)
        if dbg and t == NT - 1 and s == 0 and l == L - 1:
            for b in range(4):
                dt_, Bdt = SC[2]
                E("act", lambda e, b=b: e.copy(out=dt_[:, :, 0:T], in_=yb[b][0][:]), [yb[b][1]], [Bdt])
                kb.dma("sp", ods, lambda q, b=b: q.dma_start(out=dbg_d[b], in_=dt_[:, :, 0:T]), reads=[Bdt])
        merge_pass(l, [0], False, True)
        out_proj(l, s, t, nxt)

    def merge_pass(l, blist, init, final):
        for bi, b in enumerate(blist):
            first = init and bi == 0
            lastb = final and bi == len(blist) - 1
            for hf in range(2):
                wbv, Bs = load_img(l, ("wbr", b, hf))
                gvw, Bg = load_win(l, C_G + b * 1024 + hf * 512)
                for jj in range(4):
                    j = hf * 4 + jj
                    ps, Bp = proj_fm(gvw, Bg, jj)
                    gs_, Bgs = gsb[0]
                    E("act", lambda e, ps=ps, gs_=gs_: e.activation(out=gs_[:], in_=ps[:], func=AF.Sigmoid), [Bp], [Bgs])
                    ps2, Bp2 = nextps()
                    for cc in range(4):
                        E("pe", lambda e, cc=cc, jj=jj, ps2=ps2, b=b: e.matmul(ps2[:], wbv[:, cc, jj * 128:(jj + 1) * 128], yb[b][0][:, cc, :], start=(cc == 0), stop=(cc == 3)), [Bs, yb[b][1]], [Bp2])
                    if first:
                        mul("dve", SC[j // 4][0][:, j % 4, 0:T], ps2[:], gs_[:], [Bp2, Bgs], [SC[j // 4][1]])
                    else:
                        tm_, Btm = tmpm[0]
                        mul("dve", tm_[:], ps2[:], gs_[:], [Bp2, Bgs], [Btm])
                        if not lastb:
                            tt("dve", SC[j // 4][0][:, j % 4, 0:T], SC[j // 4][0][:, j % 4, 0:T], tm_[:], ALU.add, [SC[j // 4][1], Btm], [SC[j // 4][1]])
                        else:
                            tt("dve", mrgT[:, j, :], SC[j // 4][0][:, j % 4, 0:T], tm_[:], ALU.add, [SC[j // 4][1], Btm], [BmrgT])

    def out_proj(l, s, t, nxt):
        xtile = xt[0][0]
        _, dst_d, _, dkey = dram_io(l, s, t)
        wo = []
        for hf in range(2):
            wo.append([load_img(l, ("wo", hf, q2)) for q2 in range(2)])
        for tb in range(4):
            pss = []
            for hf in range(2):
                ps, Bp = nextps()
                for q2 in range(2):
                    for j in range(8):
                        E("pe", lambda e, j=j, ps=ps, hf=hf, q2=q2: e.matmul(ps[:, q2 * 256:(q2 + 1) * 256], mrgT[:, j, tb * 128:(tb + 1) * 128], wo[hf][q2][0][:, j, :], start=(j == 0), stop=(j == 7)),
                          [BmrgT[tb], wo[hf][q2][1]], [Bp])
                E("act", lambda e, ps=ps, hf=hf: e.activation(out=junk[:, 0:512], in_=ps[:], func=AF.Square, accum_out=st4[:, 12 + hf:13 + hf]), [Bp], [Bjunk, Bst4])
                pss.append((ps, Bp))
            tt("dve", st4[:, 14:15], st4[:, 12:13], st4[:, 13:14], ALU.add, [Bst4], [Bst4])
            E("dve", lambda e: e.tensor_scalar(out=st4[:, 14:15], in0=st4[:, 14:15], scalar1=1.0 / D, scalar2=EPS, op0=ALU.mult, op1=ALU.add), [Bst4], [Bst4])
            E("act", lambda e: e.activation(out=st4[:, 14:15], in_=st4[:, 14:15], func=AF.Sqrt), [Bst4], [Bst4])
            E("dve", lambda e: e.reciprocal(out=st4[:, 15:16], in_=st4[:, 14:15]), [Bst4], [Bst4])
            for hf in range(2):
                ps, Bp = pss[hf]
                E("dve", lambda e, ps=ps, hf=hf: e.scalar_tensor_tensor(out=junk[:, hf * 512:(hf + 1) * 512], in0=ps[:], scalar=st4[:, 15:16], in1=ppw[:, hf * 512:(hf + 1) * 512], op0=ALU.mult, op1=ALU.mult),
                  [Bp, Bst4, Bppw], [Bjunk])
            tt("dve", xtile[:, tb, :], xtile[:, tb, :], junk[:], ALU.add, [Bx4[tb], Bjunk], [Bx4[tb]])
            kb.dma("sp", ods4[tb], lambda q, tb=tb: q.dma_start(out=dst_d[s, t * T + tb * 128:t * T + (tb + 1) * 128, :], in_=xtile[:, tb, :]), reads=[Bx4[tb]], writes=[dbuf(dkey)])
            if nxt is not None:
                emit_load_prenorm(nxt[0], nxt[1], nxt[2], tb, do_b=False)
        if nxt is not None:
            for tb in range(4):
                emit_transposes(tb)
        return None

    last = None
    convert_layer(0)
    for l in range(L):
        if l > 0:
            kb.new_epoch("_%d" % l)
        layer_prep(l)
        if l + 1 < L:
            convert_layer(l + 1)
        order = [(s, t) for s in range(NSEQ) for t in range(NT)]
        for i, (s, t) in enumerate(order):
            if t == 0:
                seq_reset()
            nxt = (l, order[i + 1][0], order[i + 1][1]) if i + 1 < len(order) else None
            if not PIPE:
                nxt = None
            tile_layer(l, s, t, (i > 0) and PIPE, nxt)
    kb._wait("sp", [Tok(d_.sem, d_.cnt) for d_ in ods4] + [Tok(ods.sem, ods.cnt)])
    es.close()
    return nc, kb


_CACHE = {}
WNAMES = ["norm_pre_w", "norm_post_w", "w_in", "s5_A_re", "s5_A_im", "s5_log_dt", "s5_B_re", "s5_B_im", "s5_C_re", "s5_C_im",
          "s5_D", "s5_w_glu", "pool_w", "pool_scale", "sconv_w", "mlstm_conv_w", "mlstm_b_i", "mlstm_b_f", "mlstm_norm_w",
          "w_branch", "w_out"]


def run_layers(x, weights, ncores, en=(1, 1, 1, 1), dbg=False):
    B, S, _ = x.shape
    nseq = B // ncores
    NT = S // T
    Lw = weights["w_in"].shape[0]
    key = (nseq, NT, en, dbg)
    consts = host_consts()
    cur = np.ascontiguousarray(x, dtype=np.float32)
    dbg_out = None
    for l in range(Lw):
        nc, _ = build(1, nseq, NT, en, dbg)
        in_maps = []
        for c in range(ncores):
            m = {"x": cur[c * nseq:(c + 1) * nseq]}
            for k in WNAMES:
                m[k] = np.ascontiguousarray(weights[k][l:l + 1], dtype=np.float32)
            m.update(consts)
            in_maps.append(m)
        res = run_bass_kernel_spmd(nc, in_maps, core_ids=list(range(ncores)))
        cur = np.concatenate([r["y"] for r in res.results], axis=0)
        if dbg:
            dbg_out = res.results[0]["dbg"]
    return (cur, dbg_out) if dbg else cur


def run_fused(x, weights, ncores, dbg=False):
    B, S, _ = x.shape
    nseq = B // ncores
    NT = S // T
    Lw = weights["w_in"].shape[0]
    consts = host_consts()
    nc, _ = build(Lw, nseq, NT, (1, 1, 1, 1), dbg)
    xs = np.ascontiguousarray(x, dtype=np.float32)
    wts = {k: np.ascontiguousarray(weights[k], dtype=np.float32) for k in WNAMES}
    in_maps = []
    for c in range(ncores):
        m = {"x": xs[c * nseq:(c + 1) * nseq]}
        m.update(wts)
        m.update(consts)
        in_maps.append(m)
    res = run_bass_kernel_spmd(nc, in_maps, core_ids=list(range(ncores)))
    return np.concatenate([r["y"] for r in res.results], axis=0)


def kernel(**inputs):
    x = inputs["x"]
    weights = {k: inputs[k] for k in WNAMES}
    return run_fused(x, weights, 8).astype(np.float32)
```

```python
import math
import contextlib
import numpy as np
import concourse.bass as bass
import concourse.mybir as mybir
from concourse.bass_utils import run_bass_kernel_spmd

F32 = mybir.dt.float32
BF16 = mybir.dt.bfloat16
I32 = mybir.dt.int32
AF = mybir.ActivationFunctionType
ALU = mybir.AluOpType

SAME_SYNC = True
import os as _os
PIPE = int(_os.environ.get('KPIPE', '1'))
D = 1024
W = 512
NIN = 10760
T = 512
EPS = 1e-6
C_S5U, C_S5Z, C_PU, C_PZ, C_SX, C_SB, C_SC, C_SZ = 0, 512, 1024, 1536, 2048, 2560, 3072, 3584
C_Q, C_K, C_V, C_O, C_IF, C_MZ, C_G = 4096, 4608, 5120, 5632, 6144, 6152, 6664
POOL_WINDOWS = (2, 4, 8, 16)


class Tok:
    __slots__ = ("sem", "val")

    def __init__(self, sem, val):
        self.sem = sem
        self.val = val


class Buf:
    def __init__(self, name=""):
        self.name = name
        self.w = None
        self.r = {}


class DSem:
    def __init__(self, sem):
        self.sem = sem
        self.cnt = 0


class KB:
    def __init__(self, nc, es):
        self.nc = nc
        self.es = es
        self.eng = {"pe": nc.tensor, "act": nc.scalar, "dve": nc.vector, "pool": nc.gpsimd, "sp": nc.sync}
        self.sem = {e: es.enter_context(nc.semaphore("s_" + e)) for e in self.eng}
        self.cnt = {e: 0 for e in self.eng}
        self.seen = {e: {} for e in self.eng}
        self.nins = 0
        self.uid = 0

    def dsem(self, name):
        return DSem(self.es.enter_context(self.nc.semaphore(name)))

    def new_epoch(self, tag):
        self._keep = getattr(self, '_keep', []) + [self.sem]
        self.sem = {e: self.es.enter_context(self.nc.semaphore("s_" + e + tag)) for e in self.eng}
        self.cnt = {e: 0 for e in self.eng}

    def sb(self, name, shape, dt):
        return self.es.enter_context(self.nc.sbuf_tensor(name, list(shape), dt))

    def _wait(self, e, toks):
        need = {}
        for t in toks:
            if t is None:
                continue
            k = id(t.sem)
            if k not in need or need[k].val < t.val:
                need[k] = t
        for k, t in need.items():
            if t.sem is self.sem[e] and (e == "pe" or not SAME_SYNC):
                continue
            if self.seen[e].get(k, 0) >= t.val:
                continue
            self.eng[e].wait_ge(t.sem, t.val)
            self.seen[e][k] = t.val

    @staticmethod
    def _flat(lst):
        out = []
        for b in lst:
            if isinstance(b, (list, tuple)):
                out.extend(KB._flat(b))
            else:
                out.append(b)
        return out

    def _deps(self, reads, writes):
        reads = self._flat(reads); writes = self._flat(writes)
        toks = []
        for b in reads:
            toks.append(b.w)
        for b in writes:
            toks.append(b.w)
            toks.extend(b.r.values())
        return toks

    def _upd(self, tok, reads, writes):
        reads = self._flat(reads); writes = self._flat(writes)
        k = id(tok.sem)
        for b in reads:
            b.r[k] = tok
        for b in writes:
            b.w = tok
            b.r = {}

    def op(self, e, fn, reads=(), writes=()):
        self._wait(e, self._deps(reads, writes))
        ins = fn(self.eng[e])
        self.cnt[e] += 1
        ins.then_inc(self.sem[e], 1)
        tok = Tok(self.sem[e], self.cnt[e])
        self._upd(tok, reads, writes)
        self.nins += 1
        return tok

    def dma(self, e, ds, fn, reads=(), writes=()):
        self._wait(e, self._deps(reads, writes))
        ins = fn(self.eng[e])
        ds.cnt += 16
        ins.then_inc(ds.sem, 16)
        tok = Tok(ds.sem, ds.cnt)
        self._upd(tok, reads, writes)
        self.nins += 1
        return tok


def host_consts():
    ident = np.eye(128, dtype=np.float32)
    m = np.arange(128)[:, None]
    l = np.arange(128)[None, :]
    tri = (m <= l).astype(np.float32)
    ones = np.ones((128, 128), np.float32)
    band = np.zeros((3, 4, 128, 128), np.float32)
    for g, win in enumerate(POOL_WINDOWS):
        dlt = l - m
        inw = (dlt >= 0) & (dlt < win)
        band[0, g] = inw / win - ident
        dp = l + 128 - m
        band[1, g] = ((dp >= 0) & (dp < win)) / win
        band[2, g] = inw / np.minimum(l + 1, win) - ident
    return {"c_ident": ident, "c_tri": tri, "c_ones": ones,
            "c_band": np.ascontiguousarray(band.transpose(2, 0, 1, 3)).reshape(128, 12 * 128)}


def build(L, NSEQ, NT, en=(1, 1, 1, 1), dbg=False):
    S = NT * T
    nc = bass.Bass("TRN2", target_bir_lowering=False)
    es = contextlib.ExitStack()
    kb = KB(nc, es)

    def din(name, shape):
        return nc.dram_tensor(name, list(shape), F32, kind="ExternalInput").ap()

    x_d = din("x", [NSEQ, S, D])
    npw_d = din("norm_pre_w", [L, D]); ppw_d = din("norm_post_w", [L, D])
    win_d = din("w_in", [L, D, NIN])
    are_d = din("s5_A_re", [L, 32, 64]); aim_d = din("s5_A_im", [L, 32, 64]); ldt_d = din("s5_log_dt", [L, 32])
    bre_d = din("s5_B_re", [L, 32, 64, 16]); bim_d = din("s5_B_im", [L, 32, 64, 16])
    cre_d = din("s5_C_re", [L, 32, 16, 64]); cim_d = din("s5_C_im", [L, 32, 16, 64])
    s5d_d = din("s5_D", [L, W]); glu_d = din("s5_w_glu", [L, W, W])
    pw_d = din("pool_w", [L, 4, 128, 128]); psc_d = din("pool_scale", [L, W])
    scw_d = din("sconv_w", [L, 3, W]); mcw_d = din("mlstm_conv_w", [L, 4, 2 * W])
    mbi_d = din("mlstm_b_i", [L, 4]); mbf_d = din("mlstm_b_f", [L, 4]); mnw_d = din("mlstm_norm_w", [L, W])
    wbr_d = din("w_branch", [L, 4, W, D]); wo_d = din("w_out", [L, D, D])
    cid_d = din("c_ident", [128, 128]); ctri_d = din("c_tri", [128, 128]); cone_d = din("c_ones", [128, 128])
    cband_d = din("c_band", [128, 12 * 128])
    y_d = nc.dram_tensor("y", [NSEQ, S, D], F32, kind="ExternalOutput").ap()
    dbg_d = nc.dram_tensor("dbg", [4, 128, 4, T], F32, kind="ExternalOutput").ap() if dbg else None
    scr_d = [nc.dram_tensor("xscr%d" % i, [NSEQ, S, D], F32, kind="Internal").ap() for i in range(2)] if L > 1 else []
    dram_bufs = {}
    NIMG = 55
    SLW = 8 * 264
    wimg_d = [nc.dram_tensor("wimg%d" % l_, [NIMG, 128, SLW], BF16, kind="Internal").ap() for l_ in range(L)]
    Bimg = [Buf("wimg") for _ in range(L)]
    cvds = [None] * L
    img_index = {}

    def img_specs(l):
        sp = []

        def wv_(c0, n):
            return win_d[l, :, c0:c0 + n].rearrange("(k p) c -> p k c", p=128)
        for c0 in (C_S5U, C_S5Z, C_PU, C_PZ, C_SX, C_SB, C_SC, C_SZ, C_Q, C_K, C_V, C_O) + tuple(C_G + i * 512 for i in range(8)):
            for h in range(2):
                sp.append((("win", c0, h), 8, 256, wv_(c0 + 256 * h, 256)))
        sp.append((("win", C_IF, 0), 8, 264, wv_(C_IF, 264)))
        sp.append((("win", C_IF, 1), 8, 256, wv_(C_IF + 264, 256)))
        sp.append((("glu",), 4, 512, glu_d[l].rearrange("(k p) c -> p k c", p=128)))
        for b in range(4):
            for hf in range(2):
                sp.append((("wbr", b, hf), 4, 512, wbr_d[l, b, :, hf * 512:(hf + 1) * 512].rearrange("(k p) c -> p k c", p=128)))
        for hf in range(2):
            for q2 in range(2):
                c0 = hf * 512 + q2 * 256
                sp.append((("wo", hf, q2), 8, 256, wo_d[l, :, c0:c0 + 256].rearrange("(k p) c -> p k c", p=128)))
        return sp

    def convert_layer(l):
        cvds[l] = kb.dsem("cvds%d" % l)
        for i, (key, k, c, src) in enumerate(img_specs(l)):
            img_index[(l,) + key] = (i, k, c)
            for kk in range(k):
                dst = wimg_d[l][i, :, kk * c:(kk + 1) * c]
                kb.dma("pool", cvds[l], lambda q, dst=dst, src=src, kk=kk: q.dma_start(out=dst, in_=src[:, kk, :]), writes=[Bimg[l]])
        Bimg[l].w = Tok(cvds[l].sem, cvds[l].cnt)

    def load_img(l, key):
        i, k, c = img_index[(l,) + key]
        si = wsi[0] % NSL
        wsi[0] += 1
        slot, Bs = wsl[si]
        kb.dma("sp", wds[si], lambda q: q.dma_start(out=slot[:, 0:k * c], in_=wimg_d[l][i, :, 0:k * c]), reads=[Bimg[l]], writes=[Bs])
        return slot[:, 0:k * c].rearrange("p (k c) -> p k c", k=k), Bs

    def dbuf(key):
        if key not in dram_bufs:
            dram_bufs[key] = Buf("dram")
        return dram_bufs[key]

    uid = [0]

    def sbt(shape, dt, name=None):
        uid[0] += 1
        return kb.sb((name or "t") + str(uid[0]), shape, dt), Buf(name or "t")
    sbt_global = sbt

    NPS = 8
    psb = [es.enter_context(nc.psum_tensor("ps%d" % i, [128, 512], F32)) for i in range(NPS)]
    psB = [Buf("ps%d" % i) for i in range(NPS)]
    psi = [0]

    def nextps():
        i = psi[0] % NPS
        psi[0] += 1
        return psb[i], psB[i]

    def E(e, fn, r=(), w=()):
        return kb.op(e, fn, r, w)

    cdsem = kb.dsem("cdsem")

    cbufs = []

    def cbarrier():
        for b_ in cbufs:
            b_.w = Tok(cdsem.sem, cdsem.cnt)
        del cbufs[:]

    def cload(e, out_ap, in_ap, wb, slow=False):
        cbufs.append(wb)
        return kb.dma(e, cdsem, lambda q: q.dma_start(out=out_ap, in_=in_ap, allow_slow_non_contiguous=True) if slow
                      else q.dma_start(out=out_ap, in_=in_ap), writes=[wb])

    identf, Bidf = sbt([128, 128], F32, "identf")
    identb, Bidb = sbt([128, 128], BF16, "identb")
    trif, Btrif = sbt([128, 128], F32, "trif")
    onesf, Bonesf = sbt([128, 128], F32, "onesf")
    band, Bband = sbt([128, 3, 4, 128], BF16, "band")
    cload("sp", identf[:], cid_d, Bidf)
    cload("pool", identb[:], cid_d, Bidb)
    cload("sp", trif[:], ctri_d, Btrif)
    cload("sp", onesf[:], cone_d, Bonesf)
    cload("pool", band[:].rearrange("p a g l -> p (a g l)"), cband_d, Bband)
    cbarrier()

    xt = [sbt([128, 4, D], F32, "xt") for _ in range(1)]
    xds = [kb.dsem("xds%d" % i) for i in range(1)]
    ods = kb.dsem("ods")
    hT, BhT = sbt([128, 8, T], BF16, "hT")
    st4, Bst4 = sbt([128, 16], F32, "st4")
    NSL = 6
    wsl = [sbt([128, SLW], BF16, "wsl") for _ in range(NSL)]
    wds = [kb.dsem("wds%d" % i) for i in range(NSL)]
    wsi = [0]

    def load_w(views):
        i = wsi[0] % NSL
        wsi[0] += 1
        slot, Bs = wsl[i]
        for (off, kk, cc, src) in views:
            dst = slot[:, off:off + kk * cc].rearrange("p (k c) -> p k c", k=kk)
            kb.dma("pool", wds[i], lambda q, dst=dst, src=src: q.dma_start(out=dst, in_=src), writes=[Bs])
        return slot, Bs

    def win_view(l, c0, n):
        return win_d[l, :, c0:c0 + n].rearrange("(k p) c -> p k c", p=128)

    def load_win(l, c0, n=512):
        return [load_img(l, ("win", c0, h)) for h in range(2)], None

    def proj_fm(wg, _, jb):
        wv, Bw = wg[jb // 2]
        jj_ = jb % 2
        ps, Bp = nextps()
        for kc in range(8):
            E("pe", lambda e, kc=kc: e.matmul(ps[:], wv[:, kc, jj_ * 128:(jj_ + 1) * 128], hT[:, kc, :], start=(kc == 0), stop=(kc == 7)),
              [Bw, BhT], [Bp])
        return ps, Bp

    def proj_tm(wg, _, tb, c0, n):
        ps, Bp = nextps()
        if n == 8:
            parts = [(0, 0, 8, 0)]
        elif c0 == 8:
            parts = [(0, 8, 256, 0), (1, 0, 256, 256)]
        else:
            parts = [(0, 0, 256, 0), (1, 0, 256, 256)]
        for (h, wc, nn, pc) in parts:
            wv, Bw = wg[h]
            for kc in range(8):
                E("pe", lambda e, kc=kc, wv=wv: e.matmul(ps[:, pc:pc + nn], hT[:, kc, tb * 128:(tb + 1) * 128], wv[:, kc, wc:wc + nn], start=(kc == 0), stop=(kc == 7)),
                  [Bw, BhT], [Bp])
        return ps, Bp

    yb = [sbt([128, 4, T], BF16, "yb") for _ in range(4)]
    SC = [sbt([128, 4, T + 4], F32, "scr") for _ in range(4)]

    def scflat(i):
        return SC[i][0][:].rearrange("p a b -> p (a b)")

    class _V0:
        def __init__(self, ap):
            self.ap = ap

        def __getitem__(self, k):
            return self.ap[k]
    junk, Bjunk = _V0(scflat(2)[:, 0:D]), SC[2][1]

    def sc_half(i, h):
        return scflat(i)[:, h * 1024:(h + 1) * 1024].rearrange("p (g c) -> p g c", g=16)
    qkT, _bq = sbt([128, 8, T], BF16, "qkT")
    BqkT = [Buf("mrg%d" % i) for i in range(4)]
    mrgT, BmrgT = qkT, BqkT
    gsb = [(_V0(SC[3][0][:, 0, 0:T]), SC[3][1])]
    tmpm = [(_V0(SC[3][0][:, 1, 0:T]), SC[3][1])]

    npw, Bnpw = sbt([128, D], F32, "npw"); ppw, Bppw = sbt([128, D], F32, "ppw")
    scw, Bscw = sbt([128, 3, 4], F32, "scw"); mcw, Bmcw = sbt([128, 4, 8], F32, "mcw")
    psc, Bpsc = sbt([128, 4], F32, "psc"); s5dd, Bs5dd = sbt([128, 4], F32, "s5dd")
    mnw, Bmnw = sbt([128, W], F32, "mnw"); mbi, Bmbi = sbt([128, 4], F32, "mbi"); mbf, Bmbf = sbt([128, 4], F32, "mbf")
    poolw, Bpoolw = sbt([128, 4, 128], BF16, "poolw")
    schalo, Bschalo = sbt([128, 4, 2], F32, "schalo")
    putok, Bputok = sbt([128, 5, W], BF16, "putok")
    mlhalo, Bmlhalo = sbt([128, 8, 3], F32, "mlhalo")
    C32, BC32 = sbt([128, 4, 129], F32, "C32"); Cbf, BCbf = sbt([128, 4, 129], BF16, "Cbf")
    car = [sbt([128, 16], F32, "car") for _ in range(2)]
    Min, BMin = sbt([128, 4, 8, 2, 128], BF16, "Min")
    Mout, BMout = sbt([128, 16, 8, 2, 32], BF16, "Mout")
    Ktoe, BKtoe = sbt([128, 4, 8, 128], BF16, "Ktoe")
    Etab = [sbt([128, 16, 64], F32, "Etab") for _ in range(2)]
    rho, Brho = sbt([128, 16], F32, "rho")

    class _V:
        def __init__(self, ap):
            self.ap = ap

        def __getitem__(self, k):
            return self.ap[k]
    Zpad = [(_V(scflat(i)[:, 0:2048].rearrange("p (g c) -> p g c", g=16)), SC[i][1]) for i in range(2)]
    Cpad = [(_V(scflat(i)[:, 0:2048].rearrange("p (g c) -> p g c", g=16)), SC[i][1]) for i in range(2, 4)]
    E("dve", lambda e: e.memset(Mout[:], 0.0), w=[BMout])
    qT, BqT = _V(qkT[:, 0:4, :]), BqkT; kT, BkT = _V(qkT[:, 4:8, :]), BqkT
    vtok, Bvtok = sbt([128, 4, 4, 129], BF16, "vtok")
    E("dve", lambda e: e.memset(vtok[:], 1.0), w=[Bvtok])
    ogt, Bogt = sbt([128, 4, W], BF16, "ogt"); zwt, Bzwt = sbt([128, 4, W], BF16, "zwt")
    gt, Bgt = sbt([128, 4, 40], F32, "gt")
    ktil2 = [sbt([128, 128], BF16, "ktil") for _ in range(2)]; PT2 = [sbt([128, 128], BF16, "PT") for _ in range(2)]
    hh, Bhh = sbt([128, 4, 128], F32, "hh"); ydtok, Bydtok = sbt([128, W], BF16, "ydtok")
    sm, Bsm = sbt([128, 16], F32, "sm")
    uT, BuT = sbt([128, 4, T], BF16, "uT"); u32, Bu32 = uT, BuT
    s5t = [(_V(sc_half(0, h)), SC[0][1]) for h in range(2)]
    Hs = [(_V(sc_half(1, h)), SC[1][1]) for h in range(2)]
    Xr = [(_V(sc_half(2, h)), SC[2][1]) for h in range(2)]
    Hu = Xr
    HpT, BHpT = sbt([128, 2, 16, 64], BF16, "HpT")
    Hp = [(_V(HpT[:, c]), BHpT) for c in range(2)]
    pooledT, BpooledT = _V(HpT[:].rearrange("p a g c -> p (a g c)").rearrange("p (g t) -> p g t", g=4)), BHpT

    def mul(e, o, a, b, r, w):
        return E(e, lambda q: q.tensor_tensor(out=o, in0=a, in1=b, op=ALU.mult), r, w)

    def tt(e, o, a, b, op, r, w):
        return E(e, lambda q: q.tensor_tensor(out=o, in0=a, in1=b, op=op), r, w)

    prep_cache = {}

    def layer_prep(l):
        pidx = [0]

        def sbt(shape, dt, name=None):
            pidx[0] += 1
            k = (name, pidx[0])
            if k not in prep_cache:
                prep_cache[k] = sbt_global(shape, dt, name)
            return prep_cache[k]

        def bc(v):
            return v.partition_broadcast(128)
        cload("sp", npw[:], bc(npw_d[l]), Bnpw); cload("sp", ppw[:], bc(ppw_d[l]), Bppw)
        cload("sp", scw[:], scw_d[l].rearrange("k (cb p) -> p k cb", p=128), Bscw, slow=True)
        cload("sp", mcw[:], mcw_d[l].rearrange("k (cb p) -> p k cb", p=128), Bmcw, slow=True)
        cload("sp", psc[:], psc_d[l].rearrange("(cb p) -> p cb", p=128), Bpsc, slow=True)
        cload("sp", s5dd[:], s5d_d[l].rearrange("(cb p) -> p cb", p=128), Bs5dd, slow=True)
        cload("sp", mnw[:], bc(mnw_d[l]), Bmnw); cload("sp", mbi[:], bc(mbi_d[l]), Bmbi); cload("sp", mbf[:], bc(mbf_d[l]), Bmbf)
        cload("pool", poolw[:], pw_d[l].rearrange("g c d -> c g d"), Bpoolw)
        if not en[0]:
            cbarrier()
            return
        for tt_, bb_ in Zpad + Cpad:
            E("dve", lambda e, tt_=tt_: e.memset(tt_[:], 0.0), w=[bb_])
        def arena(i):
            return yb[i][0][:].rearrange("p a b -> p (a b)").bitcast(F32)

        def carve(i, off, shp):
            n = shp[0] * shp[1]
            return (_V(arena(i)[:, off:off + n].rearrange("p (a b) -> p a b", a=shp[0])), yb[i][1])
        A = [sbt([128, 16], F32, "A") for _ in range(2)]
        ldt, Bldt = sbt([128, 16], F32, "ldt")
        Bm = [carve(0, c * 256, (16, 16)) for c in range(2)]
        Cm = [carve(0, 512 + c * 256, (16, 16)) for c in range(2)]
        cload("sp", A[0][0][:], are_d[l].rearrange("(gp gl) n -> (gl n) gp", gl=2), A[0][1], slow=True)
        cload("sp", A[1][0][:], aim_d[l].rearrange("(gp gl) n -> (gl n) gp", gl=2), A[1][1], slow=True)
        for gl in range(2):
            cload("sp", ldt[gl * 64:(gl + 1) * 64, :], ldt_d[l, gl::2].partition_broadcast(64), Bldt, slow=True)
        for c, src in enumerate((bre_d, bim_d)):
            cload("sp", Bm[c][0][:], src[l].rearrange("(gp gl) n q -> (gl n) gp q", gl=2), Bm[c][1], slow=True)
        for c, src in enumerate((cre_d, cim_d)):
            for gl in range(2):
                for gp in range(16):
                    cload("sp", Cm[c][0][gl * 64:(gl + 1) * 64, gp, :], src[l, 2 * gp + gl].rearrange("p n -> n p"), Cm[c][1], slow=True)
        cbarrier()
        tn = [sbt([128, 16], F32, "tn") for _ in range(12)]

        def V(i):
            return tn[i][0][:]

        def Bv(i):
            return tn[i][1]
        Are, Aim = A[0][0][:], A[1][0][:]
        BA = [A[0][1], A[1][1]]
        E("act", lambda e: e.activation(out=ldt[:], in_=ldt[:], func=AF.Exp), [Bldt], [Bldt])
        mul("dve", V(0), Are, ldt[:], [BA[0], Bldt], [Bv(0)])
        mul("dve", V(1), Aim, ldt[:], [BA[1], Bldt], [Bv(1)])
        E("act", lambda e: e.activation(out=V(2), in_=V(0), func=AF.Exp), [Bv(0)], [Bv(2)])
        E("act", lambda e: e.activation(out=V(11), in_=V(0), func=AF.Exp, scale=8.0), [Bv(0)], [Bv(11)])
        ki, Bki = sbt([128, 16], I32, "ki")
        E("dve", lambda e: e.tensor_scalar(out=V(3), in0=V(1), scalar1=1.0 / (2 * math.pi), scalar2=None, op0=ALU.mult), [Bv(1)], [Bv(3)])
        E("dve", lambda e: e.tensor_copy(out=ki[:], in_=V(3)), [Bv(3)], [Bki])
        E("dve", lambda e: e.tensor_copy(out=V(3), in_=ki[:]), [Bki], [Bv(3)])
        E("dve", lambda e: e.scalar_tensor_tensor(out=V(4), in0=V(3), scalar=-2 * math.pi, in1=V(1), op0=ALU.mult, op1=ALU.add), [Bv(3), Bv(1)], [Bv(4)])
        E("act", lambda e: e.activation(out=V(5), in_=V(4), func=AF.Sin, scale=0.5), [Bv(4)], [Bv(5)])
        E("act", lambda e: e.activation(out=V(6), in_=V(4), func=AF.Sin, scale=0.25), [Bv(4)], [Bv(6)])
        mul("dve", V(7), V(6), V(6), [Bv(6)], [Bv(7)])
        E("dve", lambda e: e.tensor_scalar(out=V(7), in0=V(7), scalar1=-2.0, scalar2=1.0, op0=ALU.mult, op1=ALU.add), [Bv(7)], [Bv(7)])
        mul("dve", V(8), V(5), V(7), [Bv(5), Bv(7)], [Bv(8)])
        mul("dve", V(5), V(5), V(5), [Bv(5)], [Bv(5)])
        E("dve", lambda e: e.tensor_scalar(out=V(5), in0=V(5), scalar1=-2.0, scalar2=1.0, op0=ALU.mult, op1=ALU.add), [Bv(5)], [Bv(5)])
        Pw = [sbt([128, 9, 16], F32, "Pw") for _ in range(2)]
        Pr, Pi = Pw[0][0], Pw[1][0]
        BP = [Pw[0][1], Pw[1][1]]
        E("dve", lambda e: e.memset(Pr[:, 0, :], 1.0), w=[BP[0]])
        E("dve", lambda e: e.memset(Pi[:, 0, :], 0.0), w=[BP[1]])
        mul("dve", Pr[:, 1, :], V(2), V(5), [Bv(2), Bv(5)], [BP[0]])
        E("dve", lambda e: e.scalar_tensor_tensor(out=Pi[:, 1, :], in0=V(8), scalar=2.0, in1=V(2), op0=ALU.mult, op1=ALU.mult), [Bv(8), Bv(2)], [BP[1]])

        def cmul(o_r, o_i, xr, xi, yr, yi, r, w, t1, t2, Bt):
            mul("dve", t1, xr, yr, r, Bt); mul("dve", t2, xi, yi, r, Bt)
            tt("dve", o_r, t1, t2, ALU.subtract, Bt, [w[0]])
            mul("dve", t1, xr, yi, r, Bt); mul("dve", t2, xi, yr, r, Bt)
            tt("dve", o_i, t1, t2, ALU.add, Bt, [w[1]])
        for j in range(1, 8):
            cmul(Pr[:, j + 1, :], Pi[:, j + 1, :], Pr[:, j, :], Pi[:, j, :], Pr[:, 1, :], Pi[:, 1, :], BP, BP, V(9), V(10), [Bv(9), Bv(10)])
        E("dve", lambda e: e.reciprocal(out=V(0), in_=V(11)), [Bv(11)], [Bv(0)])
        wl = [sbt([128, 16], F32, "wl") for _ in range(4)]
        mul("dve", wl[0][0][:], Pr[:, 8, :], V(0), [BP[0], Bv(0)], [wl[0][1]])
        E("dve", lambda e: e.scalar_tensor_tensor(out=wl[1][0][:], in0=Pi[:, 8, :], scalar=-1.0, in1=V(0), op0=ALU.mult, op1=ALU.mult), [BP[1], Bv(0)], [wl[1][1]])
        Er, Ei = Etab[0][0], Etab[1][0]
        BE = [Etab[0][1], Etab[1][1]]
        E("dve", lambda e: e.tensor_copy(out=Er[:, :, 0], in_=wl[0][0][:]), [wl[0][1]], [BE[0]])
        E("dve", lambda e: e.tensor_copy(out=Ei[:, :, 0], in_=wl[1][0][:]), [wl[1][1]], [BE[1]])
        t3a, Bt3a = carve(2, 0, (16, 32)); t3b, Bt3b = carve(2, 512, (16, 32))
        cur = 0
        Lw = 1
        while Lw < 64:
            wr = wl[cur][0][:].unsqueeze(2).to_broadcast([128, 16, Lw]); wi = wl[cur + 1][0][:].unsqueeze(2).to_broadcast([128, 16, Lw])
            Bw = [wl[cur][1], wl[cur + 1][1]]
            cmul(Er[:, :, Lw:2 * Lw], Ei[:, :, Lw:2 * Lw], Er[:, :, 0:Lw], Ei[:, :, 0:Lw], wr, wi, BE + Bw, BE, t3a[:, :, 0:Lw], t3b[:, :, 0:Lw], [Bt3a, Bt3b])
            nxt = 2 - cur
            cmul(wl[nxt][0][:], wl[nxt + 1][0][:], wl[cur][0][:], wl[cur + 1][0][:], wl[cur][0][:], wl[cur + 1][0][:], Bw, [wl[nxt][1], wl[nxt + 1][1]], V(9), V(10), [Bv(9), Bv(10)])
            cur = nxt
            Lw *= 2
        E("dve", lambda e: e.tensor_copy(out=rho[:], in_=V(11)), [Bv(11)], [Brho])
        E("dve", lambda e: e.tensor_scalar(out=V(0), in0=Pr[:, 1, :], scalar1=-1.0, scalar2=None, op0=ALU.add), [BP[0]], [Bv(0)])
        mul("dve", V(1), Are, Are, [BA[0]], [Bv(1)]); mul("dve", V(2), Aim, Aim, [BA[1]], [Bv(2)])
        tt("dve", V(1), V(1), V(2), ALU.add, [Bv(1), Bv(2)], [Bv(1)])
        E("dve", lambda e: e.reciprocal(out=V(1), in_=V(1)), [Bv(1)], [Bv(1)])
        mul("dve", V(2), V(0), Are, [Bv(0), BA[0]], [Bv(2)]); mul("dve", V(3), Pi[:, 1, :], Aim, [BP[1], BA[1]], [Bv(3)])
        tt("dve", V(2), V(2), V(3), ALU.add, [Bv(2), Bv(3)], [Bv(2)]); mul("dve", V(4), V(2), V(1), [Bv(2), Bv(1)], [Bv(4)])
        mul("dve", V(2), Pi[:, 1, :], Are, [BP[1], BA[0]], [Bv(2)]); mul("dve", V(3), V(0), Aim, [Bv(0), BA[1]], [Bv(3)])
        tt("dve", V(2), V(2), V(3), ALU.subtract, [Bv(2), Bv(3)], [Bv(2)]); mul("dve", V(5), V(2), V(1), [Bv(2), Bv(1)], [Bv(5)])
        Bb = [carve(1, c * 256, (16, 16)) for c in range(2)]
        Zz = [carve(1, 512 + c * 256, (16, 16)) for c in range(2)]
        t4a, Bt4a = carve(3, 0, (16, 16)); t4b, Bt4b = carve(3, 256, (16, 16))

        def b16(v):
            return v.unsqueeze(2).to_broadcast([128, 16, 16])
        cmul(Bb[0][0][:], Bb[1][0][:], b16(V(4)), b16(V(5)), Bm[0][0][:], Bm[1][0][:], [Bv(4), Bv(5), Bm[0][1], Bm[1][1]], [Bb[0][1], Bb[1][1]], t4a[:], t4b[:], [Bt4a, Bt4b])
        E("dve", lambda e: e.tensor_scalar(out=t4a[:], in0=Cm[1][0][:], scalar1=-1.0, scalar2=None, op0=ALU.mult), [Cm[1][1]], [Bt4a])
        csrc = [(Cm[0][0], Cm[0][1]), (t4a, Bt4a)]

        def padfill(dst, Bd, src, Bs):
            d5 = dst[:].rearrange("p (gq j) (jj c) -> p gq j jj c", j=4, jj=4)
            s4 = src[:].rearrange("p (gq j) q -> p gq j q", j=4)
            for j in range(4):
                for gl in range(2):
                    E("act", lambda e, j=j, gl=gl: e.copy(out=d5[gl * 64:(gl + 1) * 64, :, j, j, gl * 16:(gl + 1) * 16], in_=s4[gl * 64:(gl + 1) * 64, :, j, :]), [Bs], [Bd])
        for c in range(2):
            padfill(Cpad[c][0], Cpad[c][1], csrc[c][0], csrc[c][1])
        for jp in range(8):
            cmul(Zz[0][0][:], Zz[1][0][:], b16(Pr[:, jp, :]), b16(Pi[:, jp, :]), Bb[0][0][:], Bb[1][0][:], BP + [Bb[0][1], Bb[1][1]], [Zz[0][1], Zz[1][1]], t4a[:], t4b[:], [Bt4a, Bt4b])
            for c in range(2):
                padfill(Zpad[c][0], Zpad[c][1], Zz[c][0], Zz[c][1])
            s = 7 - jp
            for gq in range(4):
                for c in range(2):
                    ps, Bp = nextps()
                    for j in range(4):
                        E("pe", lambda e, j=j, c=c, gq=gq: e.matmul(ps[:, 0:128], Zpad[c][0][:, 4 * gq + j, :], identf[:], start=(j == 0), stop=(j == 3)), [Zpad[c][1], Bidf], [Bp])
                    E("dve", lambda e, c=c, gq=gq, ps=ps: e.tensor_copy(out=Min[:, gq, s, c, :], in_=ps[:, 0:128]), [Bp], [BMin])
                ps, Bp = nextps()
                for j in range(4):
                    for c in range(2):
                        E("pe", lambda e, j=j, c=c, gq=gq: e.matmul(ps[:, 0:128], Zpad[c][0][:, 4 * gq + j, :], Cpad[c][0][:, 4 * gq + j, :], start=(j == 0 and c == 0), stop=(j == 3 and c == 1)),
                          [Zpad[c][1], Cpad[c][1]], [Bp])
                E("dve", lambda e, gq=gq, ps=ps: e.tensor_copy(out=Ktoe[:, gq, jp, :], in_=ps[:, 0:128]), [Bp], [BKtoe])
        for t in range(8):
            cmul(Zz[0][0][:], Zz[1][0][:], b16(Pr[:, t + 1, :]), b16(Pi[:, t + 1, :]), Cm[0][0][:], Cm[1][0][:], BP + [Cm[0][1], Cm[1][1]], [Zz[0][1], Zz[1][1]], t4a[:], t4b[:], [Bt4a, Bt4b])
            for gl in range(2):
                E("dve", lambda e, gl=gl, t=t: e.tensor_copy(out=Mout[gl * 64:(gl + 1) * 64, :, t, 0, gl * 16:(gl + 1) * 16], in_=Zz[0][0][gl * 64:(gl + 1) * 64]), [Zz[0][1]], [BMout])
                E("dve", lambda e, gl=gl, t=t: e.tensor_scalar(out=Mout[gl * 64:(gl + 1) * 64, :, t, 1, gl * 16:(gl + 1) * 16], in0=Zz[1][0][gl * 64:(gl + 1) * 64], scalar1=-1.0, scalar2=None, op0=ALU.mult), [Zz[1][1]], [BMout])

    def seq_reset():
        E("dve", lambda e: e.memset(schalo[:], 0.0), w=[Bschalo])
        E("dve", lambda e: e.memset(putok[:, 0, :], 0.0), w=[Bputok])
        E("dve", lambda e: e.memset(mlhalo[:], 0.0), w=[Bmlhalo])
        E("dve", lambda e: e.memset(C32[:], 0.0), w=[BC32])
        E("dve", lambda e: e.memset(Cbf[:], 0.0), w=[BCbf])
        for c in range(2):
            E("dve", lambda e, c=c: e.memset(car[c][0][:], 0.0), w=[car[c][1]])

    def branch_sconv(l):
        xs, Bxs = SC[0]; prod, Bprod = SC[1]; acc, Bacc = SC[2]; zs, Bzs = SC[3]
        wv, Bw = load_win(l, C_SX)
        for cb in range(4):
            ps, Bp = proj_fm(wv, Bw, cb)
            E("act", lambda e, cb=cb, ps=ps: e.copy(out=xs[:, cb, 0:T], in_=ps[:]), [Bp], [Bxs])
        E("dve", lambda e: e.tensor_copy(out=prod[:, :, 0:2], in_=schalo[:]), [Bschalo], [Bprod])
        wv, Bw = load_win(l, C_SC)
        for cb in range(4):
            ps, Bp = proj_fm(wv, Bw, cb)
            mul("dve", prod[:, cb, 2:T + 2], ps[:], xs[:, cb, 0:T], [Bp, Bxs], [Bprod])
        E("dve", lambda e: e.tensor_copy(out=schalo[:], in_=prod[:, :, T:T + 2]), [Bprod], [Bschalo])
        for cb in range(4):
            E("dve", lambda e, cb=cb: e.tensor_scalar(out=acc[:, cb, 0:T], in0=prod[:, cb, 2:T + 2], scalar1=scw[:, 2, cb:cb + 1], scalar2=None, op0=ALU.mult), [Bprod, Bscw], [Bacc])
            for k in (1, 0):
                E("dve", lambda e, cb=cb, k=k: e.scalar_tensor_tensor(out=acc[:, cb, 0:T], in0=prod[:, cb, k:k + T], scalar=scw[:, k, cb:cb + 1], in1=acc[:, cb, 0:T], op0=ALU.mult, op1=ALU.add),
                  [Bprod, Bscw, Bacc], [Bacc])
        wv, Bw = load_win(l, C_SB)
        for cb in range(4):
            ps, Bp = proj_fm(wv, Bw, cb)
            mul("dve", acc[:, cb, 0:T], ps[:], acc[:, cb, 0:T], [Bp, Bacc], [Bacc])
        wv, Bw = load_win(l, C_SZ)
        for cb in range(4):
            ps, Bp = proj_fm(wv, Bw, cb)
            E("act", lambda e, cb=cb, ps=ps: e.activation(out=zs[:, cb, 0:T], in_=ps[:], func=AF.Silu), [Bp], [Bzs])
            mul("dve", yb[2][0][:, cb, :], acc[:, cb, 0:T], zs[:, cb, 0:T], [Bacc, Bzs], [yb[2][1]])

    def branch_pool(l, first):
        import os
        stage = int(os.environ.get("POOLDBG", "9"))
        zs, Bzs = SC[3]
        wv, Bw = load_win(l, C_PU)
        for tb in range(4):
            if stage == -2:
                continue
            ps, Bp = proj_tm(wv, Bw, tb, 0, 512)
            if stage == -1:
                continue
            E("act", lambda e, tb=tb, ps=ps: e.activation(out=putok[:, tb + 1, :], in_=ps[:], func=AF.Identity), [Bp], [Bputok])
        if stage < 1:
            E("dve", lambda e: e.memset(yb[1][0][:], 0.0), w=[yb[1][1]])
            return
        for g in range(4):
            ps, Bp = nextps()
            for tb in range(4):
                f0 = first and tb == 0
                E("pe", lambda e, g=g, tb=tb, ps=ps, f0=f0: e.matmul(ps[:, tb * 128:(tb + 1) * 128], putok[:, tb + 1, g * 128:(g + 1) * 128], band[:, 2 if f0 else 0, g, :], start=True, stop=f0),
                  [Bputok, Bband], [Bp])
                if not f0:
                    E("pe", lambda e, g=g, tb=tb, ps=ps: e.matmul(ps[:, tb * 128:(tb + 1) * 128], putok[:, tb, g * 128:(g + 1) * 128], band[:, 1, g, :], start=False, stop=True),
                      [Bputok, Bband], [Bp])
            E("dve", lambda e, g=g, ps=ps: e.tensor_copy(out=pooledT[:, g, :], in_=ps[:]), [Bp], [BpooledT])
        E("dve", lambda e: e.tensor_copy(out=putok[:, 0, :], in_=putok[:, 4, :]), [Bputok], [Bputok])
        if stage < 2:
            E("dve", lambda e: e.memset(yb[1][0][:], 0.0), w=[yb[1][1]])
            return
        wv, Bw = load_win(l, C_PZ)
        for g in range(4):
            ps, Bp = proj_fm(wv, Bw, g)
            E("act", lambda e, g=g, ps=ps: e.activation(out=zs[:, g, 0:T], in_=ps[:], func=AF.Silu), [Bp], [Bzs])
            ps2, Bp2 = nextps()
            E("pe", lambda e, g=g, ps2=ps2: e.matmul(ps2[:], poolw[:, g, :], pooledT[:, g, :], start=True, stop=True), [Bpoolw, BpooledT], [Bp2])
            E("dve", lambda e, g=g, ps2=ps2: e.scalar_tensor_tensor(out=yb[1][0][:, g, :], in0=ps2[:], scalar=psc[:, g:g + 1], in1=zs[:, g, 0:T], op0=ALU.mult, op1=ALU.mult),
              [Bp2, Bpsc, Bzs], [yb[1][1]])

    def branch_mlstm(l):
        cq, Bcq = SC[0]; ca, Bca = SC[1]
        for qi, (c0, dst, Bd) in enumerate(((C_Q, qT, BqT), (C_K, kT, BkT))):
            wv, Bw = load_win(l, c0)
            E("dve", lambda e, qi=qi: e.tensor_copy(out=cq[:, :, 0:3], in_=mlhalo[:, qi * 4:(qi + 1) * 4, :]), [Bmlhalo], [Bcq])
            for cb in range(4):
                ps, Bp = proj_fm(wv, Bw, cb)
                E("act", lambda e, cb=cb, ps=ps: e.copy(out=cq[:, cb, 3:T + 3], in_=ps[:]), [Bp], [Bcq])
            E("dve", lambda e, qi=qi: e.tensor_copy(out=mlhalo[:, qi * 4:(qi + 1) * 4, :], in_=cq[:, :, T:T + 3]), [Bcq], [Bmlhalo])
            for cb in range(4):
                ch = qi * 4 + cb
                E("dve", lambda e, cb=cb, ch=ch: e.tensor_scalar(out=ca[:, cb, 0:T], in0=cq[:, cb, 3:T + 3], scalar1=mcw[:, 3, ch:ch + 1], scalar2=None, op0=ALU.mult), [Bcq, Bmcw], [Bca])
                for k in (2, 1, 0):
                    E("dve", lambda e, cb=cb, ch=ch, k=k: e.scalar_tensor_tensor(out=ca[:, cb, 0:T], in0=cq[:, cb, k:k + T], scalar=mcw[:, k, ch:ch + 1], in1=ca[:, cb, 0:T], op0=ALU.mult, op1=ALU.add),
                      [Bcq, Bmcw, Bca], [Bca])
                E("act", lambda e, cb=cb, dst=dst: e.activation(out=dst[:, cb, :], in_=ca[:, cb, 0:T], func=AF.Silu), [Bca], [Bd])
        wv, Bw = load_win(l, C_V)
        for tb in range(4):
            ps, Bp = proj_tm(wv, Bw, tb, 0, 512)
            E("act", lambda e, tb=tb, ps=ps: e.activation(out=vtok[:, tb, :, 0:128], in_=ps[:].rearrange("p (h d) -> p h d", h=4), func=AF.Identity), [Bp], [Bvtok])
        wv, Bw = load_win(l, C_O)
        for tb in range(4):
            ps, Bp = proj_tm(wv, Bw, tb, 0, 512)
            E("act", lambda e, tb=tb, ps=ps: e.activation(out=ogt[:, tb, :], in_=ps[:], func=AF.Sigmoid), [Bp], [Bogt])
        wv, Bw = load_win(l, C_IF, 520)
        psgs = []
        for tb in range(4):
            psgs.append(proj_tm(wv, Bw, tb, 0, 8))
        for tb in range(4):
            ps, Bp = proj_tm(wv, Bw, tb, 8, 512)
            E("act", lambda e, tb=tb, ps=ps: e.activation(out=zwt[:, tb, :], in_=ps[:], func=AF.Silu), [Bp], [Bzwt])
            mul("dve", zwt[:, tb, :], zwt[:, tb, :], mnw[:], [Bzwt, Bmnw], [Bzwt])
        for tb in range(4):
            psg, Bpg = psgs[tb]
            G = gt[:, tb, :]
            tt("dve", G[:, 0:4], psg[:, 0:4], mbi[:], ALU.add, [Bpg, Bmbi], [Bgt])
            tt("dve", G[:, 4:8], psg[:, 4:8], mbf[:], ALU.add, [Bpg, Bmbf], [Bgt])
            E("act", lambda e, G=G: e.activation(out=G[:, 4:8], in_=G[:, 4:8], func=AF.Exp, scale=-1.0), [Bgt], [Bgt])
            E("act", lambda e, G=G: e.activation(out=G[:, 4:8], in_=G[:, 4:8], func=AF.Ln, bias=1.0), [Bgt], [Bgt])
            ps2, Bp2 = nextps()
            E("pe", lambda e, G=G, ps2=ps2: e.matmul(ps2[:, 0:4], trif[:], G[:, 4:8], start=True, stop=True), [Btrif, Bgt], [Bp2])
            E("pe", lambda e, G=G, ps2=ps2: e.matmul(ps2[:, 4:8], onesf[:], G[:, 4:8], start=True, stop=True), [Bonesf, Bgt], [Bp2])
            E("act", lambda e, G=G, ps2=ps2: e.activation(out=G[:, 8:12], in_=ps2[:, 0:4], func=AF.Exp, scale=-1.0, bias=math.log(128 ** -0.5)), [Bp2], [Bgt])
            tt("dve", G[:, 24:28], G[:, 0:4], ps2[:, 0:4], ALU.add, [Bgt, Bp2], [Bgt])
            E("act", lambda e, G=G: e.activation(out=G[:, 12:16], in_=G[:, 24:28], func=AF.Exp), [Bgt], [Bgt])
            tt("dve", G[:, 24:28], G[:, 24:28], ps2[:, 4:8], ALU.subtract, [Bgt, Bp2], [Bgt])
            E("act", lambda e, G=G: e.activation(out=G[:, 16:20], in_=G[:, 24:28], func=AF.Exp), [Bgt], [Bgt])
            E("act", lambda e, G=G, ps2=ps2: e.activation(out=G[:, 20:24], in_=ps2[:, 4:8], func=AF.Exp, scale=-1.0), [Bp2], [Bgt])
        pbt = None
        for tb in range(4):
            G = gt[:, tb, :]
            sl = slice(tb * 128, (tb + 1) * 128)
            stA = {}
            def stage_a(hd):
                pk, Bpk = nextps()
                pkb = pk[:].bitcast(BF16)
                E("pe", lambda e, hd=hd, pkb=pkb: e.transpose(pkb[:, 0:128], kT[:, hd, sl], identb[:]), [BkT, Bidb], [Bpk])
                E("dve", lambda e, hd=hd, pkb=pkb, G=G: e.tensor_scalar(out=ktil2[hd % 2][0][:], in0=pkb[:, 0:128], scalar1=G[:, 16 + hd:17 + hd], scalar2=None, op0=ALU.mult), [Bpk, Bgt], [ktil2[hd % 2][1]])
                pS, BpS = nextps()
                E("pe", lambda e, hd=hd, pS=pS: e.matmul(pS[:, 0:128], kT[:, hd, sl], qT[:, hd, sl], start=True, stop=True), [BkT, BqT], [BpS])
                E("dve", lambda e, hd=hd, pS=pS, G=G: e.scalar_tensor_tensor(out=PT2[hd % 2][0][:], in0=pS[:, 0:128], scalar=G[:, 12 + hd:13 + hd], in1=trif[:], op0=ALU.mult, op1=ALU.mult), [BpS, Bgt, Btrif], [PT2[hd % 2][1]])
                stA[hd] = True
            def stage_b(hd):
                pN, BpN = nextps()
                E("pe", lambda e, hd=hd, pN=pN: e.matmul(pN[:, 0:129], PT2[hd % 2][0][:], vtok[:, tb, hd, :], start=True, stop=False), [PT2[hd % 2][1], Bvtok], [BpN])
                E("pe", lambda e, hd=hd, pN=pN: e.matmul(pN[:, 0:129], qT[:, hd, sl], Cbf[:, hd, :], start=False, stop=True), [BqT, BCbf], [BpN])
                pC, BpC = nextps()
                E("pe", lambda e, hd=hd, pC=pC: e.matmul(pC[:, 0:129], ktil2[hd % 2][0][:], vtok[:, tb, hd, :], start=True, stop=True), [ktil2[hd % 2][1], Bvtok], [BpC])
                E("dve", lambda e, hd=hd, pC=pC, G=G: e.scalar_tensor_tensor(out=C32[:, hd, :], in0=C32[:, hd, :], scalar=G[:, 20 + hd:21 + hd], in1=pC[:, 0:129], op0=ALU.mult, op1=ALU.add), [BC32, Bgt, BpC], [BC32])
                E("dve", lambda e, hd=hd: e.tensor_copy(out=Cbf[:, hd, :], in_=C32[:, hd, :]), [BC32], [BCbf])
                E("act", lambda e, hd=hd, pN=pN, G=G: e.activation(out=sm[:, 0:1], in_=pN[:, 128:129], func=AF.Abs, scale=G[:, 8 + hd:9 + hd]), [BpN, Bgt], [Bsm])
                E("dve", lambda e: e.tensor_scalar(out=sm[:, 0:1], in0=sm[:, 0:1], scalar1=1.0, scalar2=None, op0=ALU.max), [Bsm], [Bsm])
                E("dve", lambda e: e.reciprocal(out=sm[:, 1:2], in_=sm[:, 0:1]), [Bsm], [Bsm])
                mul("dve", sm[:, 2:3], sm[:, 1:2], G[:, 8 + hd:9 + hd], [Bsm, Bgt], [Bsm])
                E("dve", lambda e, hd=hd, pN=pN: e.scalar_tensor_tensor(out=hh[:, hd, :], in0=pN[:, 0:128], scalar=sm[:, 2:3], in1=ogt[:, tb, hd * 128:(hd + 1) * 128], op0=ALU.mult, op1=ALU.mult), [BpN, Bsm, Bogt], [Bhh])
                E("act", lambda e, hd=hd: e.activation(out=junk[:, 0:128], in_=hh[:, hd, :], func=AF.Square, accum_out=sm[:, 4 + hd:5 + hd]), [Bhh], [Bjunk, Bsm])
            stage_a(0); stage_a(1); stage_b(0); stage_a(2); stage_b(1); stage_a(3); stage_b(2); stage_b(3)
            E("dve", lambda e: e.tensor_scalar(out=sm[:, 8:12], in0=sm[:, 4:8], scalar1=1.0 / 128, scalar2=EPS, op0=ALU.mult, op1=ALU.add), [Bsm], [Bsm])
            E("act", lambda e: e.activation(out=sm[:, 8:12], in_=sm[:, 8:12], func=AF.Sqrt), [Bsm], [Bsm])
            E("dve", lambda e: e.reciprocal(out=sm[:, 12:16], in_=sm[:, 8:12]), [Bsm], [Bsm])
            for hd in range(4):
                E("dve", lambda e, hd=hd: e.scalar_tensor_tensor(out=ydtok[:, hd * 128:(hd + 1) * 128], in0=hh[:, hd, :], scalar=sm[:, 12 + hd:13 + hd], in1=zwt[:, tb, hd * 128:(hd + 1) * 128], op0=ALU.mult, op1=ALU.mult),
                  [Bhh, Bsm, Bzwt], [Bydtok])
            pt, Bpt = nextps()
            ptb = pt[:].bitcast(BF16)
            for cb in range(4):
                E("pe", lambda e, cb=cb, ptb=ptb: e.transpose(ptb[:, cb * 128:(cb + 1) * 128], ydtok[:, cb * 128:(cb + 1) * 128], identb[:]), [Bydtok, Bidb], [Bpt])
            E("act", lambda e, ptb=ptb: e.copy(out=yb[3][0][:, :, sl], in_=ptb[:, 0:512].rearrange("p (c t) -> p c t", c=4)), [Bpt], [yb[3][1]])

    def s5_p1(l):
        wv, Bw = load_win(l, C_S5U)
        for cb in range(4):
            ps, Bp = proj_fm(wv, Bw, cb)
            E("act", lambda e, cb=cb, ps=ps: e.activation(out=uT[:, cb, :], in_=ps[:], func=AF.Identity), [Bp], [BuT])
        uS = uT[:].rearrange("p g (c s) -> p g s c", s=8)
        Er, Ei = Etab[0][0], Etab[1][0]
        BE = [Etab[0][1], Etab[1][1]]
        Xps = {}
        for c in range(2):
            for hf in range(2):
                Xps[(c, hf)] = nextps()
        for gp in range(16):
            gq, j = divmod(gp, 4)
            for c in range(2):
                ps, Bp = Xps[(c, gp // 8)]
                pv = ps[:].rearrange("p (g c) -> p g c", g=8)
                for s in range(8):
                    E("pe", lambda e, pv=pv, gp=gp, gq=gq, j=j, c=c, s=s: e.matmul(pv[:, gp % 8, :], Min[32 * j:32 * j + 32, gq, s, c, :], uS[32 * j:32 * j + 32, gq, s, :],
                                                                        start=(s == 0), stop=(s == 7), tile_position=(32 * j, 0)), [BMin, BuT], [Bp])
        for hf in range(2):
            gs = slice(hf * 8, hf * 8 + 8)
            xr, Bxr = Xps[(0, hf)]; xi, Bxi = Xps[(1, hf)]
            xrv = xr[:].rearrange("p (g c) -> p g c", g=8); xiv = xi[:].rearrange("p (g c) -> p g c", g=8)
            t1, Bt1 = s5t[0]; t2, Bt2 = s5t[1]
            mul("dve", t1[:, gs, :], xrv, Er[:, gs, :], [Bxr, BE[0]], [Bt1]); mul("dve", t2[:, gs, :], xiv, Ei[:, gs, :], [Bxi, BE[1]], [Bt2])
            tt("dve", Xr[0][0][:, gs, :], t1[:, gs, :], t2[:, gs, :], ALU.subtract, [Bt1, Bt2], [Xr[0][1]])
            mul("dve", t1[:, gs, :], xiv, Er[:, gs, :], [Bxi, BE[0]], [Bt1]); mul("dve", t2[:, gs, :], xrv, Ei[:, gs, :], [Bxr, BE[1]], [Bt2])
            tt("dve", Xr[1][0][:, gs, :], t1[:, gs, :], t2[:, gs, :], ALU.add, [Bt1, Bt2], [Xr[1][1]])
        for c in range(2):
            for gp in range(16):
                E("dve", lambda e, c=c, gp=gp: e.tensor_tensor_scan(out=Hs[c][0][:, gp, :], data0=rho[:, gp:gp + 1].to_broadcast([128, 64]), data1=Xr[c][0][:, gp, :], initial=car[c][0][:, gp:gp + 1], op0=ALU.mult, op1=ALU.add),
                  [Brho, Xr[c][1], car[c][1]], [Hs[c][1]])
        t1, Bt1 = s5t[0]; t2, Bt2 = s5t[1]
        mul("dve", t1[:], Er[:], Hs[0][0][:], [BE[0], Hs[0][1]], [Bt1]); mul("dve", t2[:], Ei[:], Hs[1][0][:], [BE[1], Hs[1][1]], [Bt2])
        tt("dve", Hu[0][0][:], t1[:], t2[:], ALU.add, [Bt1, Bt2], [Hu[0][1]])
        mul("dve", t1[:], Er[:], Hs[1][0][:], [BE[0], Hs[1][1]], [Bt1]); mul("dve", t2[:], Ei[:], Hs[0][0][:], [BE[1], Hs[0][1]], [Bt2])
        tt("dve", Hu[1][0][:], t1[:], t2[:], ALU.subtract, [Bt1, Bt2], [Hu[1][1]])
        for c in range(2):
            E("dve", lambda e, c=c: e.tensor_copy(out=Hp[c][0][:, :, 0], in_=car[c][0][:]), [car[c][1]], [Hp[c][1]])
            E("dve", lambda e, c=c: e.tensor_copy(out=Hp[c][0][:, :, 1:64], in_=Hu[c][0][:, :, 0:63]), [Hu[c][1]], [Hp[c][1]])
            E("dve", lambda e, c=c: e.tensor_copy(out=car[c][0][:], in_=Hu[c][0][:, :, 63]), [Hu[c][1]], [car[c][1]])

    def s5_p2a(l):
        ya, Bya = SC[1]; yg, Byg = SC[2]
        uS = uT[:].rearrange("p g (c s) -> p g s c", s=8)
        for gq in range(4):
            ps, Bp = nextps()
            pv = ps[:].rearrange("p (t c) -> p t c", t=8)
            for t in range(8):
                for s in range(t + 1):
                    E("pe", lambda e, pv=pv, gq=gq, t=t, s=s: e.matmul(pv[:, t, :], Ktoe[:, gq, t - s, :], uS[:, gq, s, :], start=(s == 0), stop=False), [BKtoe, BuT], [Bp])
                for j in range(4):
                    gp = 4 * gq + j
                    for c in range(2):
                        E("pe", lambda e, pv=pv, gp=gp, j=j, t=t, c=c: e.matmul(pv[32 * j:32 * j + 32, t, :], Mout[:, gp, t, c, :], Hp[c][0][:, gp, :], start=False, stop=(c == 1), tile_position=(0, 32 * j)),
                          [BMout, Hp[c][1]], [Bp])
            E("dve", lambda e, gq=gq, pv=pv: e.scalar_tensor_tensor(out=ya[:, gq, 0:T].rearrange("p (c t) -> p c t", t=8), in0=u32[:, gq, :].rearrange("p (c t) -> p c t", t=8), scalar=s5dd[:, gq:gq + 1],
                                                                    in1=pv.rearrange("p t c -> p c t"), op0=ALU.mult, op1=ALU.add), [Bu32, Bs5dd, Bp], [Bya])
        t1, Bt1 = SC[0]
        for cb in range(4):
            Y = ya[:, cb, 0:T]
            mul("dve", t1[:, cb, 0:T], Y, Y, [Bya], [Bt1])
            E("dve", lambda e, cb=cb: e.tensor_scalar(out=t1[:, cb, 0:T], in0=t1[:, cb, 0:T], scalar1=0.044715, scalar2=1.0, op0=ALU.mult, op1=ALU.add), [Bt1], [Bt1])
            mul("dve", t1[:, cb, 0:T], t1[:, cb, 0:T], Y, [Bt1, Bya], [Bt1])
            E("act", lambda e, cb=cb: e.activation(out=t1[:, cb, 0:T], in_=t1[:, cb, 0:T], func=AF.Sigmoid, scale=2.0 * math.sqrt(2.0 / math.pi)), [Bt1], [Bt1])
            mul("dve", yg[:, cb, 0:T], t1[:, cb, 0:T], Y, [Bt1, Bya], [Byg])
            E("dve", lambda e, cb=cb: e.tensor_copy(out=uT[:, cb, :], in_=yg[:, cb, 0:T]), [Byg], [BuT])

    def s5_p2b(l):
        yg, Byg = SC[2]
        B3 = SC[3][1]
        t1v = SC[3][0][:, 2, 0:T]
        zsv = SC[3][0][:, 3, 0:T]
        gv, Bs = load_img(l, ("glu",))
        wv, Bw = load_win(l, C_S5Z)
        for cb in range(4):
            ps, Bp = nextps()
            for kc in range(4):
                E("pe", lambda e, cb=cb, kc=kc, ps=ps: e.matmul(ps[:], gv[:, kc, cb * 128:(cb + 1) * 128], uT[:, kc, :], start=(kc == 0), stop=(kc == 3)), [Bs, BuT], [Bp])
            E("act", lambda e, cb=cb, ps=ps: e.activation(out=t1v, in_=ps[:], func=AF.Sigmoid), [Bp], [B3])
            mul("dve", yg[:, cb, 0:T], yg[:, cb, 0:T], t1v, [Byg, B3], [Byg])
            ps, Bp = proj_fm(wv, Bw, cb)
            E("act", lambda e, cb=cb, ps=ps: e.activation(out=zsv, in_=ps[:], func=AF.Silu), [Bp], [B3])
            mul("dve", yb[0][0][:, cb, :], yg[:, cb, 0:T], zsv, [Byg, B3], [yb[0][1]])

    Bx4 = [Buf("x%d" % i) for i in range(4)]
    xds4 = [kb.dsem("xds4_%d" % i) for i in range(4)]
    ods4 = [kb.dsem("ods4_%d" % i) for i in range(4)]
    stp, Bstp = sbt([128, 16], F32, "stp")

    def dram_io(l, s, t):
        if L == 1:
            return x_d, y_d, ("x", s, t), ("y", s, t)
        src_d = x_d if l == 0 else scr_d[(l - 1) % 2]
        dst_d = y_d if l == L - 1 else scr_d[l % 2]
        skey = ("x", s, t) if l == 0 else ((l - 1) % 2, s, t)
        dkey = ("y", s, t) if l == L - 1 else (l % 2, s, t)
        return src_d, dst_d, skey, dkey

    def emit_load_prenorm(l, s, t, tb, do_b=True):
        xtile = xt[0][0]
        Bx = Bx4[tb]
        src_d, _, skey, _ = dram_io(l, s, t)
        hblk = mrgT[:, :, tb * 128:(tb + 1) * 128]
        kb.dma("sp", xds4[tb], lambda q: q.dma_start(out=xtile[:, tb, :], in_=src_d[s, t * T + tb * 128:t * T + (tb + 1) * 128, :]), reads=[dbuf(skey)], writes=[Bx])
        E("act", lambda e: e.activation(out=junk[:], in_=xtile[:, tb, :], func=AF.Square, accum_out=stp[:, tb:tb + 1]), [Bx], [Bjunk, Bstp])
        E("dve", lambda e: e.tensor_scalar(out=stp[:, 4 + tb:5 + tb], in0=stp[:, tb:tb + 1], scalar1=1.0 / D, scalar2=EPS, op0=ALU.mult, op1=ALU.add), [Bstp], [Bstp])
        E("act", lambda e: e.activation(out=stp[:, 4 + tb:5 + tb], in_=stp[:, 4 + tb:5 + tb], func=AF.Sqrt), [Bstp], [Bstp])
        E("dve", lambda e: e.reciprocal(out=stp[:, 8 + tb:9 + tb], in_=stp[:, 4 + tb:5 + tb]), [Bstp], [Bstp])
        E("dve", lambda e: e.scalar_tensor_tensor(out=hblk, in0=xtile[:, tb, :].rearrange("p (k c) -> p k c", k=8), scalar=stp[:, 8 + tb:9 + tb], in1=npw[:].rearrange("p (k c) -> p k c", k=8), op0=ALU.mult, op1=ALU.mult),
          [Bx, Bstp, Bnpw], [BmrgT[tb]])
        if do_b:
            emit_transposes(tb)

    def emit_transposes(tb):
        pt, Bpt = nextps()
        ptb = pt[:].bitcast(BF16)
        for kc in range(8):
            E("pe", lambda e, kc=kc, ptb=ptb: e.transpose(ptb[:, kc * 128:(kc + 1) * 128], mrgT[:, kc, tb * 128:(tb + 1) * 128], identb[:]), [BmrgT[tb], Bidb], [Bpt])
        E("act", lambda e, ptb=ptb: e.copy(out=hT[:, :, tb * 128:(tb + 1) * 128], in_=ptb.rearrange("p (k c) -> p k c", k=8)), [Bpt], [BhT])

    def tile_layer(l, s, t, prenorm_done, nxt):
        xtile = xt[0][0]
        _, dst_d, _, dkey = dram_io(l, s, t)
        if not prenorm_done:
            for tb in range(4):
                emit_load_prenorm(l, s, t, tb)
        for b in range(4):
            if not en[b]:
                E("dve", lambda e, b=b: e.memset(yb[b][0][:], 0.0), w=[yb[b][1]])
        if en[2]:
            branch_sconv(l)
        if en[1]:
            branch_pool(l, t == 0)
        if en[0]:
            s5_p1(l)
        if en[3]:
            branch_mlstm(l)
        if en[0]:
            s5_p2a(l)
        merge_pass(l, [1, 2, 3], True, False)
        if en[0]:
            s5_p2b(l)
        if dbg and t == NT - 1 and s == 0 and l == L - 1:
            for b in range(4):
                dt_, Bdt = SC[2]
                E("act", lambda e, b=b: e.copy(out=dt_[:, :, 0:T], in_=yb[b][0][:]), [yb[b][1]], [Bdt])
                kb.dma("sp", ods, lambda q, b=b: q.dma_start(out=dbg_d[b], in_=dt_[:, :, 0:T]), reads=[Bdt])
        merge_pass(l, [0], False, True)
        out_proj(l, s, t, nxt)

    def merge_pass(l, blist, init, final):
        for bi, b in enumerate(blist):
            first = init and bi == 0
            lastb = final and bi == len(blist) - 1
            for hf in range(2):
                wbv, Bs = load_img(l, ("wbr", b, hf))
                gvw, Bg = load_win(l, C_G + b * 1024 + hf * 512)
                for jj in range(4):
                    j = hf * 4 + jj
                    ps, Bp = proj_fm(gvw, Bg, jj)
                    gs_, Bgs = gsb[0]
                    E("act", lambda e, ps=ps, gs_=gs_: e.activation(out=gs_[:], in_=ps[:], func=AF.Sigmoid), [Bp], [Bgs])
                    ps2, Bp2 = nextps()
                    for cc in range(4):
                        E("pe", lambda e, cc=cc, jj=jj, ps2=ps2, b=b: e.matmul(ps2[:], wbv[:, cc, jj * 128:(jj + 1) * 128], yb[b][0][:, cc, :], start=(cc == 0), stop=(cc == 3)), [Bs, yb[b][1]], [Bp2])
                    if first:
                        mul("dve", SC[j // 4][0][:, j % 4, 0:T], ps2[:], gs_[:], [Bp2, Bgs], [SC[j // 4][1]])
                    else:
                        tm_, Btm = tmpm[0]
                        mul("dve", tm_[:], ps2[:], gs_[:], [Bp2, Bgs], [Btm])
                        if not lastb:
                            tt("dve", SC[j // 4][0][:, j % 4, 0:T], SC[j // 4][0][:, j % 4, 0:T], tm_[:], ALU.add, [SC[j // 4][1], Btm], [SC[j // 4][1]])
                        else:
                            tt("dve", mrgT[:, j, :], SC[j // 4][0][:, j % 4, 0:T], tm_[:], ALU.add, [SC[j // 4][1], Btm], [BmrgT])

    def out_proj(l, s, t, nxt):
        xtile = xt[0][0]
        _, dst_d, _, dkey = dram_io(l, s, t)
        wo = []
        for hf in range(2):
            wo.append([load_img(l, ("wo", hf, q2)) for q2 in range(2)])
        for tb in range(4):
            pss = []
            for hf in range(2):
                ps, Bp = nextps()
                for q2 in range(2):
                    for j in range(8):
                        E("pe", lambda e, j=j, ps=ps, hf=hf, q2=q2: e.matmul(ps[:, q2 * 256:(q2 + 1) * 256], mrgT[:, j, tb * 128:(tb + 1) * 128], wo[hf][q2][0][:, j, :], start=(j == 0), stop=(j == 7)),
                          [BmrgT[tb], wo[hf][q2][1]], [Bp])
                E("act", lambda e, ps=ps, hf=hf: e.activation(out=junk[:, 0:512], in_=ps[:], func=AF.Square, accum_out=st4[:, 12 + hf:13 + hf]), [Bp], [Bjunk, Bst4])
                pss.append((ps, Bp))
            tt("dve", st4[:, 14:15], st4[:, 12:13], st4[:, 13:14], ALU.add, [Bst4], [Bst4])
            E("dve", lambda e: e.tensor_scalar(out=st4[:, 14:15], in0=st4[:, 14:15], scalar1=1.0 / D, scalar2=EPS, op0=ALU.mult, op1=ALU.add), [Bst4], [Bst4])
            E("act", lambda e: e.activation(out=st4[:, 14:15], in_=st4[:, 14:15], func=AF.Sqrt), [Bst4], [Bst4])
            E("dve", lambda e: e.reciprocal(out=st4[:, 15:16], in_=st4[:, 14:15]), [Bst4], [Bst4])
            for hf in range(2):
                ps, Bp = pss[hf]
                E("dve", lambda e, ps=ps, hf=hf: e.scalar_tensor_tensor(out=junk[:, hf * 512:(hf + 1) * 512], in0=ps[:], scalar=st4[:, 15:16], in1=ppw[:, hf * 512:(hf + 1) * 512], op0=ALU.mult, op1=ALU.mult),
                  [Bp, Bst4, Bppw], [Bjunk])
            tt("dve", xtile[:, tb, :], xtile[:, tb, :], junk[:], ALU.add, [Bx4[tb], Bjunk], [Bx4[tb]])
            kb.dma("sp", ods4[tb], lambda q, tb=tb: q.dma_start(out=dst_d[s, t * T + tb * 128:t * T + (tb + 1) * 128, :], in_=xtile[:, tb, :]), reads=[Bx4[tb]], writes=[dbuf(dkey)])
            if nxt is not None:
                emit_load_prenorm(nxt[0], nxt[1], nxt[2], tb, do_b=False)
        if nxt is not None:
            for tb in range(4):
                emit_transposes(tb)
        return None

    last = None
    convert_layer(0)
    for l in range(L):
        if l > 0:
            kb.new_epoch("_%d" % l)
        layer_prep(l)
        if l + 1 < L:
            convert_layer(l + 1)
        order = [(s, t) for s in range(NSEQ) for t in range(NT)]
        for i, (s, t) in enumerate(order):
            if t == 0:
                seq_reset()
            nxt = (l, order[i + 1][0], order[i + 1][1]) if i + 1 < len(order) else None
            if not PIPE:
                nxt = None
            tile_layer(l, s, t, (i > 0) and PIPE, nxt)
    kb._wait("sp", [Tok(d_.sem, d_.cnt) for d_ in ods4] + [Tok(ods.sem, ods.cnt)])
    es.close()
    return nc, kb


_CACHE = {}
WNAMES = ["norm_pre_w", "norm_post_w", "w_in", "s5_A_re", "s5_A_im", "s5_log_dt", "s5_B_re", "s5_B_im", "s5_C_re", "s5_C_im",
          "s5_D", "s5_w_glu", "pool_w", "pool_scale", "sconv_w", "mlstm_conv_w", "mlstm_b_i", "mlstm_b_f", "mlstm_norm_w",
          "w_branch", "w_out"]


def run_layers(x, weights, ncores, en=(1, 1, 1, 1), dbg=False):
    B, S, _ = x.shape
    nseq = B // ncores
    NT = S // T
    Lw = weights["w_in"].shape[0]
    key = (nseq, NT, en, dbg)
    consts = host_consts()
    cur = np.ascontiguousarray(x, dtype=np.float32)
    dbg_out = None
    for l in range(Lw):
        nc, _ = build(1, nseq, NT, en, dbg)
        in_maps = []
        for c in range(ncores):
            m = {"x": cur[c * nseq:(c + 1) * nseq]}
            for k in WNAMES:
                m[k] = np.ascontiguousarray(weights[k][l:l + 1], dtype=np.float32)
            m.update(consts)
            in_maps.append(m)
        res = run_bass_kernel_spmd(nc, in_maps, core_ids=list(range(ncores)))
        cur = np.concatenate([r["y"] for r in res.results], axis=0)
        if dbg:
            dbg_out = res.results[0]["dbg"]
    return (cur, dbg_out) if dbg else cur


def run_fused(x, weights, ncores, dbg=False):
    B, S, _ = x.shape
    nseq = B // ncores
    NT = S // T
    Lw = weights["w_in"].shape[0]
    consts = host_consts()
    nc, _ = build(Lw, nseq, NT, (1, 1, 1, 1), dbg)
    xs = np.ascontiguousarray(x, dtype=np.float32)
    wts = {k: np.ascontiguousarray(weights[k], dtype=np.float32) for k in WNAMES}
    in_maps = []
    for c in range(ncores):
        m = {"x": xs[c * nseq:(c + 1) * nseq]}
        m.update(wts)
        m.update(consts)
        in_maps.append(m)
    res = run_bass_kernel_spmd(nc, in_maps, core_ids=list(range(ncores)))
    return np.concatenate([r["y"] for r in res.results], axis=0)


def kernel(**inputs):
    x = inputs["x"]
    weights = {k: inputs[k] for k in WNAMES}
    return run_fused(x, weights, 8).astype(np.float32)
```
